# Optimizing a Trainium2 kernel written in Bass

```python
import jax, jax.numpy as jnp
from jax import lax
import numpy as np

D_MODEL = 2048
BATCH = 2
SEQ = 16384
DEPTH = 1

PE_DIM = 256
MIX_WIDTH = D_MODEL
ML_WIDTH = MIX_WIDTH // 2
ML_HEADS = 4
ML_DV = ML_WIDTH // ML_HEADS
ML_DQK = ML_DV // 2
ML_QK = ML_HEADS * ML_DQK
ML_CONV = 4
ML_CHUNK = 64
HG_WIDTH = MIX_WIDTH - ML_WIDTH
HG_EXPAND = 128
HG_HEADS = HG_WIDTH // HG_EXPAND
HG_DV = HG_WIDTH // HG_HEADS
HG_FDIM = HG_HEADS * HG_EXPAND
HG_CHUNK = 64
EPS = 1e-6

SPLIT_SIZES = (ML_QK, ML_QK, ML_WIDTH, ML_WIDTH, ML_WIDTH, ML_HEADS, ML_HEADS,
               HG_FDIM, HG_FDIM, HG_WIDTH, HG_WIDTH)
IN_COLS = sum(SPLIT_SIZES)

kernel_name = "hymba_mlstm_hgrn2_block"


def rmsnorm(x, w):
    xf = x.astype(jnp.float32)
    return xf * lax.rsqrt(jnp.mean(xf * xf, axis=-1, keepdims=True) + EPS) * w.astype(jnp.float32)


def causal_dwconv(x, w, b):
    K = w.shape[0]
    S = x.shape[1]
    xp = jnp.pad(x, ((0, 0), (K - 1, 0), (0, 0)))
    y = xp[:, 0:S, :] * w[0]
    for k in range(1, K):
        y = y + xp[:, k:k + S, :] * w[k]
    return y + b


def to_chunks(x, L):
    Bn, S = x.shape[0], x.shape[1]
    x = x.reshape((Bn, S // L, L) + x.shape[2:])
    perm = (1, 0, 3, 2) + tuple(range(4, x.ndim))
    return x.transpose(perm)


def from_chunks(y):
    NC, Bn, H, L, d = y.shape
    return y.transpose(1, 0, 3, 2, 4).reshape(Bn, NC * L, H, d)


def mlstm_chunkwise(q, k, v, i_pre, f_pre):
    L = ML_CHUNK
    Bn, S, H, dqk = q.shape
    dv = v.shape[-1]
    k = k * (dqk ** -0.5)
    logf = jax.nn.log_sigmoid(f_pre)
    xs = (to_chunks(q, L), to_chunks(k, L), to_chunks(v, L), to_chunks(i_pre, L), to_chunks(logf, L))
    causal = jnp.tril(jnp.ones((L, L), dtype=bool))

    def step(carry, inp):
        C, n, m = carry
        qc, kc, vc, ig, lf = inp
        b = jnp.cumsum(lf, axis=-1)
        logD = jnp.where(causal, b[..., :, None] - b[..., None, :] + ig[..., None, :], -jnp.inf)
        inter_log = b + m[..., None]
        m_t = jnp.maximum(inter_log, jnp.max(logD, axis=-1))
        scores = jnp.einsum('bhtd,bhjd->bhtj', qc, kc) * jnp.exp(logD - m_t[..., None])
        w_inter = jnp.exp(inter_log - m_t)
        num = (jnp.einsum('bhtj,bhjv->bhtv', scores, vc)
               + w_inter[..., None] * jnp.einsum('bhtd,bhdv->bhtv', qc, C))
        den = jnp.sum(scores, axis=-1) + w_inter * jnp.einsum('bhtd,bhd->bht', qc, n)
        h = num / jnp.maximum(jnp.abs(den), jnp.exp(-m_t))[..., None]
        g = b[..., -1]
        a = g[..., None] - b + ig
        m_new = jnp.maximum(g + m, jnp.max(a, axis=-1))
        wa = jnp.exp(a - m_new[..., None])
        ws = jnp.exp(g + m - m_new)
        C_new = ws[..., None, None] * C + jnp.einsum('bhj,bhjd,bhjv->bhdv', wa, kc, vc)
        n_new = ws[..., None] * n + jnp.einsum('bhj,bhjd->bhd', wa, kc)
        return (C_new, n_new, m_new), h

    init = (jnp.zeros((Bn, H, dqk, dv), jnp.float32),
            jnp.zeros((Bn, H, dqk), jnp.float32),
            jnp.zeros((Bn, H), jnp.float32))
    _, hs = lax.scan(step, init, xs)
    return from_chunks(hs)


def hgrn2_chunkwise(q, k, logf, iv):
    L = HG_CHUNK
    Bn, S, H, E = q.shape
    dv = iv.shape[-1]
    xs = (to_chunks(q, L), to_chunks(k, L), to_chunks(logf, L), to_chunks(iv, L))
    causal = jnp.tril(jnp.ones((L, L), dtype=bool))[:, :, None]

    def step(Sst, inp):
        qc, kc, lf, vc = inp
        b = jnp.cumsum(lf, axis=2)
        decay = jnp.exp(jnp.where(causal, b[:, :, :, None, :] - b[:, :, None, :, :], -jnp.inf))
        A = jnp.einsum('bhte,bhje,bhtje->bhtj', qc, kc, decay)
        o = (jnp.einsum('bhtj,bhjv->bhtv', A, vc)
             + jnp.einsum('bhte,bhev->bhtv', qc * jnp.exp(b), Sst))
        g = b[:, :, -1, :]
        S_new = (jnp.exp(g)[..., None] * Sst
                 + jnp.einsum('bhje,bhjv->bhev', kc * jnp.exp(g[:, :, None, :] - b), vc))
        return S_new, o

    _, os_ = lax.scan(step, jnp.zeros((Bn, H, E, dv), jnp.float32), xs)
    return from_chunks(os_)


def setup_inputs(seed: int = 0) -> dict:
    key = jax.random.key(seed)
    ks = jax.random.split(key, 16)
    f32 = jnp.float32
    nrm = lambda k, shape, s: jax.random.normal(k, shape, f32) * s
    x = jax.random.normal(ks[0], (BATCH, SEQ, D_MODEL), f32)
    p = jax.random.normal(ks[1], (DEPTH, BATCH, SEQ, PE_DIM), f32)
    norm_w = 1.0 + nrm(ks[2], (DEPTH, D_MODEL), 0.05)
    w_in = nrm(ks[3], (DEPTH, D_MODEL, IN_COLS), D_MODEL ** -0.5)
    conv_w = nrm(ks[4], (DEPTH, ML_CONV, 2 * ML_QK), ML_CONV ** -0.5)
    conv_b = nrm(ks[5], (DEPTH, 2 * ML_QK), 0.02)
    ml_b_i = nrm(ks[6], (DEPTH, ML_HEADS), 0.1)
    ml_b_f = jnp.linspace(3.0, 6.0, ML_HEADS, dtype=f32)[None, :] + nrm(ks[7], (DEPTH, ML_HEADS), 0.1)
    ml_norm_w = 1.0 + nrm(ks[8], (DEPTH, ML_WIDTH), 0.05)
    hg_lb = nrm(ks[9], (DEPTH + 1, HG_FDIM), 0.1)
    hg_norm_w = 1.0 + nrm(ks[10], (DEPTH, HG_WIDTH), 0.05)
    w_out = nrm(ks[11], (DEPTH, MIX_WIDTH, D_MODEL), MIX_WIDTH ** -0.5)
    pe_norm_w = 1.0 + nrm(ks[12], (DEPTH, D_MODEL), 0.05)
    w_pg = nrm(ks[13], (DEPTH, D_MODEL, D_MODEL), D_MODEL ** -0.5)
    w_pe = nrm(ks[14], (DEPTH, PE_DIM, D_MODEL), PE_DIM ** -0.5)
    final_norm_w = 1.0 + nrm(ks[15], (D_MODEL,), 0.05)
    return {"x": x, "p": p, "norm_w": norm_w, "w_in": w_in, "conv_w": conv_w,
            "conv_b": conv_b, "ml_b_i": ml_b_i, "ml_b_f": ml_b_f, "ml_norm_w": ml_norm_w,
            "hg_lb": hg_lb, "hg_norm_w": hg_norm_w, "w_out": w_out, "pe_norm_w": pe_norm_w,
            "w_pg": w_pg, "w_pe": w_pe, "final_norm_w": final_norm_w}


def reference(x, p, norm_w, w_in, conv_w, conv_b, ml_b_i, ml_b_f, ml_norm_w,
              hg_lb, hg_norm_w, w_out, pe_norm_w, w_pg, w_pe, final_norm_w):
    Bn, S, _ = x.shape
    split_points = [int(s) for s in np.cumsum(SPLIT_SIZES)[:-1]]
    lb_all = jnp.cumsum(jax.nn.softmax(hg_lb.astype(jnp.float32), axis=0), axis=0)
    h = x.astype(jnp.float32)
    for l in range(DEPTH):
        u = rmsnorm(h, norm_w[l])
        proj = jnp.einsum('bsd,dc->bsc', u, w_in[l].astype(jnp.float32))
        (ml_q, ml_k, ml_v, ml_o, ml_z, ml_i, ml_f,
         hg_q, hg_f, hg_i, hg_g) = jnp.split(proj, split_points, axis=-1)

        qk = jax.nn.silu(causal_dwconv(jnp.concatenate([ml_q, ml_k], axis=-1),
                                       conv_w[l].astype(jnp.float32), conv_b[l].astype(jnp.float32)))
        q_a = qk[..., :ML_QK].reshape(Bn, S, ML_HEADS, ML_DQK)
        k_a = qk[..., ML_QK:].reshape(Bn, S, ML_HEADS, ML_DQK)
        v_a = ml_v.reshape(Bn, S, ML_HEADS, ML_DV)
        i_pre = ml_i + ml_b_i[l].astype(jnp.float32)
        f_pre = ml_f + ml_b_f[l].astype(jnp.float32)
        h_a = mlstm_chunkwise(q_a, k_a, v_a, i_pre, f_pre)
        h_a = rmsnorm(h_a, ml_norm_w[l].reshape(ML_HEADS, ML_DV)).reshape(Bn, S, ML_WIDTH)
        y_a = jax.nn.sigmoid(ml_o) * h_a * jax.nn.silu(ml_z)

        lb = lb_all[l]
        f_b = lb + (1.0 - lb) * jax.nn.sigmoid(hg_f)
        logf_b = jnp.log(f_b).reshape(Bn, S, HG_HEADS, HG_EXPAND)
        k_b = ((1.0 - lb) * jax.nn.sigmoid(-hg_f)).reshape(Bn, S, HG_HEADS, HG_EXPAND)
        q_b = hg_q.reshape(Bn, S, HG_HEADS, HG_EXPAND)
        i_b = hg_i.reshape(Bn, S, HG_HEADS, HG_DV)
        h_b = hgrn2_chunkwise(q_b, k_b, logf_b, i_b)
        h_b = rmsnorm(h_b, hg_norm_w[l].reshape(HG_HEADS, HG_DV)).reshape(Bn, S, HG_WIDTH)
        y_b = h_b * jax.nn.silu(hg_g)

        y = jnp.concatenate([y_a, y_b], axis=-1)
        h = h + jnp.einsum('bsc,cd->bsd', y, w_out[l].astype(jnp.float32))

        e = jnp.einsum('bsr,rd->bsd', p[l].astype(jnp.float32), w_pe[l].astype(jnp.float32))
        gate = jax.nn.sigmoid(jnp.einsum('bsd,de->bse', rmsnorm(h, pe_norm_w[l]), w_pg[l].astype(jnp.float32)))
        h = h + gate * e
    out = rmsnorm(h, final_norm_w)
    return out.astype(x.dtype)
```

```python
import numpy as np
from contextlib import ExitStack
import concourse.bass as bass
import concourse.mybir as mybir
from concourse.bass_utils import run_bass_kernel_spmd

F32 = mybir.dt.float32
BF16 = mybir.dt.bfloat16
AF = mybir.ActivationFunctionType
ALU = mybir.AluOpType
AX = mybir.AxisListType

D = 2048
INC = 8200
NCORES = 8
SEG = 4096
EPS = 1e-6
QSCALE = 128 ** -0.5
CC_INC = 16

C_Q, C_K, C_V, C_O, C_Z, C_I, C_F, C_HQ, C_HF, C_HI, C_HG = (
    0, 512, 1024, 2048, 3072, 4096, 4100, 4104, 5128, 6152, 7176)


class Buf:
    __slots__ = ("name", "w", "r")

    def __init__(self, name):
        self.name = name
        self.w = None
        self.r = {}


class Sched:
    def __init__(self, nc, stack, same_engine_sync=True):
        self.nc = nc
        self.stack = stack
        self.eng = {"pe": nc.tensor, "act": nc.scalar, "dve": nc.vector,
                    "pool": nc.gpsimd, "sp": nc.sync}
        self.sems = {}
        self.cnt = {}
        self.seen = {e: {} for e in self.eng}
        self.same = same_engine_sync
        for e in self.eng:
            self.newsem(e)
        self.ninst = {e: 0 for e in self.eng}
        self.stopped = False
        self.stop_at = None

    def mark(self, label):
        if self.stop_at is not None and label == self.stop_at:
            self.stopped = True

    def newsem(self, key):
        self.sems[key] = self.stack.enter_context(self.nc.semaphore("s_" + key))
        self.cnt[key] = 0
        return key

    def _deps(self, reads, writes):
        deps = {}

        def add(k, v):
            if deps.get(k, 0) < v:
                deps[k] = v
        for b in reads:
            if b.w is not None:
                add(*b.w)
        for b in writes:
            if b.w is not None:
                add(*b.w)
            for k, v in b.r.items():
                add(k, v)
        return deps

    def _wait(self, e, deps):
        seen = self.seen[e]
        for k, v in deps.items():
            if k == e and (not self.same or e == "pe"):
                continue
            if seen.get(k, 0) >= v:
                continue
            self.eng[e].wait_ge(self.sems[k], v)
            seen[k] = v

    def op(self, e, fn, reads=(), writes=()):
        if self.stopped:
            return None
        self._wait(e, self._deps(reads, writes))
        ins = fn()
        self.cnt[e] += 1
        v = self.cnt[e]
        ins.then_inc(self.sems[e], 1)
        self.ninst[e] += 1
        for b in reads:
            if b.r.get(e, 0) < v:
                b.r[e] = v
        for b in writes:
            b.w = (e, v)
            b.r = {}
        return ins

    def ops(self, e, fns, reads=(), writes=()):
        if self.stopped:
            return None
        self._wait(e, self._deps(reads, writes))
        ins = None
        for fn in fns:
            ins = fn()
        self.cnt[e] += 1
        v = self.cnt[e]
        ins.then_inc(self.sems[e], 1)
        self.ninst[e] += len(fns)
        for b in reads:
            if b.r.get(e, 0) < v:
                b.r[e] = v
        for b in writes:
            b.w = (e, v)
            b.r = {}
        return ins

    def dma(self, q, semkey, out, in_, reads=(), writes=(), **kw):
        if self.stopped:
            return None
        self._wait(q, self._deps(reads, writes))
        ins = self.eng[q].dma_start(out=out, in_=in_, **kw)
        self.cnt[semkey] += 16
        v = self.cnt[semkey]
        ins.then_inc(self.sems[semkey], 16)
        self.ninst[q] += 1
        for b in reads:
            if b.r.get(semkey, 0) < v:
                b.r[semkey] = v
        for b in writes:
            b.w = (semkey, v)
            b.r = {}
        return ins

    def barrier(self):
        if self.stopped:
            return
        deps = {k: v for k, v in self.cnt.items() if v > 0}
        for e in self.eng:
            self._wait(e, deps)

    def finish(self, e, bufs):
        deps = {}
        for b in bufs:
            toks = list(b.r.items())
            if b.w is not None:
                toks.append(b.w)
            for k, v in toks:
                if deps.get(k, 0) < v:
                    deps[k] = v
        self._wait(e, deps)


def build_program(ntok=SEG, NT=2, NT_SO=4, nprev=3, debug=None, stop_at=None, use_wsc=True):
    assert ntok % (128 * NT) == 0 and (nprev * ntok) % (128 * NT_SO) == 0
    nc = bass.Bass("TRN2", target_bir_lowering=False)
    dram_in = lambda n, s: nc.dram_tensor(n, s, F32, kind="ExternalInput").ap()
    x_d = dram_in("x", [ntok, D])
    xprev_d = dram_in("xprev", [max(nprev, 1) * ntok, D])
    p_d = dram_in("p", [ntok, 256])
    normw_d = dram_in("norm_w", [D])
    win_d = dram_in("w_in", [D, INC])
    convw_d = dram_in("conv_w", [4, 1024])
    convb_d = dram_in("conv_b", [1024])
    bi_d = dram_in("ml_b_i", [4])
    bf_d = dram_in("ml_b_f", [4])
    mlnw_d = dram_in("ml_norm_w", [1024])
    hglb_d = dram_in("hg_lb", [2, 1024])
    hgnw_d = dram_in("hg_norm_w", [1024])
    wout_d = dram_in("w_out", [D, D])
    penw_d = dram_in("pe_norm_w", [D])
    wpg_d = dram_in("w_pg", [D, D])
    wpe_d = dram_in("w_pe", [256, D])
    fnw_d = dram_in("final_norm_w", [D])
    flags_d = dram_in("flags", [128, 16])
    out_d = nc.dram_tensor("out", [ntok, D], F32, kind="ExternalOutput").ap()
    dbg_d = {}
    if debug:
        for n, s in debug.items():
            dbg_d[n] = nc.dram_tensor("dbg_" + n, list(s), F32, kind="ExternalOutput").ap()

    SW = 8
    with ExitStack() as st:
        S = Sched(nc, st)
        S.stop_at = stop_at

        def sb(name, shape, dt):
            t = st.enter_context(nc.sbuf_tensor(name, list(shape), dt))
            return t, Buf(name)

        def ps(name, shape, dt):
            t = st.enter_context(nc.psum_tensor(name, list(shape), dt))
            return t, Buf(name)

        V, A, P_, T = nc.vector, nc.scalar, nc.gpsimd, nc.tensor

        NW = 3
        Wb = [sb("W%d" % i, [128, 16, 512], BF16) for i in range(NW)]
        ub = [sb("ub%d" % i, [128, D], BF16) for i in range(2)]
        ptr = [ps("ptr%d" % i, [128, 8, 128], BF16) for i in range(2)]
        pmm = [ps("pmm%d" % i, [128, 512], F32) for i in range(2)]
        pst, bpst = ps("pst", [128, 512], F32)
        patt, bpatt = ps("patt", [128, 512], F32)
        po = [ps("po%d" % i, [128, 512], F32) for i in range(2)]
        ident, bident = sb("ident", [128, 128], BF16)
        identf, bidentf = sb("identf", [128, 128], F32)
        maskc, bmaskc = sb("maskc", [128, 128], F32)
        mask1, bmask1 = sb("mask1", [128, 128], F32)
        nwc, bnwc = sb("nwc", [128, 16], F32)
        pnc, bpnc = sb("pnc", [128, 16], F32)
        mixc, bmixc = sb("mixc", [128, 16], F32)
        cw, bcw = sb("cw", [128, 8, 4], F32)
        cbias, bcbias = sb("cbias", [128, 8], F32)
        lbt, blbt = sb("lbt", [128, 2, 8], F32)
        lb, blb = sb("lb", [128, 8], F32)
        oml, boml = sb("oml", [128, 8], F32)
        noml, bnoml = sb("noml", [128, 8], F32)
        gbias, bgbias = sb("gbias", [4, 3], F32)
        fnw, bfnw = sb("fnw", [128, D], F32)
        ones4, bones4 = sb("ones4", [4, 128], F32)
        flg, bflg = sb("flg", [128, 16], F32)
        halo, bhalo = sb("halo", [128, 8, 3], F32)
        mcar, bmcar = sb("mcar", [4, 1], F32)
        gsum, bgsum = sb("gsum", [4, 1], F32)
        Cst, bC = sb("Cst", [128, 4, 258], F32)
        Cb, bCb = sb("Cb", [128, 4, 258], BF16)
        Sst, bS = sb("Sst", [128, 8, 128], F32)
        Sb, bSb = sb("Sb", [128, 8, 128], BF16)
        Gseg, bGseg = sb("Gseg", [128, 8], F32)
        ssq, bssq = sb("ssq", [128, 8], F32)
        rstd, brstd = sb("rstd", [128, 8], F32)
        fin, bfin = sb("fin", [128, 64], F32)
        NTMAX = max(NT, NT_SO)
        for k in (["ldx%d_%d" % (q_, i) for q_ in range(2) for i in range(NTMAX)] + ["ldw%d" % i for i in range(NW)] +
                  ["ldc", "ldp0", "ldp1", "dbg"] + ["wbk%d" % i for i in range(NW)] +
                  ["ldwh%d" % i for i in range(NW)] + ["sto%d_%d" % (q_, i) for q_ in range(2) for i in range(NTMAX)]):
            S.newsem(k)

        def dbg(name, ap, bufs):
            if name in dbg_d:
                S.dma("sp", "dbg", dbg_d[name], ap, reads=bufs)

        def cdma(out, in_, wb, **kw):
            S.dma("sp", "ldc", out, in_, writes=[wb], **kw)
        cdma(nwc[:], normw_d.rearrange("(k p) -> p k", p=128), bnwc, allow_slow_non_contiguous=True)
        cdma(pnc[:], penw_d.rearrange("(k p) -> p k", p=128), bpnc, allow_slow_non_contiguous=True)
        cdma(mixc[:, 0:8], mlnw_d.rearrange("(k p) -> p k", p=128), bmixc, allow_slow_non_contiguous=True)
        cdma(mixc[:, 8:16], hgnw_d.rearrange("(k p) -> p k", p=128), bmixc, allow_slow_non_contiguous=True)
        for kk in range(4):
            cdma(cw[:, :, kk], convw_d[kk].rearrange("(c p) -> p c", p=128), bcw, allow_slow_non_contiguous=True)
        cdma(cbias[:], convb_d.rearrange("(c p) -> p c", p=128), bcbias, allow_slow_non_contiguous=True)
        for r_ in range(2):
            cdma(lbt[:, r_, :], hglb_d[r_].rearrange("(h p) -> p h", p=128), blbt, allow_slow_non_contiguous=True)
        cdma(gbias[:, 0:1], bi_d.rearrange("(h o) -> h o", o=1), bgbias, allow_slow_non_contiguous=True)
        cdma(gbias[:, 1:2], bf_d.rearrange("(h o) -> h o", o=1), bgbias, allow_slow_non_contiguous=True)
        cdma(fnw[:], fnw_d.partition_broadcast(128), bfnw)
        cdma(flg[:], flags_d, bflg)
        for b_ in (bnwc, bpnc, bmixc, bcw, bcbias, blbt, bgbias, bfnw, bflg):
            b_.w = ("ldc", S.cnt["ldc"])

        S.op("pool", lambda: P_.memset(identf[:], 0.0), writes=[bidentf])
        S.op("pool", lambda: P_.affine_select(out=identf[:], in_=identf[:], pattern=[[-1, 128]],
                                              compare_op=ALU.not_equal, fill=1.0, base=0, channel_multiplier=1),
             reads=[bidentf], writes=[bidentf])
        S.op("dve", lambda: V.tensor_copy(out=ident[:], in_=identf[:]), reads=[bidentf], writes=[bident])
        for (mk, bm, val) in ((maskc, bmaskc, QSCALE), (mask1, bmask1, 1.0)):
            S.op("pool", lambda: P_.memset(mk[:], val), writes=[bm])
            S.op("pool", lambda: P_.affine_select(out=mk[:], in_=mk[:], pattern=[[1, 128]],
                                                  compare_op=ALU.is_ge, fill=0.0, base=0, channel_multiplier=-1),
                 reads=[bm], writes=[bm])
        S.op("pool", lambda: P_.memset(ones4[:], 1.0), writes=[bones4])
        S.op("dve", lambda: V.tensor_scalar(out=gbias[:, 2:3], in0=gbias[:, 1:2], scalar1=-1.0, scalar2=None, op0=ALU.mult),
             reads=[bgbias], writes=[bgbias])
        S.op("dve", lambda: V.tensor_sub(out=lb[:], in0=lbt[:, 0, :], in1=lbt[:, 1, :]), reads=[blbt], writes=[blb])
        S.op("act", lambda: A.activation(out=oml[:], in_=lb[:], func=AF.Sigmoid, scale=-1.0), reads=[blb], writes=[boml])
        S.op("act", lambda: A.activation(out=lb[:], in_=lb[:], func=AF.Sigmoid), reads=[blb], writes=[blb])
        S.op("dve", lambda: V.tensor_scalar(out=noml[:], in0=oml[:], scalar1=-1.0, scalar2=None, op0=ALU.mult),
             reads=[boml], writes=[bnoml])

        S.mark("consts")
        wslot = [0]

        wsc = {}

        def load_w(src_ap, kchunks=16, ncols=512, key=None, rowscale=None):
            s = wslot[0] % NW
            wslot[0] += 1
            wt, bw = Wb[s]
            if key is not None and key in wsc:
                sc_ap, bsc = wsc[key]
                S.dma("sp", "ldwh%d" % s, wt[:, 0:kchunks, 0:ncols],
                      sc_ap.rearrange("p (k c) -> p k c", c=ncols), reads=[bsc], writes=[bw])
                return wt, bw
            v = src_ap.rearrange("(k p) c -> p k c", p=128)
            step = 4 if kchunks >= 4 else kchunks
            for k0 in range(0, kchunks, step):
                S.dma("pool", "ldw%d" % s, wt[:, k0:k0 + step, 0:ncols], v[:, k0:k0 + step, :], writes=[bw])
            if rowscale is not None:
                colw_, bcolw_ = rowscale
                for k in range(kchunks):
                    eng_ = "dve" if k % 2 == 0 else "pool"
                    E_ = V if k % 2 == 0 else P_
                    S.op(eng_, lambda: E_.tensor_scalar(out=wt[:, k, 0:ncols], in0=wt[:, k, 0:ncols],
                                                        scalar1=colw_[:, k:k + 1], scalar2=None, op0=ALU.mult),
                         reads=[bw, bcolw_], writes=[bw])
            if key is not None and use_wsc:
                sc_ap = nc.dram_tensor("wsc_" + key, [128, kchunks * ncols], BF16).ap()
                bsc = Buf("wsc_" + key)
                S.dma("sp", "wbk%d" % s, sc_ap.rearrange("p (k c) -> p k c", c=ncols), wt[:, 0:kchunks, 0:ncols],
                      reads=[bw], writes=[bsc])
                wsc[key] = (sc_ap, bsc)
            return wt, bw

        def rstd_from_ssq(col, n, width=1):
            sl = slice(col, col + width)
            S.op("dve", lambda: V.tensor_scalar(out=rstd[:, sl], in0=ssq[:, sl], scalar1=1.0 / n,
                                                scalar2=EPS, op0=ALU.mult, op1=ALU.add), reads=[bssq], writes=[brstd])
            S.op("act", lambda: A.activation(out=rstd[:, sl], in_=rstd[:, sl], func=AF.Ln), reads=[brstd], writes=[brstd])
            S.op("act", lambda: A.activation(out=rstd[:, sl], in_=rstd[:, sl], func=AF.Exp, scale=-0.5),
                 reads=[brstd], writes=[brstd])

        S.op("dve", lambda: V.memset(halo[:], 0.0), writes=[bhalo])

        def emit_pass(full, NT, src_d, ngroups_, flagcol_=None):
            G = 128 * NT
            NCH = NT
            QOFF = 0 if full else 4
            with ExitStack() as st2:
                def sb(name, shape, dt):
                    t = st2.enter_context(nc.sbuf_tensor(name + ("_f" if full else "_s"), list(shape), dt))
                    return t, Buf(name)

                NPAR = 2 if full else 1
                XH_l = [sb("XH%d" % q_, [128, NT, D], F32) for q_ in range(NPAR)]
                bXHt_l = [[Buf("XH%d_%d" % (q_, i)) for i in range(NT)] for q_ in range(NPAR)]
                actT_l = [sb("actT%d" % q_, [128, 16, G], BF16) for q_ in range(NPAR)]
                XH, bXHt = XH_l[0][0], bXHt_l[0]
                actT, bactT = actT_l[0]

                cur_par = 0

                def select(par):
                    nonlocal XH, bXHt, actT, bactT, cur_par
                    par = par % NPAR
                    cur_par = par
                    XH, bXHt = XH_l[par][0], bXHt_l[par]
                    actT, bactT = actT_l[par]
                mreset, bmreset = sb("mreset", [128, G], F32)
                dmask, bdmask = sb("dmask", [4, 4, NCH], F32)
                cpre = [sb("cpre%d" % i, [128, G + 3], F32) for i in range(2)]
                cacc = [sb("cacc%d" % i, [128, G], F32) for i in range(2)]
                qkT, bqkT = sb("qkT", [128, 8 if full else 4, G], BF16)
                bqk = [Buf("qk%d" % i) for i in range(8)]
                kd, bkd_ = sb("kd", [128, 8, G], BF16)
                bkd = [Buf("kd%d" % i) for i in range(8)]
                bqd = [Buf("qd%d" % i) for i in range(8)]
                beb = [Buf("eb%d" % i) for i in range(2)]
                if full:
                    qd, bqd_ = sb("qd", [128, 8, G], BF16)
                    ebf, bebf_ = sb("ebf", [128, 2, G], F32)
                expg, bexpg_ = sb("expg", [128, 8, NCH], F32)
                eref, beref_ = sb("eref", [128, 8, NCH], F32)
                egr, begr_ = sb("egr", [128, 8, NCH], F32)
                hsm, bhsm_ = sb("hsm", [128, 8, 4, NCH], F32)
                bhsm = [Buf("hsm%d" % i) for i in range(8)]
                bexpg = [Buf("expg%d" % i) for i in range(8)]
                hft = [sb("hft%d_%d" % (i, j), [128, G], F32) for i in range(2) for j in range(3)]
                kgt = [sb("kgt%d" % i, [128, G], BF16) for i in range(8)]
                vaug, bvaug_ = sb("vaug", [128, NT, 4, 258], BF16)
                bvaug = [Buf("vaug%d" % i) for i in range(NT)]
                bGY = [[Buf("GY%d_%d" % (i, j)) for j in range(4)] for i in range(NT)]
                if full:
                    GY, bGY_ = sb("GY", [128, NT, D], BF16)
                iv, biv_ = sb("iv", [128, NT, 1024], BF16)
                biv = [Buf("iv%d" % i) for i in range(NT)]
                kTM, bkTM_ = sb("kTM", [128, NT, 512], BF16)
                bkTM = Buf("kTM")
                kgTM, bkgTM_ = sb("kgTM", [128, NT, 1024], BF16)
                bkgTM = [Buf("kgTM%d" % i) for i in range(8)]
                vs = [sb("vs%d" % i, [128, 4, 258], BF16) for i in range(2)]
                (gi_r, bgi_r), (gf_r, bgf_r), (gb_r, bgb_r), (ga_r, bga_r), (gea_r, bgea_r), (gfl_r, bgfl_r) = [
                    (t[0:4, :], b) for (t, b) in hft]
                gsm, bgsm = sb("gsm", [4, 8, NCH], F32)
                scR, bscR = sb("scR", [4, 4, NCH], F32)
                scb, bscb = sb("scb", [128, 4, NCH], F32)
                eaT, beaT = sb("eaT", [128, NT, 4], F32)
                flT, bflT = sb("flT", [128, NT, 4], F32)
                if full:
                    ztmp = [sb("ztmp%d" % i, [128, 512], BF16) for i in range(2)]
                    stm, bstm = sb("stm", [128, 512], BF16)
                    sqt, bsqt = sb("sqt", [128, 1024], F32)
                    pTt, bpT = sb("pT", [128, 2, G], BF16)
                    ptile = [sb("ptile%d" % i, [128, 256], F32) for i in range(2)]
                    pbt = [sb("pbt%d" % i, [128, 256], BF16) for i in range(2)]
                    gt = [sb("gt%d" % i, [128, 512], F32) for i in range(2)]
                S.op("pool", lambda: P_.memset(mreset[:], 1.0), writes=[bmreset])
                S.op("pool", lambda: P_.memset(mreset[:].rearrange("p (c l) -> p c l", l=128)[:, :, 0:1], 0.0),
                     writes=[bmreset])
                S.op("pool", lambda: P_.memset(dmask[:], 1.0), writes=[bdmask])
                S.op("pool", lambda: P_.affine_select(out=dmask[:], in_=dmask[:], pattern=[[1, 4], [0, NCH]],
                                                      compare_op=ALU.is_equal, fill=0.0, base=0, channel_multiplier=-1),
                     reads=[bdmask], writes=[bdmask])
                S.op("pool", lambda: P_.memset(vaug[:], 1.0), writes=bvaug)

                tcount = [0]

                def norm_transpose(src_ap, src_buf, i, colw, bcolw):
                    u, bu = ub[tcount[0] % 2]
                    S.op("act", lambda: A.activation(out=u[:], in_=src_ap, func=AF.Square, accum_out=ssq[:, i:i + 1]),
                         reads=[src_buf], writes=[bu, bssq])
                    rstd_from_ssq(i, D)
                    S.op("dve", lambda: V.tensor_scalar(out=u[:], in0=src_ap, scalar1=rstd[:, i:i + 1], scalar2=None,
                                                        op0=ALU.mult), reads=[src_buf, brstd], writes=[bu])
                    transpose16(u, bu, i, colw, bcolw)

                def transpose16(u, bu, i, colw, bcolw):
                    for hb in range(2):
                        pt, bpt = ptr[tcount[0] % 2]
                        tcount[0] += 1
                        S.ops("pe", [(lambda j=j: T.transpose(out=pt[:, j, :], in_=u[:, (hb * 8 + j) * 128:(hb * 8 + j + 1) * 128],
                                                              identity=ident[:])) for j in range(8)],
                              reads=[bu, bident], writes=[bpt])
                        if hb == 0:
                            S.op("act", lambda: A.copy(out=actT[:, 0:8, i * 128:(i + 1) * 128], in_=pt[:, :, :]),
                                 reads=[bpt], writes=[bactT])
                        else:
                            S.op("dve", lambda: V.tensor_copy(out=actT[:, 8:16, i * 128:(i + 1) * 128], in_=pt[:, :, :]),
                                 reads=[bpt], writes=[bactT])

                mmc = [0]

                def mm_tm(i, wt, bw, kchunks=16, lhs=None, blhs=None, ncols=512):
                    pm, bpm = pmm[mmc[0] % 2]
                    mmc[0] += 1
                    lhs = actT if lhs is None else lhs
                    blhs = bactT if blhs is None else blhs
                    S.ops("pe", [(lambda k=k: T.matmul(pm[:, 0:ncols], lhsT=lhs[:, k, i * 128:(i + 1) * 128],
                                                       rhs=wt[:, k, 0:ncols], start=(k == 0), stop=(k == kchunks - 1)))
                                 for k in range(kchunks)], reads=[blhs, bw], writes=[bpm])
                    return pm, bpm

                def mm_fm(wt, bw, c0, m, ntoks=G, tok0=0):
                    pm, bpm = pmm[mmc[0] % 2]
                    mmc[0] += 1
                    S.ops("pe", [(lambda k=k: T.matmul(pm[0:m, 0:ntoks], lhsT=wt[:, k, c0:c0 + m],
                                                       rhs=actT[:, k, tok0:tok0 + ntoks], start=(k == 0), stop=(k == 15)))
                                 for k in range(16)], reads=[bactT, bw], writes=[bpm])
                    return pm, bpm

                cvc = [0]

                def conv_block(pm, bpm, c8, ntoks=G, only_halo=False):
                    cp, bcp = cpre[cvc[0] % 2]
                    ca, bca = cacc[cvc[0] % 2]
                    cvc[0] += 1
                    S.op("dve", lambda: V.tensor_copy(out=cp[:, 0:3], in_=halo[:, c8, :]), reads=[bhalo], writes=[bcp])
                    S.op("act", lambda: A.copy(out=cp[:, 3:3 + ntoks], in_=pm[:, 0:ntoks]), reads=[bpm], writes=[bcp])
                    S.op("dve", lambda: V.tensor_copy(out=halo[:, c8, :], in_=cp[:, ntoks:ntoks + 3]), reads=[bcp], writes=[bhalo])
                    if only_halo:
                        return
                    S.op("dve", lambda: V.tensor_scalar(out=ca[:, 0:ntoks], in0=cp[:, 0:ntoks], scalar1=cw[:, c8, 0:1],
                                                        scalar2=cbias[:, c8:c8 + 1], op0=ALU.mult, op1=ALU.add),
                         reads=[bcp, bcw, bcbias], writes=[bca])
                    for kk in range(1, 4):
                        S.op("dve", lambda: V.scalar_tensor_tensor(out=ca[:, 0:ntoks], in0=cp[:, kk:kk + ntoks],
                                                                   scalar=cw[:, c8, kk:kk + 1], in1=ca[:, 0:ntoks],
                                                                   op0=ALU.mult, op1=ALU.add),
                             reads=[bcp, bcw, bca], writes=[bca])
                    S.op("act", lambda: A.activation(out=qkT[:, c8 - QOFF, 0:ntoks], in_=ca[:, 0:ntoks], func=AF.Silu),
                         reads=[bca], writes=[bqk[c8]])

                def k_to_tm():
                    for h in range(4):
                        pt, bpt = ptr[tcount[0] % 2]
                        tcount[0] += 1
                        S.ops("pe", [(lambda i=i: T.transpose(out=pt[:, i, :], in_=qkT[:, 4 + h - QOFF, i * 128:(i + 1) * 128],
                                                              identity=ident[:])) for i in range(NT)],
                              reads=[bqk[4 + h], bident], writes=[bpt])
                        S.op("act", lambda: A.copy(out=kTM[:, :, h * 128:(h + 1) * 128], in_=pt[:, 0:NT, :]),
                             reads=[bpt], writes=[bkTM])

                hfc = [0]
                E38 = float(np.exp(38.0))

                def hf_block(pm, bpm, h, full):
                    s = hfc[0] % 2
                    hfc[0] += 1
                    (t0, b0), (t1, b1), (t2, b2) = hft[s * 3], hft[s * 3 + 1], hft[s * 3 + 2]
                    kg, bkg = kgt[h]
                    hl = h % 2
                    S.op("act", lambda: A.activation(out=t0[:], in_=pm[:, 0:G], func=AF.Exp, scale=-1.0), reads=[bpm], writes=[b0])
                    S.op("dve", lambda: V.tensor_scalar(out=t0[:], in0=t0[:], scalar1=1.0, scalar2=None, op0=ALU.add),
                         reads=[b0], writes=[b0])
                    S.op("dve", lambda: V.reciprocal(out=t0[:], in_=t0[:]), reads=[b0], writes=[b0])
                    S.op("act", lambda: A.activation(out=t1[:], in_=t0[:], func=AF.Ln, scale=oml[:, h:h + 1], bias=lb[:, h:h + 1]),
                         reads=[b0, boml, blb], writes=[b1])
                    S.op("act", lambda: A.activation(out=t0[:], in_=t0[:], func=AF.Identity, scale=noml[:, h:h + 1],
                                                     bias=oml[:, h:h + 1]), reads=[b0, bnoml, boml], writes=[b0])
                    S.op("dve", lambda: V.tensor_tensor_scan(out=t2[:], data0=mreset[:], data1=t1[:], initial=0.0,
                                                             op0=ALU.mult, op1=ALU.add), reads=[bmreset, b1], writes=[b2])
                    b3 = t2[:].rearrange("p (c l) -> p c l", l=128)
                    S.op("dve", lambda: V.tensor_copy(out=hsm[:, h, 0, :], in_=b3[:, :, 63]), reads=[b2], writes=[bhsm[h]])
                    S.op("dve", lambda: V.tensor_scalar(out=hsm[:, h, 1, :], in0=b3[:, :, 63], scalar1=-1.0, scalar2=None,
                                                        op0=ALU.mult), reads=[b2], writes=[bhsm[h]])
                    S.op("dve", lambda: V.tensor_sub(out=hsm[:, h, 2, :], in0=b3[:, :, 127], in1=b3[:, :, 63]),
                         reads=[b2], writes=[bhsm[h]])
                    S.op("act", lambda: A.activation(out=expg[:, h, :], in_=b3[:, :, 127], func=AF.Exp),
                         reads=[b2], writes=[bexpg[h]])
                    S.op("act", lambda: A.activation(out=eref[:, h, :], in_=hsm[:, h, 0, :], func=AF.Exp),
                         reads=[bhsm[h]], writes=[bexpg[h]])
                    S.op("act", lambda: A.activation(out=egr[:, h, :], in_=hsm[:, h, 2, :], func=AF.Exp),
                         reads=[bhsm[h]], writes=[bexpg[h]])
                    S.op("dve", lambda: V.tensor_reduce(out=hsm[:, h, 3, 0:1], in_=b3[:, :, 127], axis=AX.X, op=ALU.add),
                         reads=[b2], writes=[bhsm[h]])
                    S.op("dve", lambda: V.tensor_add(out=Gseg[:, h:h + 1], in0=Gseg[:, h:h + 1], in1=hsm[:, h, 3, 0:1]),
                         reads=[bhsm[h], bGseg], writes=[bGseg])
                    for c in range(NCH):
                        cs = slice(c * 128, (c + 1) * 128)
                        S.op("act", lambda: A.activation(out=t1[:, cs], in_=t2[:, cs], func=AF.Exp, scale=-1.0,
                                                         bias=hsm[:, h, 0, c:c + 1]), reads=[b2, bhsm[h]], writes=[b1])
                        if full:
                            S.op("act", lambda: A.activation(out=ebf[:, hl, cs], in_=t2[:, cs], func=AF.Exp,
                                                             bias=hsm[:, h, 1, c:c + 1]), reads=[b2, bhsm[h]], writes=[beb[hl]])
                    S.op("dve", lambda: V.scalar_tensor_tensor(out=kd[:, h, :], in0=t1[:], scalar=E38, in1=t0[:],
                                                               op0=ALU.min, op1=ALU.mult), reads=[b0, b1], writes=[bkd[h]])
                    S.op("dve", lambda: V.tensor_tensor(out=kg[:].rearrange("p (c l) -> p c l", l=128),
                                                        in0=kd[:, h, :].rearrange("p (c l) -> p c l", l=128),
                                                        in1=egr[:, h, :].unsqueeze(2).to_broadcast([128, NCH, 128]),
                                                        op=ALU.mult), reads=[bkd[h], bexpg[h]], writes=[bkg])

                def kg_to_tm(h):
                    kg, bkg = kgt[h]
                    pt, bpt = ptr[tcount[0] % 2]
                    tcount[0] += 1
                    S.ops("pe", [(lambda i=i: T.transpose(out=pt[:, i, :], in_=kg[:, i * 128:(i + 1) * 128], identity=ident[:]))
                                 for i in range(NT)], reads=[bkg, bident], writes=[bpt])
                    S.op("act", lambda: A.copy(out=kgTM[:, :, h * 128:(h + 1) * 128], in_=pt[:, 0:NT, :]),
                         reads=[bpt], writes=[bkgTM[h]])

                def gates_rows(pm_i, bpm_i, pm_f, bpm_f):
                    S.op("act", lambda: A.activation(out=gi_r, in_=pm_i[0:4, 0:G], func=AF.Identity, bias=gbias[:, 0:1]),
                         reads=[bpm_i, bgbias], writes=[bgi_r])
                    S.op("act", lambda: A.activation(out=gf_r, in_=pm_f[0:4, 0:G], func=AF.Exp, scale=-1.0, bias=gbias[:, 2:3]),
                         reads=[bpm_f, bgbias], writes=[bgf_r])
                    S.op("act", lambda: A.activation(out=gf_r, in_=gf_r, func=AF.Ln, bias=1.0), reads=[bgf_r], writes=[bgf_r])
                    S.op("dve", lambda: V.tensor_scalar(out=gf_r, in0=gf_r, scalar1=-1.0, scalar2=None, op0=ALU.mult),
                         reads=[bgf_r], writes=[bgf_r])
                    S.op("dve", lambda: V.tensor_tensor_scan(out=gb_r, data0=mreset[0:4, :], data1=gf_r, initial=0.0,
                                                             op0=ALU.mult, op1=ALU.add), reads=[bmreset, bgf_r], writes=[bgb_r])
                    S.op("dve", lambda: V.tensor_sub(out=ga_r, in0=gi_r, in1=gb_r), reads=[bgi_r, bgb_r], writes=[bga_r])
                    a3 = ga_r.rearrange("p (c l) -> p c l", l=128)
                    b3 = gb_r.rearrange("p (c l) -> p c l", l=128)
                    AMAX, GSH, MM, GM, MPREV, SC = range(6)
                    S.op("dve", lambda: V.tensor_reduce(out=gsm[:, AMAX, :], in_=a3, axis=AX.X, op=ALU.max),
                         reads=[bga_r], writes=[bgsm])
                    S.op("dve", lambda: V.memset(gsm[:, GSH, 0:1], 0.0), writes=[bgsm])
                    if NCH > 1:
                        S.op("dve", lambda: V.tensor_copy(out=gsm[:, GSH, 1:NCH], in_=b3[:, 0:NCH - 1, 127]),
                             reads=[bgb_r], writes=[bgsm])
                    S.op("dve", lambda: V.tensor_tensor_scan(out=gsm[:, MM, :], data0=gsm[:, GSH, :], data1=gsm[:, AMAX, :],
                                                             initial=mcar[:, 0:1], op0=ALU.add, op1=ALU.max),
                         reads=[bgsm, bmcar], writes=[bgsm])
                    S.op("dve", lambda: V.tensor_add(out=gsm[:, GM, :], in0=gsm[:, MM, :], in1=b3[:, :, 127]),
                         reads=[bgsm, bgb_r], writes=[bgsm])
                    S.op("dve", lambda: V.tensor_copy(out=gsm[:, MPREV, 0:1], in_=mcar[:, 0:1]), reads=[bmcar], writes=[bgsm])
                    if NCH > 1:
                        S.op("dve", lambda: V.tensor_copy(out=gsm[:, MPREV, 1:NCH], in_=gsm[:, GM, 0:NCH - 1]),
                             reads=[bgsm], writes=[bgsm])
                    S.op("dve", lambda: V.tensor_copy(out=mcar[:, 0:1], in_=gsm[:, GM, NCH - 1:NCH]), reads=[bgsm], writes=[bmcar])
                    S.op("dve", lambda: V.tensor_reduce(out=gsm[:, 6, 0:1], in_=b3[:, :, 127], axis=AX.X, op=ALU.add),
                         reads=[bgb_r], writes=[bgsm])
                    S.op("dve", lambda: V.tensor_add(out=gsum[:], in0=gsum[:], in1=gsm[:, 6, 0:1]), reads=[bgsm, bgsum],
                         writes=[bgsum])
                    S.op("dve", lambda: V.tensor_sub(out=gsm[:, SC, :], in0=gsm[:, MPREV, :], in1=gsm[:, MM, :]),
                         reads=[bgsm], writes=[bgsm])
                    S.op("act", lambda: A.activation(out=gsm[:, SC, :], in_=gsm[:, SC, :], func=AF.Exp), reads=[bgsm], writes=[bgsm])
                    Mb = gsm[:, MM, :].unsqueeze(2).to_broadcast([4, NCH, 128])
                    S.op("dve", lambda: V.tensor_tensor(out=gea_r.rearrange("p (c l) -> p c l", l=128), in0=a3, in1=Mb,
                                                        op=ALU.subtract), reads=[bga_r, bgsm], writes=[bgea_r])
                    S.op("act", lambda: A.activation(out=gea_r, in_=gea_r, func=AF.Exp), reads=[bgea_r], writes=[bgea_r])
                    S.op("dve", lambda: V.tensor_tensor(out=gfl_r.rearrange("p (c l) -> p c l", l=128), in0=b3, in1=Mb,
                                                        op=ALU.add), reads=[bgb_r, bgsm], writes=[bgfl_r])
                    S.op("act", lambda: A.activation(out=gfl_r, in_=gfl_r, func=AF.Exp, scale=-1.0),
                         reads=[bgfl_r], writes=[bgfl_r])
                    S.op("dve", lambda: V.tensor_tensor(out=scR[:], in0=dmask[:],
                                                        in1=gsm[:, SC, :].unsqueeze(1).to_broadcast([4, 4, NCH]), op=ALU.mult),
                         reads=[bdmask, bgsm], writes=[bscR])
                    pm, bpm = pmm[mmc[0] % 2]
                    mmc[0] += 1
                    S.op("pe", lambda: T.matmul(pm[:, 0:4 * NCH], lhsT=ones4[:], rhs=scR[:].rearrange("p h c -> p (h c)"),
                                                start=True, stop=True), reads=[bones4, bscR], writes=[bpm])
                    S.op("dve", lambda: V.tensor_copy(out=scb[:].rearrange("p h c -> p (h c)"), in_=pm[:, 0:4 * NCH]),
                         reads=[bpm], writes=[bscb])
                    pm, bpm = pmm[mmc[0] % 2]
                    mmc[0] += 1
                    for i in range(NT):
                        S.op("pe", lambda: T.matmul(pm[:, 8 * i:8 * i + 4], lhsT=gea_r[:, i * 128:(i + 1) * 128],
                                                    rhs=identf[0:4, 0:4], start=True, stop=True),
                             reads=[bgea_r, bidentf], writes=[bpm])
                        S.op("pe", lambda: T.matmul(pm[:, 8 * i + 4:8 * i + 8], lhsT=gfl_r[:, i * 128:(i + 1) * 128],
                                                    rhs=identf[0:4, 0:4], start=True, stop=True),
                             reads=[bgfl_r, bidentf], writes=[bpm])
                    pv = pm[:, 0:8 * NT].rearrange("p (i e) -> p i e", e=8)
                    if cur_flag[0] is None:
                        S.op("dve", lambda: V.tensor_copy(out=eaT[:], in_=pv[:, :, 0:4]), reads=[bpm], writes=[beaT])
                    else:
                        fc = cur_flag[0]
                        S.op("dve", lambda: V.tensor_scalar(out=eaT[:], in0=pv[:, :, 0:4], scalar1=flg[:, fc:fc + 1], scalar2=None,
                                                            op0=ALU.mult), reads=[bpm, bflg], writes=[beaT])
                    S.op("dve", lambda: V.tensor_copy(out=flT[:], in_=pv[:, :, 4:8]), reads=[bpm], writes=[bflT])

                vsc = [0]
                cur_flag = [None]

                def mixer_chunk(c, full):
                    i = c
                    tc = slice(c * 128, (c + 1) * 128)
                    v_, bv_ = vs[vsc[0] % 2]
                    vsc[0] += 1
                    S.op("dve", lambda: V.tensor_tensor(out=v_[:], in0=vaug[:, i, :, :],
                                                        in1=eaT[:, i, :].unsqueeze(2).to_broadcast([128, 4, 258]),
                                                        op=ALU.mult), reads=[bvaug[i], beaT], writes=[bv_])
                    S.op("dve", lambda: V.tensor_tensor(out=Cst[:], in0=Cst[:],
                                                        in1=scb[:, :, c:c + 1].to_broadcast([128, 4, 258]), op=ALU.mult),
                         reads=[bC, bscb], writes=[bC])
                    if full:
                        S.op("act", lambda: A.activation(out=Cb[:], in_=Cst[:], func=AF.Copy, scale=QSCALE),
                             reads=[bC], writes=[bCb])
                        for h in range(4):
                            S.op("pe", lambda: T.matmul(patt[:, h * 128:(h + 1) * 128], lhsT=qkT[:, 4 + h, tc],
                                                        rhs=qkT[:, h, tc], start=True, stop=True),
                                 reads=[bqk[4 + h], bqk[h]], writes=[bpatt])
                        S.op("dve", lambda: V.tensor_tensor(out=stm[:].rearrange("p (h t) -> p h t", t=128),
                                                            in0=patt[:, :].rearrange("p (h t) -> p h t", t=128),
                                                            in1=maskc[:].unsqueeze(1).to_broadcast([128, 4, 128]), op=ALU.mult),
                             reads=[bpatt, bmaskc], writes=[bstm])
                        pmd, bpmd = pmm[mmc[0] % 2]
                        mmc[0] += 1
                        for h in range(4):
                            pm, bpm = po[h // 2]
                            osl = slice((h % 2) * 256, (h % 2) * 256 + 256)
                            S.op("pe", lambda: T.matmul(pm[:, osl], lhsT=stm[:, h * 128:(h + 1) * 128],
                                                        rhs=v_[:, h, 0:256], start=True, stop=False),
                                 reads=[bstm, bv_], writes=[bpm])
                            S.op("pe", lambda: T.matmul(pm[:, osl], lhsT=qkT[:, h, tc], rhs=Cb[:, h, 0:256],
                                                        start=False, stop=True), reads=[bqk[h], bCb], writes=[bpm])
                            S.op("pe", lambda: T.matmul(pmd[:, h:h + 1], lhsT=stm[:, h * 128:(h + 1) * 128],
                                                        rhs=v_[:, h, 256:257], start=True, stop=False),
                                 reads=[bstm, bv_], writes=[bpmd])
                            S.op("pe", lambda: T.matmul(pmd[:, h:h + 1], lhsT=qkT[:, h, tc], rhs=Cb[:, h, 256:257],
                                                        start=False, stop=True), reads=[bqk[h], bCb], writes=[bpmd])
                        f_ = fin
                        S.op("act", lambda: A.activation(out=f_[:, 0:4], in_=pmd[:, 0:4], func=AF.Abs),
                             reads=[bpmd], writes=[bfin])
                        S.op("dve", lambda: V.tensor_tensor(out=f_[:, 0:4], in0=f_[:, 0:4], in1=flT[:, i, :], op=ALU.max),
                             reads=[bfin, bflT], writes=[bfin])
                        S.op("dve", lambda: V.reciprocal(out=f_[:, 0:4], in_=f_[:, 0:4]), reads=[bfin], writes=[bfin])
                        for half in range(2):
                            pm, bpm = po[half]
                            S.op("act", lambda: A.activation(out=sqt[:, half * 512:(half + 1) * 512], in_=pm[:, :],
                                                             func=AF.Square), reads=[bpm], writes=[bsqt])
                        S.op("dve", lambda: V.tensor_reduce(out=f_[:, 4:8], in_=sqt[:, :].rearrange("p (h v) -> p h v", v=256),
                                                            axis=AX.X, op=ALU.add), reads=[bsqt], writes=[bfin])
                        S.op("dve", lambda: V.tensor_tensor(out=f_[:, 8:12], in0=f_[:, 0:4], in1=f_[:, 0:4], op=ALU.mult),
                             reads=[bfin], writes=[bfin])
                        S.op("dve", lambda: V.tensor_tensor(out=f_[:, 8:12], in0=f_[:, 8:12], in1=f_[:, 4:8], op=ALU.mult),
                             reads=[bfin], writes=[bfin])
                        S.op("dve", lambda: V.tensor_scalar(out=f_[:, 8:12], in0=f_[:, 8:12], scalar1=1.0 / 256, scalar2=EPS,
                                                            op0=ALU.mult, op1=ALU.add), reads=[bfin], writes=[bfin])
                        S.op("act", lambda: A.activation(out=f_[:, 8:12], in_=f_[:, 8:12], func=AF.Ln),
                             reads=[bfin], writes=[bfin])
                        S.op("act", lambda: A.activation(out=f_[:, 8:12], in_=f_[:, 8:12], func=AF.Exp, scale=-0.5),
                             reads=[bfin], writes=[bfin])
                        S.op("dve", lambda: V.tensor_tensor(out=f_[:, 8:12], in0=f_[:, 8:12], in1=f_[:, 0:4], op=ALU.mult),
                             reads=[bfin], writes=[bfin])
                        for h in range(4):
                            pm, bpm = po[h // 2]
                            osl = slice((h % 2) * 256, (h % 2) * 256 + 256)
                            gsl = slice(h * 256, (h + 1) * 256)
                            S.op("dve", lambda: V.scalar_tensor_tensor(out=GY[:, i, gsl], in0=pm[:, osl],
                                                                       scalar=f_[:, 8 + h:9 + h], in1=GY[:, i, gsl],
                                                                       op0=ALU.mult, op1=ALU.mult),
                                 reads=[bpm, bfin, bGY[i][h // 2]], writes=[bGY[i][h // 2]])
                    for half in range(2):
                        for hq in range(2):
                            h = half * 2 + hq
                            S.op("pe", lambda: T.matmul(pst[:, hq * 256:(hq + 1) * 256], lhsT=kTM[:, i, h * 128:(h + 1) * 128],
                                                        rhs=v_[:, h, 0:256], start=True, stop=True),
                                 reads=[bkTM, bv_], writes=[bpst])
                        S.op("dve", lambda: V.tensor_tensor(out=Cst[:, half * 2:half * 2 + 2, 0:256],
                                                            in0=Cst[:, half * 2:half * 2 + 2, 0:256],
                                                            in1=pst[:, :].rearrange("p (h v) -> p h v", v=256), op=ALU.add),
                             reads=[bC, bpst], writes=[bC])
                    pmn, bpmn = pmm[mmc[0] % 2]
                    mmc[0] += 1
                    for h in range(4):
                        S.op("pe", lambda: T.matmul(pmn[:, h:h + 1], lhsT=kTM[:, i, h * 128:(h + 1) * 128],
                                                    rhs=v_[:, h, 256:257], start=True, stop=True),
                             reads=[bkTM, bv_], writes=[bpmn])
                    S.op("dve", lambda: V.tensor_tensor(out=Cst[:, :, 256:257], in0=Cst[:, :, 256:257],
                                                        in1=pmn[:, 0:4].unsqueeze(2), op=ALU.add),
                         reads=[bC, bpmn], writes=[bC])
                    if full:
                        for h in range(8):
                            S.op("act", lambda: A.activation(out=Sb[:, h, :], in_=Sst[:, h, :], func=AF.Copy,
                                                             scale=eref[:, h, c:c + 1]), reads=[bS, bexpg[h]], writes=[bSb])
                        for half in range(2):
                            for hq in range(4):
                                h = half * 4 + hq
                                S.op("pe", lambda: T.matmul(patt[:, hq * 128:(hq + 1) * 128], lhsT=kd[:, h, tc], rhs=qd[:, h, tc],
                                                            start=True, stop=True), reads=[bkd[h], bqd[h]], writes=[bpatt])
                            S.op("dve", lambda: V.tensor_tensor(out=stm[:].rearrange("p (h t) -> p h t", t=128),
                                                                in0=patt[:, :].rearrange("p (h t) -> p h t", t=128),
                                                                in1=mask1[:].unsqueeze(1).to_broadcast([128, 4, 128]),
                                                                op=ALU.mult), reads=[bpatt, bmask1], writes=[bstm])
                            pm, bpm = po[half]
                            for hq in range(4):
                                h = half * 4 + hq
                                osl = slice(hq * 128, hq * 128 + 128)
                                S.op("pe", lambda: T.matmul(pm[:, osl], lhsT=stm[:, hq * 128:(hq + 1) * 128],
                                                            rhs=iv[:, i, h * 128:(h + 1) * 128], start=True, stop=False),
                                     reads=[bstm, biv[i]], writes=[bpm])
                                S.op("pe", lambda: T.matmul(pm[:, osl], lhsT=qd[:, h, tc], rhs=Sb[:, h, :],
                                                            start=False, stop=True), reads=[bqd[h], bSb], writes=[bpm])
                        f_ = fin
                        for half in range(2):
                            pm, bpm = po[half]
                            S.op("act", lambda: A.activation(out=sqt[:, half * 512:(half + 1) * 512], in_=pm[:, :],
                                                             func=AF.Square), reads=[bpm], writes=[bsqt])
                        S.op("dve", lambda: V.tensor_reduce(out=f_[:, 16:24], in_=sqt[:, :].rearrange("p (h v) -> p h v", v=128),
                                                            axis=AX.X, op=ALU.add), reads=[bsqt], writes=[bfin])
                        S.op("dve", lambda: V.tensor_scalar(out=f_[:, 16:24], in0=f_[:, 16:24], scalar1=1.0 / 128, scalar2=EPS,
                                                            op0=ALU.mult, op1=ALU.add), reads=[bfin], writes=[bfin])
                        S.op("act", lambda: A.activation(out=f_[:, 16:24], in_=f_[:, 16:24], func=AF.Ln),
                             reads=[bfin], writes=[bfin])
                        S.op("act", lambda: A.activation(out=f_[:, 16:24], in_=f_[:, 16:24], func=AF.Exp, scale=-0.5),
                             reads=[bfin], writes=[bfin])
                        for h in range(8):
                            pm, bpm = po[h // 4]
                            osl = slice((h % 4) * 128, (h % 4) * 128 + 128)
                            gsl = slice(1024 + h * 128, 1024 + (h + 1) * 128)
                            S.op("dve", lambda: V.scalar_tensor_tensor(out=GY[:, i, gsl], in0=pm[:, osl],
                                                                       scalar=f_[:, 16 + h:17 + h], in1=GY[:, i, gsl],
                                                                       op0=ALU.mult, op1=ALU.mult),
                                 reads=[bpm, bfin, bGY[i][2 + h // 4]], writes=[bGY[i][2 + h // 4]])
                    for half in range(2):
                        for hq in range(4):
                            h = half * 4 + hq
                            S.op("pe", lambda: T.matmul(pst[:, hq * 128:(hq + 1) * 128], lhsT=kgTM[:, i, h * 128:(h + 1) * 128],
                                                        rhs=iv[:, i, h * 128:(h + 1) * 128], start=True, stop=True),
                                 reads=[bkgTM[h], biv[i]], writes=[bpst])
                        for hq in range(4):
                            h = half * 4 + hq
                            S.op("dve", lambda: V.scalar_tensor_tensor(out=Sst[:, h, :], in0=Sst[:, h, :],
                                                                       scalar=expg[:, h, c:c + 1],
                                                                       in1=pst[:, hq * 128:(hq + 1) * 128],
                                                                       op0=ALU.mult, op1=ALU.add),
                                 reads=[bS, bexpg[h], bpst], writes=[bS])

                def run_pass(full, src_d, ngroups, flagcol=None):
                    def phase_a(g_):
                        ta = g_ * G
                        for i in range(NT):
                            S.dma("act", "ldx%d_%d" % (cur_par, i), XH[:, i, :], src_d[ta + i * 128:ta + (i + 1) * 128, :], writes=[bXHt[i]])
                        for i in range(NT):
                            norm_transpose(XH[:, i, :], bXHt[i], i, nwc, bnwc)

                    for g in range(ngroups):
                        t0 = g * G
                        cur_flag[0] = None if flagcol is None else flagcol(t0)
                        select(g)
                        if g == 0:
                            phase_a(g)
                        S.mark("phaseA")
                        wt, bw = load_w(win_d[:, C_I:C_I + 8], ncols=8, key="gates", rowscale=(nwc, bnwc))
                        pm_i, bpm_i = mm_fm(wt, bw, 0, 4)
                        pm_f, bpm_f = mm_fm(wt, bw, 4, 4)
                        gates_rows(pm_i, bpm_i, pm_f, bpm_f)
                        S.mark("gates")
                        for blk in ((0, 1) if full else (1,)):
                            wt, bw = load_w(win_d[:, blk * 512:(blk + 1) * 512], key="qk%d" % blk, rowscale=(nwc, bnwc))
                            for j in range(4):
                                pm, bpm = mm_fm(wt, bw, j * 128, 128)
                                conv_block(pm, bpm, blk * 4 + j)
                        S.mark("qk")
                        if (not full) and g == ngroups - 1:
                            wt, bw = load_w(win_d[:, 0:512], key="qk0", rowscale=(nwc, bnwc))
                            for j in range(4):
                                pm, bpm = mm_fm(wt, bw, j * 128, 128, ntoks=128, tok0=G - 128)
                                conv_block(pm, bpm, j, ntoks=128, only_halo=True)
                        S.mark("ktm")
                        for cb in range(2):
                            wt, bw = load_w(win_d[:, C_V + cb * 512:C_V + (cb + 1) * 512], key="v%d" % cb, rowscale=(nwc, bnwc))
                            for i in range(NT):
                                pm, bpm = mm_tm(i, wt, bw)
                                S.op("act", lambda: A.copy(out=vaug[:, i, 2 * cb:2 * cb + 2, 0:256],
                                                           in_=pm[:, :].rearrange("p (h v) -> p h v", v=256)),
                                     reads=[bpm], writes=[bvaug[i]])
                        S.mark("v")
                        if full:
                            for cb in range(2):
                                wt, bw = load_w(win_d[:, C_O + cb * 512:C_O + (cb + 1) * 512], key="o%d" % cb, rowscale=(nwc, bnwc))
                                for i in range(NT):
                                    pm, bpm = mm_tm(i, wt, bw)
                                    S.op("act", lambda: A.activation(out=GY[:, i, cb * 512:(cb + 1) * 512], in_=pm[:, :],
                                                                     func=AF.Sigmoid), reads=[bpm], writes=[bGY[i][cb]])
                            for cb in range(2):
                                wt, bw = load_w(win_d[:, C_Z + cb * 512:C_Z + (cb + 1) * 512], key="z%d" % cb, rowscale=(nwc, bnwc))
                                for i in range(NT):
                                    pm, bpm = mm_tm(i, wt, bw)
                                    zt, bzt = ztmp[(cb * NT + i) % 2]
                                    S.op("act", lambda: A.activation(out=zt[:], in_=pm[:, :], func=AF.Silu),
                                         reads=[bpm], writes=[bzt])
                                    S.op("dve", lambda: V.tensor_tensor(out=GY[:, i, cb * 512:(cb + 1) * 512],
                                                                        in0=GY[:, i, cb * 512:(cb + 1) * 512], in1=zt[:],
                                                                        op=ALU.mult), reads=[bzt, bGY[i][cb]], writes=[bGY[i][cb]])
                        S.mark("oz")
                        for hb in range(2):
                            wt, bw = load_w(win_d[:, C_HF + hb * 512:C_HF + (hb + 1) * 512], key="hf%d" % hb, rowscale=(nwc, bnwc))
                            if full:
                                wtq, bwq = load_w(win_d[:, C_HQ + hb * 512:C_HQ + (hb + 1) * 512], key="hq%d" % hb, rowscale=(nwc, bnwc))
                            for j in range(4):
                                h = hb * 4 + j
                                pm, bpm = mm_fm(wt, bw, j * 128, 128)
                                hf_block(pm, bpm, h, full)
                                if full:
                                    pm, bpm = mm_fm(wtq, bwq, j * 128, 128)
                                    S.op("dve", lambda: V.tensor_tensor(out=qd[:, h, :], in0=pm[:, 0:G], in1=ebf[:, h % 2, :],
                                                                        op=ALU.mult), reads=[bpm, beb[h % 2]], writes=[bqd[h]])
                        S.mark("hgrn")
                        for cb in range(2):
                            wt, bw = load_w(win_d[:, C_HI + cb * 512:C_HI + (cb + 1) * 512], key="hi%d" % cb, rowscale=(nwc, bnwc))
                            for i in range(NT):
                                pm, bpm = mm_tm(i, wt, bw)
                                S.op("act", lambda: A.copy(out=iv[:, i, cb * 512:(cb + 1) * 512], in_=pm[:, :]),
                                     reads=[bpm], writes=[biv[i]])
                        if full:
                            for cb in range(2):
                                wt, bw = load_w(win_d[:, C_HG + cb * 512:C_HG + (cb + 1) * 512], key="hg%d" % cb, rowscale=(nwc, bnwc))
                                for i in range(NT):
                                    pm, bpm = mm_tm(i, wt, bw)
                                    S.op("act", lambda: A.activation(out=GY[:, i, 1024 + cb * 512:1024 + (cb + 1) * 512],
                                                                     in_=pm[:, :], func=AF.Silu),
                                         reads=[bpm], writes=[bGY[i][2 + cb]])
                        S.mark("hi_hg")
                        k_to_tm()
                        for h_ in range(8):
                            kg_to_tm(h_)
                        if g + 1 < ngroups:
                            select(g + 1)
                            phase_a(g + 1)
                            select(g)
                        for c in range(NCH):
                            mixer_chunk(c, full)
                            S.mark("mix%d" % c)
                        S.mark("mixer")
                        if not full:
                            continue
                        for i in range(NT):
                            for hb in range(2):
                                pt, bpt = ptr[tcount[0] % 2]
                                tcount[0] += 1
                                S.ops("pe", [(lambda j=j: T.transpose(out=pt[:, j, :],
                                                                      in_=GY[:, i, (hb * 8 + j) * 128:(hb * 8 + j + 1) * 128],
                                                                      identity=ident[:])) for j in range(8)],
                                      reads=[bGY[i][2 * hb], bGY[i][2 * hb + 1], bident], writes=[bpt])
                                if hb == 0:
                                    S.op("act", lambda: A.copy(out=actT[:, 0:8, i * 128:(i + 1) * 128], in_=pt[:, :, :]),
                                         reads=[bpt], writes=[bactT])
                                else:
                                    S.op("dve", lambda: V.tensor_copy(out=actT[:, 8:16, i * 128:(i + 1) * 128], in_=pt[:, :, :]),
                                         reads=[bpt], writes=[bactT])
                        S.mark("yT")
                        for cb in range(4):
                            wt, bw = load_w(wout_d[:, cb * 512:(cb + 1) * 512], key="wo%d" % cb, rowscale=(mixc, bmixc))
                            for i in range(NT):
                                pm, bpm = mm_tm(i, wt, bw)
                                S.op("dve", lambda: V.tensor_tensor(out=XH[:, i, cb * 512:(cb + 1) * 512],
                                                                    in0=XH[:, i, cb * 512:(cb + 1) * 512], in1=pm[:, :],
                                                                    op=ALU.add), reads=[bpm, bXHt[i]], writes=[bXHt[i]])
                        S.mark("wout")
                        for i in range(NT):
                            norm_transpose(XH[:, i, :], bXHt[i], i, pnc, bpnc)
                        S.mark("hT")
                        for i in range(NT):
                            pl, bpl = ptile[i % 2]
                            pb_, bpb_ = pbt[i % 2]
                            S.dma("act", "ldp%d" % (i % 2), pl[:], p_d[t0 + i * 128:t0 + (i + 1) * 128, :], writes=[bpl])
                            S.op("dve", lambda: V.tensor_copy(out=pb_[:], in_=pl[:]), reads=[bpl], writes=[bpb_])
                            pt, bpt = ptr[tcount[0] % 2]
                            tcount[0] += 1
                            for kk in range(2):
                                S.op("pe", lambda: T.transpose(out=pt[:, kk, :], in_=pb_[:, kk * 128:(kk + 1) * 128],
                                                               identity=ident[:]), reads=[bpb_, bident], writes=[bpt])
                            S.op("act", lambda: A.copy(out=pTt[:, :, i * 128:(i + 1) * 128], in_=pt[:, 0:2, :]),
                                 reads=[bpt], writes=[bpT])
                        S.mark("pT")
                        for cb in range(4):
                            wt, bw = load_w(wpg_d[:, cb * 512:(cb + 1) * 512], key="pg%d" % cb, rowscale=(pnc, bpnc))
                            wt2, bw2 = load_w(wpe_d[:, cb * 512:(cb + 1) * 512], kchunks=2, key="pe%d" % cb)
                            for i in range(NT):
                                pm, bpm = mm_tm(i, wt, bw)
                                g_, bg_ = gt[i % 2]
                                S.op("act", lambda: A.activation(out=g_[:], in_=pm[:, :], func=AF.Sigmoid),
                                     reads=[bpm], writes=[bg_])
                                pm2, bpm2 = mm_tm(i, wt2, bw2, kchunks=2, lhs=pTt, blhs=bpT)
                                S.op("dve", lambda: V.tensor_tensor(out=g_[:], in0=g_[:], in1=pm2[:, :], op=ALU.mult),
                                     reads=[bg_, bpm2], writes=[bg_])
                                S.op("dve", lambda: V.tensor_tensor(out=XH[:, i, cb * 512:(cb + 1) * 512],
                                                                    in0=XH[:, i, cb * 512:(cb + 1) * 512], in1=g_[:],
                                                                    op=ALU.add), reads=[bg_, bXHt[i]], writes=[bXHt[i]])
                        S.mark("gate_e")
                        for i in range(NT):
                            u, bu = ub[i % 2]
                            S.op("act", lambda: A.activation(out=u[:], in_=XH[:, i, :], func=AF.Square,
                                                             accum_out=ssq[:, 4 + i:5 + i]),
                                 reads=[bXHt[i]], writes=[bu, bssq])
                            rstd_from_ssq(4 + i, D)
                            S.op("dve", lambda: V.scalar_tensor_tensor(out=XH[:, i, :], in0=XH[:, i, :],
                                                                       scalar=rstd[:, 4 + i:5 + i], in1=fnw[:],
                                                                       op0=ALU.mult, op1=ALU.mult),
                                 reads=[bXHt[i], brstd, bfnw], writes=[bXHt[i]])
                            S.dma("act", "sto%d_%d" % (cur_par, i), out_d[t0 + i * 128:t0 + (i + 1) * 128, :], XH[:, i, :], reads=[bXHt[i]])

                run_pass(full, src_d, ngroups_, flagcol_)
                S.barrier()

        def zero_state():
            S.op("dve", lambda: V.memset(Cst[:], 0.0), writes=[bC])
            S.op("dve", lambda: V.memset(Sst[:], 0.0), writes=[bS])
            S.op("dve", lambda: V.memset(Sb[:], 0.0), writes=[bSb])
            S.op("dve", lambda: V.memset(mcar[:], 0.0), writes=[bmcar])

        zero_state()
        S.op("dve", lambda: V.memset(Gseg[:], 0.0), writes=[bGseg])
        S.op("dve", lambda: V.memset(gsum[:], 0.0), writes=[bgsum])
        if nprev > 0:
            emit_pass(False, NT_SO, xprev_d, nprev * ntok // (128 * NT_SO), lambda t0: t0 // ntok)
        emit_pass(True, NT, x_d, ntok // (128 * NT))
    build_program.last_ninst = dict(S.ninst)
    return nc


_CFG = dict(ntok=SEG, NT=2, NT_SO=4, nprev=3)


def _flags_for(sgm, nprev):
    f = np.zeros((128, 16), np.float32)
    for j in range(nprev):
        if sgm - nprev + j >= 0:
            f[:, j] = 1.0
    return f


def kernel(**inputs):
    x = np.asarray(inputs["x"], np.float32)
    p = np.asarray(inputs["p"], np.float32)
    B, SQ, _ = x.shape
    nseg = NCORES // B
    seg = SQ // nseg
    cfg = dict(_CFG)
    cfg["ntok"] = seg
    cfg["nprev"] = nseg - 1
    nprev = cfg["nprev"]
    nc = build_program(**cfg)
    shared = {}
    for k in ["norm_w", "w_in", "conv_w", "conv_b", "ml_b_i", "ml_b_f", "ml_norm_w", "hg_norm_w", "w_out",
              "pe_norm_w", "w_pg", "w_pe"]:
        shared[k] = np.ascontiguousarray(np.asarray(inputs[k], np.float32)[0])
    shared["hg_lb"] = np.ascontiguousarray(np.asarray(inputs["hg_lb"], np.float32))
    shared["final_norm_w"] = np.ascontiguousarray(np.asarray(inputs["final_norm_w"], np.float32))
    in_maps = []
    for c in range(NCORES):
        b, sgm = c // nseg, c % nseg
        m = dict(shared)
        m["x"] = np.ascontiguousarray(x[b, sgm * seg:(sgm + 1) * seg])
        xp = np.zeros((nprev * seg, D), np.float32)
        if sgm > 0:
            xp[(nprev - sgm) * seg:] = x[b, 0:sgm * seg]
        m["xprev"] = xp
        m["p"] = np.ascontiguousarray(p[0, b, sgm * seg:(sgm + 1) * seg])
        m["flags"] = _flags_for(sgm, nprev)
        in_maps.append(m)
    res = run_bass_kernel_spmd(nc, in_maps, core_ids=list(range(NCORES)))
    out = np.empty((B, SQ, D), np.float32)
    for c in range(NCORES):
        b, sgm = c // nseg, c % nseg
        out[b, sgm * seg:(sgm + 1) * seg] = res.results[c]["out"]
    return out
```

```python
import numpy as np
from contextlib import ExitStack
import concourse.bass as bass
import concourse.mybir as mybir
from concourse.bass_utils import run_bass_kernel_spmd

F32 = mybir.dt.float32
BF16 = mybir.dt.bfloat16
AF = mybir.ActivationFunctionType
ALU = mybir.AluOpType
AX = mybir.AxisListType

D = 2048
INC = 8200
NCORES = 8
SEG = 4096
EPS = 1e-6
QSCALE = 128 ** -0.5
CC_INC = 16

C_Q, C_K, C_V, C_O, C_Z, C_I, C_F, C_HQ, C_HF, C_HI, C_HG = (
    0, 512, 1024, 2048, 3072, 4096, 4100, 4104, 5128, 6152, 7176)


class Buf:
    __slots__ = ("name", "w", "r")

    def __init__(self, name):
        self.name = name
        self.w = None
        self.r = {}


class Sched:
    def __init__(self, nc, stack, same_engine_sync=True):
        self.nc = nc
        self.stack = stack
        self.eng = {"pe": nc.tensor, "act": nc.scalar, "dve": nc.vector,
                    "pool": nc.gpsimd, "sp": nc.sync}
        self.sems = {}
        self.cnt = {}
        self.seen = {e: {} for e in self.eng}
        self.same = same_engine_sync
        for e in self.eng:
            self.newsem(e)
        self.ninst = {e: 0 for e in self.eng}
        self.stopped = False
        self.stop_at = None

    def mark(self, label):
        if self.stop_at is not None and label == self.stop_at:
            self.stopped = True

    def newsem(self, key):
        self.sems[key] = self.stack.enter_context(self.nc.semaphore("s_" + key))
        self.cnt[key] = 0
        return key

    def _deps(self, reads, writes):
        deps = {}

        def add(k, v):
            if deps.get(k, 0) < v:
                deps[k] = v
        for b in reads:
            if b.w is not None:
                add(*b.w)
        for b in writes:
            if b.w is not None:
                add(*b.w)
            for k, v in b.r.items():
                add(k, v)
        return deps

    def _wait(self, e, deps):
        seen = self.seen[e]
        for k, v in deps.items():
            if k == e and (not self.same or e == "pe"):
                continue
            if seen.get(k, 0) >= v:
                continue
            self.eng[e].wait_ge(self.sems[k], v)
            seen[k] = v

    def op(self, e, fn, reads=(), writes=()):
        if self.stopped:
            return None
        self._wait(e, self._deps(reads, writes))
        ins = fn()
        self.cnt[e] += 1
        v = self.cnt[e]
        ins.then_inc(self.sems[e], 1)
        self.ninst[e] += 1
        for b in reads:
            if b.r.get(e, 0) < v:
                b.r[e] = v
        for b in writes:
            b.w = (e, v)
            b.r = {}
        return ins

    def ops(self, e, fns, reads=(), writes=()):
        if self.stopped:
            return None
        self._wait(e, self._deps(reads, writes))
        ins = None
        for fn in fns:
            ins = fn()
        self.cnt[e] += 1
        v = self.cnt[e]
        ins.then_inc(self.sems[e], 1)
        self.ninst[e] += len(fns)
        for b in reads:
            if b.r.get(e, 0) < v:
                b.r[e] = v
        for b in writes:
            b.w = (e, v)
            b.r = {}
        return ins

    def dma(self, q, semkey, out, in_, reads=(), writes=(), **kw):
        if self.stopped:
            return None
        self._wait(q, self._deps(reads, writes))
        ins = self.eng[q].dma_start(out=out, in_=in_, **kw)
        self.cnt[semkey] += 16
        v = self.cnt[semkey]
        ins.then_inc(self.sems[semkey], 16)
        self.ninst[q] += 1
        for b in reads:
            if b.r.get(semkey, 0) < v:
                b.r[semkey] = v
        for b in writes:
            b.w = (semkey, v)
            b.r = {}
        return ins

    def barrier(self):
        if self.stopped:
            return
        deps = {k: v for k, v in self.cnt.items() if v > 0}
        for e in self.eng:
            self._wait(e, deps)

    def finish(self, e, bufs):
        deps = {}
        for b in bufs:
            toks = list(b.r.items())
            if b.w is not None:
                toks.append(b.w)
            for k, v in toks:
                if deps.get(k, 0) < v:
                    deps[k] = v
        self._wait(e, deps)


def build_program(ntok=SEG, NT=2, NT_SO=4, nprev=3, debug=None, stop_at=None, use_wsc=True):
    assert ntok % (128 * NT) == 0 and (nprev * ntok) % (128 * NT_SO) == 0
    nc = bass.Bass("TRN2", target_bir_lowering=False)
    dram_in = lambda n, s: nc.dram_tensor(n, s, F32, kind="ExternalInput").ap()
    x_d = dram_in("x", [ntok, D])
    xprev_d = dram_in("xprev", [max(nprev, 1) * ntok, D])
    p_d = dram_in("p", [ntok, 256])
    normw_d = dram_in("norm_w", [D])
    win_d = dram_in("w_in", [D, INC])
    convw_d = dram_in("conv_w", [4, 1024])
    convb_d = dram_in("conv_b", [1024])
    bi_d = dram_in("ml_b_i", [4])
    bf_d = dram_in("ml_b_f", [4])
    mlnw_d = dram_in("ml_norm_w", [1024])
    hglb_d = dram_in("hg_lb", [2, 1024])
    hgnw_d = dram_in("hg_norm_w", [1024])
    wout_d = dram_in("w_out", [D, D])
    penw_d = dram_in("pe_norm_w", [D])
    wpg_d = dram_in("w_pg", [D, D])
    wpe_d = dram_in("w_pe", [256, D])
    fnw_d = dram_in("final_norm_w", [D])
    flags_d = dram_in("flags", [128, 16])
    out_d = nc.dram_tensor("out", [ntok, D], F32, kind="ExternalOutput").ap()
    dbg_d = {}
    if debug:
        for n, s in debug.items():
            dbg_d[n] = nc.dram_tensor("dbg_" + n, list(s), F32, kind="ExternalOutput").ap()

    SW = 8
    with ExitStack() as st:
        S = Sched(nc, st)
        S.stop_at = stop_at

        def sb(name, shape, dt):
            t = st.enter_context(nc.sbuf_tensor(name, list(shape), dt))
            return t, Buf(name)

        def ps(name, shape, dt):
            t = st.enter_context(nc.psum_tensor(name, list(shape), dt))
            return t, Buf(name)

        V, A, P_, T = nc.vector, nc.scalar, nc.gpsimd, nc.tensor

        NW = 3
        Wb = [sb("W%d" % i, [128, 16, 512], BF16) for i in range(NW)]
        ub = [sb("ub%d" % i, [128, D], BF16) for i in range(2)]
        ptr = [ps("ptr%d" % i, [128, 8, 128], BF16) for i in range(2)]
        pmm = [ps("pmm%d" % i, [128, 512], F32) for i in range(2)]
        pst, bpst = ps("pst", [128, 512], F32)
        patt, bpatt = ps("patt", [128, 512], F32)
        po = [ps("po%d" % i, [128, 512], F32) for i in range(2)]
        ident, bident = sb("ident", [128, 128], BF16)
        identf, bidentf = sb("identf", [128, 128], F32)
        maskc, bmaskc = sb("maskc", [128, 128], F32)
        mask1, bmask1 = sb("mask1", [128, 128], F32)
        nwc, bnwc = sb("nwc", [128, 16], F32)
        pnc, bpnc = sb("pnc", [128, 16], F32)
        mixc, bmixc = sb("mixc", [128, 16], F32)
        cw, bcw = sb("cw", [128, 8, 4], F32)
        cbias, bcbias = sb("cbias", [128, 8], F32)
        lbt, blbt = sb("lbt", [128, 2, 8], F32)
        lb, blb = sb("lb", [128, 8], F32)
        oml, boml = sb("oml", [128, 8], F32)
        noml, bnoml = sb("noml", [128, 8], F32)
        gbias, bgbias = sb("gbias", [4, 3], F32)
        fnw, bfnw = sb("fnw", [128, D], F32)
        ones4, bones4 = sb("ones4", [4, 128], F32)
        flg, bflg = sb("flg", [128, 16], F32)
        halo, bhalo = sb("halo", [128, 8, 3], F32)
        mcar, bmcar = sb("mcar", [4, 1], F32)
        gsum, bgsum = sb("gsum", [4, 1], F32)
        Cst, bC = sb("Cst", [128, 4, 258], F32)
        Cb, bCb = sb("Cb", [128, 4, 258], BF16)
        Sst, bS = sb("Sst", [128, 8, 128], F32)
        Sb, bSb = sb("Sb", [128, 8, 128], BF16)
        Gseg, bGseg = sb("Gseg", [128, 8], F32)
        ssq, bssq = sb("ssq", [128, 8], F32)
        rstd, brstd = sb("rstd", [128, 8], F32)
        fin, bfin = sb("fin", [128, 64], F32)
        NTMAX = max(NT, NT_SO)
        for k in (["ldx%d_%d" % (q_, i) for q_ in range(2) for i in range(NTMAX)] + ["ldw%d" % i for i in range(NW)] +
                  ["ldc", "ldp0", "ldp1", "dbg"] + ["wbk%d" % i for i in range(NW)] +
                  ["ldwh%d" % i for i in range(NW)] + ["sto%d_%d" % (q_, i) for q_ in range(2) for i in range(NTMAX)]):
            S.newsem(k)

        def dbg(name, ap, bufs):
            if name in dbg_d:
                S.dma("sp", "dbg", dbg_d[name], ap, reads=bufs)

        def cdma(out, in_, wb, **kw):
            S.dma("sp", "ldc", out, in_, writes=[wb], **kw)
        cdma(nwc[:], normw_d.rearrange("(k p) -> p k", p=128), bnwc, allow_slow_non_contiguous=True)
        cdma(pnc[:], penw_d.rearrange("(k p) -> p k", p=128), bpnc, allow_slow_non_contiguous=True)
        cdma(mixc[:, 0:8], mlnw_d.rearrange("(k p) -> p k", p=128), bmixc, allow_slow_non_contiguous=True)
        cdma(mixc[:, 8:16], hgnw_d.rearrange("(k p) -> p k", p=128), bmixc, allow_slow_non_contiguous=True)
        for kk in range(4):
            cdma(cw[:, :, kk], convw_d[kk].rearrange("(c p) -> p c", p=128), bcw, allow_slow_non_contiguous=True)
        cdma(cbias[:], convb_d.rearrange("(c p) -> p c", p=128), bcbias, allow_slow_non_contiguous=True)
        for r_ in range(2):
            cdma(lbt[:, r_, :], hglb_d[r_].rearrange("(h p) -> p h", p=128), blbt, allow_slow_non_contiguous=True)
        cdma(gbias[:, 0:1], bi_d.rearrange("(h o) -> h o", o=1), bgbias, allow_slow_non_contiguous=True)
        cdma(gbias[:, 1:2], bf_d.rearrange("(h o) -> h o", o=1), bgbias, allow_slow_non_contiguous=True)
        cdma(fnw[:], fnw_d.partition_broadcast(128), bfnw)
        cdma(flg[:], flags_d, bflg)
        for b_ in (bnwc, bpnc, bmixc, bcw, bcbias, blbt, bgbias, bfnw, bflg):
            b_.w = ("ldc", S.cnt["ldc"])

        S.op("pool", lambda: P_.memset(identf[:], 0.0), writes=[bidentf])
        S.op("pool", lambda: P_.affine_select(out=identf[:], in_=identf[:], pattern=[[-1, 128]],
                                              compare_op=ALU.not_equal, fill=1.0, base=0, channel_multiplier=1),
             reads=[bidentf], writes=[bidentf])
        S.op("dve", lambda: V.tensor_copy(out=ident[:], in_=identf[:]), reads=[bidentf], writes=[bident])
        for (mk, bm, val) in ((maskc, bmaskc, QSCALE), (mask1, bmask1, 1.0)):
            S.op("pool", lambda: P_.memset(mk[:], val), writes=[bm])
            S.op("pool", lambda: P_.affine_select(out=mk[:], in_=mk[:], pattern=[[1, 128]],
                                                  compare_op=ALU.is_ge, fill=0.0, base=0, channel_multiplier=-1),
                 reads=[bm], writes=[bm])
        S.op("pool", lambda: P_.memset(ones4[:], 1.0), writes=[bones4])
        S.op("dve", lambda: V.tensor_scalar(out=gbias[:, 2:3], in0=gbias[:, 1:2], scalar1=-1.0, scalar2=None, op0=ALU.mult),
             reads=[bgbias], writes=[bgbias])
        S.op("dve", lambda: V.tensor_sub(out=lb[:], in0=lbt[:, 0, :], in1=lbt[:, 1, :]), reads=[blbt], writes=[blb])
        S.op("act", lambda: A.activation(out=oml[:], in_=lb[:], func=AF.Sigmoid, scale=-1.0), reads=[blb], writes=[boml])
        S.op("act", lambda: A.activation(out=lb[:], in_=lb[:], func=AF.Sigmoid), reads=[blb], writes=[blb])
        S.op("dve", lambda: V.tensor_scalar(out=noml[:], in0=oml[:], scalar1=-1.0, scalar2=None, op0=ALU.mult),
             reads=[boml], writes=[bnoml])

        S.mark("consts")
        wslot = [0]

        wsc = {}

        def load_w(src_ap, kchunks=16, ncols=512, key=None, rowscale=None):
            s = wslot[0] % NW
            wslot[0] += 1
            wt, bw = Wb[s]
            if key is not None and key in wsc:
                sc_ap, bsc = wsc[key]
                S.dma("sp", "ldwh%d" % s, wt[:, 0:kchunks, 0:ncols],
                      sc_ap.rearrange("p (k c) -> p k c", c=ncols), reads=[bsc], writes=[bw])
                return wt, bw
            v = src_ap.rearrange("(k p) c -> p k c", p=128)
            step = 4 if kchunks >= 4 else kchunks
            for k0 in range(0, kchunks, step):
                S.dma("pool", "ldw%d" % s, wt[:, k0:k0 + step, 0:ncols], v[:, k0:k0 + step, :], writes=[bw])
            if rowscale is not None:
                colw_, bcolw_ = rowscale
                for k in range(kchunks):
                    if k % 2 == 0:
                        S.op("dve", lambda: V.tensor_scalar(out=wt[:, k, 0:ncols], in0=wt[:, k, 0:ncols],
                                                            scalar1=colw_[:, k:k + 1], scalar2=None, op0=ALU.mult),
                             reads=[bw, bcolw_], writes=[bw])
                    else:
                        S.op("act", lambda: A.activation(out=wt[:, k, 0:ncols], in_=wt[:, k, 0:ncols], func=AF.Copy,
                                                         scale=colw_[:, k:k + 1]), reads=[bw, bcolw_], writes=[bw])
            if key is not None and use_wsc:
                sc_ap = nc.dram_tensor("wsc_" + key, [128, kchunks * ncols], BF16).ap()
                bsc = Buf("wsc_" + key)
                S.dma("sp", "wbk%d" % s, sc_ap.rearrange("p (k c) -> p k c", c=ncols), wt[:, 0:kchunks, 0:ncols],
                      reads=[bw], writes=[bsc])
                wsc[key] = (sc_ap, bsc)
            return wt, bw

        def rstd_from_ssq(col, n, width=1):
            sl = slice(col, col + width)
            S.op("dve", lambda: V.tensor_scalar(out=rstd[:, sl], in0=ssq[:, sl], scalar1=1.0 / n,
                                                scalar2=EPS, op0=ALU.mult, op1=ALU.add), reads=[bssq], writes=[brstd])
            S.op("act", lambda: A.activation(out=rstd[:, sl], in_=rstd[:, sl], func=AF.Ln), reads=[brstd], writes=[brstd])
            S.op("act", lambda: A.activation(out=rstd[:, sl], in_=rstd[:, sl], func=AF.Exp, scale=-0.5),
                 reads=[brstd], writes=[brstd])

        S.op("dve", lambda: V.memset(halo[:], 0.0), writes=[bhalo])

        def emit_pass(full, NT, src_d, ngroups_, flagcol_=None):
            G = 128 * NT
            NCH = NT
            QOFF = 0 if full else 4
            with ExitStack() as st2:
                def sb(name, shape, dt):
                    t = st2.enter_context(nc.sbuf_tensor(name + ("_f" if full else "_s"), list(shape), dt))
                    return t, Buf(name)

                NPAR = 2 if full else 1
                XH_l = [sb("XH%d" % q_, [128, NT, D], F32) for q_ in range(NPAR)]
                bXHt_l = [[Buf("XH%d_%d" % (q_, i)) for i in range(NT)] for q_ in range(NPAR)]
                actT_l = [sb("actT%d" % q_, [128, 16, G], BF16) for q_ in range(NPAR)]
                XH, bXHt = XH_l[0][0], bXHt_l[0]
                actT, bactT = actT_l[0]

                cur_par = 0

                def select(par):
                    nonlocal XH, bXHt, actT, bactT, cur_par
                    par = par % NPAR
                    cur_par = par
                    XH, bXHt = XH_l[par][0], bXHt_l[par]
                    actT, bactT = actT_l[par]
                mreset, bmreset = sb("mreset", [128, G], F32)
                dmask, bdmask = sb("dmask", [4, 4, NCH], F32)
                cpre = [sb("cpre%d" % i, [128, G + 3], F32) for i in range(2)]
                cacc = [sb("cacc%d" % i, [128, G], F32) for i in range(2)]
                qkT, bqkT = sb("qkT", [128, 8 if full else 4, G], BF16)
                bqk = [Buf("qk%d" % i) for i in range(8)]
                kd, bkd_ = sb("kd", [128, 8, G], BF16)
                bkd = [Buf("kd%d" % i) for i in range(8)]
                bqd = [Buf("qd%d" % i) for i in range(8)]
                beb = [Buf("eb%d" % i) for i in range(2)]
                if full:
                    qd, bqd_ = sb("qd", [128, 8, G], BF16)
                    ebf, bebf_ = sb("ebf", [128, 2, G], F32)
                expg, bexpg_ = sb("expg", [128, 8, NCH], F32)
                eref, beref_ = sb("eref", [128, 8, NCH], F32)
                egr, begr_ = sb("egr", [128, 8, NCH], F32)
                hsm, bhsm_ = sb("hsm", [128, 8, 4, NCH], F32)
                bhsm = [Buf("hsm%d" % i) for i in range(8)]
                bexpg = [Buf("expg%d" % i) for i in range(8)]
                hft = [sb("hft%d_%d" % (i, j), [128, G], F32) for i in range(2) for j in range(3)]
                kgt = [sb("kgt%d" % i, [128, G], BF16) for i in range(8)]
                vaug, bvaug_ = sb("vaug", [128, NT, 4, 258], BF16)
                bvaug = [Buf("vaug%d" % i) for i in range(NT)]
                bGY = [[Buf("GY%d_%d" % (i, j)) for j in range(4)] for i in range(NT)]
                if full:
                    GY, bGY_ = sb("GY", [128, NT, D], BF16)
                iv, biv_ = sb("iv", [128, NT, 1024], BF16)
                biv = [Buf("iv%d" % i) for i in range(NT)]
                kTM, bkTM_ = sb("kTM", [128, NT, 512], BF16)
                bkTM = Buf("kTM")
                kgTM, bkgTM_ = sb("kgTM", [128, NT, 1024], BF16)
                bkgTM = [Buf("kgTM%d" % i) for i in range(8)]
                vs = [sb("vs%d" % i, [128, 4, 258], BF16) for i in range(2)]
                (gi_r, bgi_r), (gf_r, bgf_r), (gb_r, bgb_r), (ga_r, bga_r), (gea_r, bgea_r), (gfl_r, bgfl_r) = [
                    (t[0:4, :], b) for (t, b) in hft]
                gsm, bgsm = sb("gsm", [4, 8, NCH], F32)
                scR, bscR = sb("scR", [4, 4, NCH], F32)
                scb, bscb = sb("scb", [128, 4, NCH], F32)
                eaT, beaT = sb("eaT", [128, NT, 4], F32)
                flT, bflT = sb("flT", [128, NT, 4], F32)
                if full:
                    ztmp = [sb("ztmp%d" % i, [128, 512], BF16) for i in range(2)]
                    stm, bstm = sb("stm", [128, 512], BF16)
                    sqt, bsqt = sb("sqt", [128, 1024], F32)
                    pTt, bpT = sb("pT", [128, 2, G], BF16)
                    ptile = [sb("ptile%d" % i, [128, 256], F32) for i in range(2)]
                    pbt = [sb("pbt%d" % i, [128, 256], BF16) for i in range(2)]
                    gt = [sb("gt%d" % i, [128, 512], F32) for i in range(2)]
                S.op("pool", lambda: P_.memset(mreset[:], 1.0), writes=[bmreset])
                S.op("pool", lambda: P_.memset(mreset[:].rearrange("p (c l) -> p c l", l=128)[:, :, 0:1], 0.0),
                     writes=[bmreset])
                S.op("pool", lambda: P_.memset(dmask[:], 1.0), writes=[bdmask])
                S.op("pool", lambda: P_.affine_select(out=dmask[:], in_=dmask[:], pattern=[[1, 4], [0, NCH]],
                                                      compare_op=ALU.is_equal, fill=0.0, base=0, channel_multiplier=-1),
                     reads=[bdmask], writes=[bdmask])
                S.op("pool", lambda: P_.memset(vaug[:], 1.0), writes=bvaug)

                tcount = [0]

                def norm_transpose(src_ap, src_buf, i, colw, bcolw):
                    u, bu = ub[tcount[0] % 2]
                    S.op("act", lambda: A.activation(out=u[:], in_=src_ap, func=AF.Square, accum_out=ssq[:, i:i + 1]),
                         reads=[src_buf], writes=[bu, bssq])
                    rstd_from_ssq(i, D)
                    S.op("dve", lambda: V.tensor_scalar(out=u[:], in0=src_ap, scalar1=rstd[:, i:i + 1], scalar2=None,
                                                        op0=ALU.mult), reads=[src_buf, brstd], writes=[bu])
                    transpose16(u, bu, i, colw, bcolw)

                def transpose16(u, bu, i, colw, bcolw):
                    for hb in range(2):
                        pt, bpt = ptr[tcount[0] % 2]
                        tcount[0] += 1
                        S.ops("pe", [(lambda j=j: T.transpose(out=pt[:, j, :], in_=u[:, (hb * 8 + j) * 128:(hb * 8 + j + 1) * 128],
                                                              identity=ident[:])) for j in range(8)],
                              reads=[bu, bident], writes=[bpt])
                        if hb == 0:
                            S.op("act", lambda: A.copy(out=actT[:, 0:8, i * 128:(i + 1) * 128], in_=pt[:, :, :]),
                                 reads=[bpt], writes=[bactT])
                        else:
                            S.op("dve", lambda: V.tensor_copy(out=actT[:, 8:16, i * 128:(i + 1) * 128], in_=pt[:, :, :]),
                                 reads=[bpt], writes=[bactT])

                mmc = [0]
                pj = [0]
                pbanks = pmm + po + [(patt, bpatt)]

                def mm_tm(i, wt, bw, kchunks=16, lhs=None, blhs=None, ncols=512):
                    pm, bpm = pbanks[pj[0] % len(pbanks)]
                    pj[0] += 1
                    lhs = actT if lhs is None else lhs
                    blhs = bactT if blhs is None else blhs
                    S.ops("pe", [(lambda k=k: T.matmul(pm[:, 0:ncols], lhsT=lhs[:, k, i * 128:(i + 1) * 128],
                                                       rhs=wt[:, k, 0:ncols], start=(k == 0), stop=(k == kchunks - 1)))
                                 for k in range(kchunks)], reads=[blhs, bw], writes=[bpm])
                    return pm, bpm

                def mm_fm(wt, bw, c0, m, ntoks=G, tok0=0):
                    pm, bpm = pbanks[pj[0] % len(pbanks)]
                    pj[0] += 1
                    S.ops("pe", [(lambda k=k: T.matmul(pm[0:m, 0:ntoks], lhsT=wt[:, k, c0:c0 + m],
                                                       rhs=actT[:, k, tok0:tok0 + ntoks], start=(k == 0), stop=(k == 15)))
                                 for k in range(16)], reads=[bactT, bw], writes=[bpm])
                    return pm, bpm

                cvc = [0]

                def conv_block(pm, bpm, c8, ntoks=G, only_halo=False):
                    cp, bcp = cpre[cvc[0] % 2]
                    ca, bca = cacc[cvc[0] % 2]
                    cvc[0] += 1
                    S.op("dve", lambda: V.tensor_copy(out=cp[:, 0:3], in_=halo[:, c8, :]), reads=[bhalo], writes=[bcp])
                    S.op("act", lambda: A.copy(out=cp[:, 3:3 + ntoks], in_=pm[:, 0:ntoks]), reads=[bpm], writes=[bcp])
                    S.op("dve", lambda: V.tensor_copy(out=halo[:, c8, :], in_=cp[:, ntoks:ntoks + 3]), reads=[bcp], writes=[bhalo])
                    if only_halo:
                        return
                    S.op("dve", lambda: V.tensor_scalar(out=ca[:, 0:ntoks], in0=cp[:, 0:ntoks], scalar1=cw[:, c8, 0:1],
                                                        scalar2=cbias[:, c8:c8 + 1], op0=ALU.mult, op1=ALU.add),
                         reads=[bcp, bcw, bcbias], writes=[bca])
                    for kk in range(1, 4):
                        S.op("dve", lambda: V.scalar_tensor_tensor(out=ca[:, 0:ntoks], in0=cp[:, kk:kk + ntoks],
                                                                   scalar=cw[:, c8, kk:kk + 1], in1=ca[:, 0:ntoks],
                                                                   op0=ALU.mult, op1=ALU.add),
                             reads=[bcp, bcw, bca], writes=[bca])
                    S.op("act", lambda: A.activation(out=qkT[:, c8 - QOFF, 0:ntoks], in_=ca[:, 0:ntoks], func=AF.Silu),
                         reads=[bca], writes=[bqk[c8]])

                def k_to_tm():
                    for h in range(4):
                        pt, bpt = ptr[tcount[0] % 2]
                        tcount[0] += 1
                        S.ops("pe", [(lambda i=i: T.transpose(out=pt[:, i, :], in_=qkT[:, 4 + h - QOFF, i * 128:(i + 1) * 128],
                                                              identity=ident[:])) for i in range(NT)],
                              reads=[bqk[4 + h], bident], writes=[bpt])
                        S.op("act", lambda: A.copy(out=kTM[:, :, h * 128:(h + 1) * 128], in_=pt[:, 0:NT, :]),
                             reads=[bpt], writes=[bkTM])

                hfc = [0]
                E38 = float(np.exp(38.0))

                def hf_block(pm, bpm, h, full):
                    s = hfc[0] % 2
                    hfc[0] += 1
                    (t0, b0), (t1, b1), (t2, b2) = hft[s * 3], hft[s * 3 + 1], hft[s * 3 + 2]
                    kg, bkg = kgt[h]
                    hl = h % 2
                    S.op("act", lambda: A.activation(out=t0[:], in_=pm[:, 0:G], func=AF.Exp, scale=-1.0), reads=[bpm], writes=[b0])
                    S.op("dve", lambda: V.tensor_scalar(out=t0[:], in0=t0[:], scalar1=1.0, scalar2=None, op0=ALU.add),
                         reads=[b0], writes=[b0])
                    S.op("dve", lambda: V.reciprocal(out=t0[:], in_=t0[:]), reads=[b0], writes=[b0])
                    S.op("act", lambda: A.activation(out=t1[:], in_=t0[:], func=AF.Ln, scale=oml[:, h:h + 1], bias=lb[:, h:h + 1]),
                         reads=[b0, boml, blb], writes=[b1])
                    S.op("act", lambda: A.activation(out=t0[:], in_=t0[:], func=AF.Identity, scale=noml[:, h:h + 1],
                                                     bias=oml[:, h:h + 1]), reads=[b0, bnoml, boml], writes=[b0])
                    S.op("dve", lambda: V.tensor_tensor_scan(out=t2[:], data0=mreset[:], data1=t1[:], initial=0.0,
                                                             op0=ALU.mult, op1=ALU.add), reads=[bmreset, b1], writes=[b2])
                    b3 = t2[:].rearrange("p (c l) -> p c l", l=128)
                    S.op("dve", lambda: V.tensor_copy(out=hsm[:, h, 0, :], in_=b3[:, :, 63]), reads=[b2], writes=[bhsm[h]])
                    S.op("dve", lambda: V.tensor_scalar(out=hsm[:, h, 1, :], in0=b3[:, :, 63], scalar1=-1.0, scalar2=None,
                                                        op0=ALU.mult), reads=[b2], writes=[bhsm[h]])
                    S.op("dve", lambda: V.tensor_sub(out=hsm[:, h, 2, :], in0=b3[:, :, 127], in1=b3[:, :, 63]),
                         reads=[b2], writes=[bhsm[h]])
                    S.op("act", lambda: A.activation(out=expg[:, h, :], in_=b3[:, :, 127], func=AF.Exp),
                         reads=[b2], writes=[bexpg[h]])
                    S.op("act", lambda: A.activation(out=eref[:, h, :], in_=hsm[:, h, 0, :], func=AF.Exp),
                         reads=[bhsm[h]], writes=[bexpg[h]])
                    S.op("act", lambda: A.activation(out=egr[:, h, :], in_=hsm[:, h, 2, :], func=AF.Exp),
                         reads=[bhsm[h]], writes=[bexpg[h]])
                    for c in range(NCH):
                        cs = slice(c * 128, (c + 1) * 128)
                        S.op("act", lambda: A.activation(out=t1[:, cs], in_=t2[:, cs], func=AF.Exp, scale=-1.0,
                                                         bias=hsm[:, h, 0, c:c + 1]), reads=[b2, bhsm[h]], writes=[b1])
                        if full:
                            S.op("act", lambda: A.activation(out=ebf[:, hl, cs], in_=t2[:, cs], func=AF.Exp,
                                                             bias=hsm[:, h, 1, c:c + 1]), reads=[b2, bhsm[h]], writes=[beb[hl]])
                    S.op("dve", lambda: V.scalar_tensor_tensor(out=kd[:, h, :], in0=t1[:], scalar=E38, in1=t0[:],
                                                               op0=ALU.min, op1=ALU.mult), reads=[b0, b1], writes=[bkd[h]])
                    S.op("dve", lambda: V.tensor_tensor(out=kg[:].rearrange("p (c l) -> p c l", l=128),
                                                        in0=kd[:, h, :].rearrange("p (c l) -> p c l", l=128),
                                                        in1=egr[:, h, :].unsqueeze(2).to_broadcast([128, NCH, 128]),
                                                        op=ALU.mult), reads=[bkd[h], bexpg[h]], writes=[bkg])

                def kg_to_tm(h):
                    kg, bkg = kgt[h]
                    pt, bpt = ptr[tcount[0] % 2]
                    tcount[0] += 1
                    S.ops("pe", [(lambda i=i: T.transpose(out=pt[:, i, :], in_=kg[:, i * 128:(i + 1) * 128], identity=ident[:]))
                                 for i in range(NT)], reads=[bkg, bident], writes=[bpt])
                    S.op("act", lambda: A.copy(out=kgTM[:, :, h * 128:(h + 1) * 128], in_=pt[:, 0:NT, :]),
                         reads=[bpt], writes=[bkgTM[h]])

                def gates_rows(pm_i, bpm_i, pm_f, bpm_f):
                    S.op("act", lambda: A.activation(out=gi_r, in_=pm_i[0:4, 0:G], func=AF.Identity, bias=gbias[:, 0:1]),
                         reads=[bpm_i, bgbias], writes=[bgi_r])
                    S.op("act", lambda: A.activation(out=gf_r, in_=pm_f[0:4, 0:G], func=AF.Exp, scale=-1.0, bias=gbias[:, 2:3]),
                         reads=[bpm_f, bgbias], writes=[bgf_r])
                    S.op("act", lambda: A.activation(out=gf_r, in_=gf_r, func=AF.Ln, bias=1.0), reads=[bgf_r], writes=[bgf_r])
                    S.op("dve", lambda: V.tensor_scalar(out=gf_r, in0=gf_r, scalar1=-1.0, scalar2=None, op0=ALU.mult),
                         reads=[bgf_r], writes=[bgf_r])
                    S.op("dve", lambda: V.tensor_tensor_scan(out=gb_r, data0=mreset[0:4, :], data1=gf_r, initial=0.0,
                                                             op0=ALU.mult, op1=ALU.add), reads=[bmreset, bgf_r], writes=[bgb_r])
                    S.op("dve", lambda: V.tensor_sub(out=ga_r, in0=gi_r, in1=gb_r), reads=[bgi_r, bgb_r], writes=[bga_r])
                    a3 = ga_r.rearrange("p (c l) -> p c l", l=128)
                    b3 = gb_r.rearrange("p (c l) -> p c l", l=128)
                    AMAX, GSH, MM, GM, MPREV, SC = range(6)
                    S.op("dve", lambda: V.tensor_reduce(out=gsm[:, AMAX, :], in_=a3, axis=AX.X, op=ALU.max),
                         reads=[bga_r], writes=[bgsm])
                    S.op("dve", lambda: V.memset(gsm[:, GSH, 0:1], 0.0), writes=[bgsm])
                    if NCH > 1:
                        S.op("dve", lambda: V.tensor_copy(out=gsm[:, GSH, 1:NCH], in_=b3[:, 0:NCH - 1, 127]),
                             reads=[bgb_r], writes=[bgsm])
                    S.op("dve", lambda: V.tensor_tensor_scan(out=gsm[:, MM, :], data0=gsm[:, GSH, :], data1=gsm[:, AMAX, :],
                                                             initial=mcar[:, 0:1], op0=ALU.add, op1=ALU.max),
                         reads=[bgsm, bmcar], writes=[bgsm])
                    S.op("dve", lambda: V.tensor_add(out=gsm[:, GM, :], in0=gsm[:, MM, :], in1=b3[:, :, 127]),
                         reads=[bgsm, bgb_r], writes=[bgsm])
                    S.op("dve", lambda: V.tensor_copy(out=gsm[:, MPREV, 0:1], in_=mcar[:, 0:1]), reads=[bmcar], writes=[bgsm])
                    if NCH > 1:
                        S.op("dve", lambda: V.tensor_copy(out=gsm[:, MPREV, 1:NCH], in_=gsm[:, GM, 0:NCH - 1]),
                             reads=[bgsm], writes=[bgsm])
                    S.op("dve", lambda: V.tensor_copy(out=mcar[:, 0:1], in_=gsm[:, GM, NCH - 1:NCH]), reads=[bgsm], writes=[bmcar])
                    S.op("dve", lambda: V.tensor_sub(out=gsm[:, SC, :], in0=gsm[:, MPREV, :], in1=gsm[:, MM, :]),
                         reads=[bgsm], writes=[bgsm])
                    S.op("act", lambda: A.activation(out=gsm[:, SC, :], in_=gsm[:, SC, :], func=AF.Exp), reads=[bgsm], writes=[bgsm])
                    Mb = gsm[:, MM, :].unsqueeze(2).to_broadcast([4, NCH, 128])
                    S.op("dve", lambda: V.tensor_tensor(out=gea_r.rearrange("p (c l) -> p c l", l=128), in0=a3, in1=Mb,
                                                        op=ALU.subtract), reads=[bga_r, bgsm], writes=[bgea_r])
                    S.op("act", lambda: A.activation(out=gea_r, in_=gea_r, func=AF.Exp), reads=[bgea_r], writes=[bgea_r])
                    S.op("dve", lambda: V.tensor_tensor(out=gfl_r.rearrange("p (c l) -> p c l", l=128), in0=b3, in1=Mb,
                                                        op=ALU.add), reads=[bgb_r, bgsm], writes=[bgfl_r])
                    S.op("act", lambda: A.activation(out=gfl_r, in_=gfl_r, func=AF.Exp, scale=-1.0),
                         reads=[bgfl_r], writes=[bgfl_r])
                    S.op("dve", lambda: V.tensor_tensor(out=scR[:], in0=dmask[:],
                                                        in1=gsm[:, SC, :].unsqueeze(1).to_broadcast([4, 4, NCH]), op=ALU.mult),
                         reads=[bdmask, bgsm], writes=[bscR])

                def gates_bcast():
                    SC = 5
                    pm, bpm = pmm[mmc[0] % 2]
                    mmc[0] += 1
                    S.op("pe", lambda: T.matmul(pm[:, 0:4 * NCH], lhsT=ones4[:], rhs=scR[:].rearrange("p h c -> p (h c)"),
                                                start=True, stop=True), reads=[bones4, bscR], writes=[bpm])
                    S.op("dve", lambda: V.tensor_copy(out=scb[:].rearrange("p h c -> p (h c)"), in_=pm[:, 0:4 * NCH]),
                         reads=[bpm], writes=[bscb])
                    pm, bpm = pmm[mmc[0] % 2]
                    mmc[0] += 1
                    for i in range(NT):
                        S.op("pe", lambda: T.matmul(pm[:, 8 * i:8 * i + 4], lhsT=gea_r[:, i * 128:(i + 1) * 128],
                                                    rhs=identf[0:4, 0:4], start=True, stop=True),
                             reads=[bgea_r, bidentf], writes=[bpm])
                        S.op("pe", lambda: T.matmul(pm[:, 8 * i + 4:8 * i + 8], lhsT=gfl_r[:, i * 128:(i + 1) * 128],
                                                    rhs=identf[0:4, 0:4], start=True, stop=True),
                             reads=[bgfl_r, bidentf], writes=[bpm])
                    pv = pm[:, 0:8 * NT].rearrange("p (i e) -> p i e", e=8)
                    if cur_flag[0] is None:
                        S.op("dve", lambda: V.tensor_copy(out=eaT[:], in_=pv[:, :, 0:4]), reads=[bpm], writes=[beaT])
                    else:
                        fc = cur_flag[0]
                        S.op("dve", lambda: V.tensor_scalar(out=eaT[:], in0=pv[:, :, 0:4], scalar1=flg[:, fc:fc + 1], scalar2=None,
                                                            op0=ALU.mult), reads=[bpm, bflg], writes=[beaT])
                    S.op("dve", lambda: V.tensor_copy(out=flT[:], in_=pv[:, :, 4:8]), reads=[bpm], writes=[bflT])

                vsc = [0]
                cur_flag = [None]

                def mixer_chunk(c, full):
                    i = c
                    tc = slice(c * 128, (c + 1) * 128)
                    v_, bv_ = vs[vsc[0] % 2]
                    vsc[0] += 1
                    S.op("dve", lambda: V.tensor_tensor(out=v_[:], in0=vaug[:, i, :, :],
                                                        in1=eaT[:, i, :].unsqueeze(2).to_broadcast([128, 4, 258]),
                                                        op=ALU.mult), reads=[bvaug[i], beaT], writes=[bv_])
                    S.op("dve", lambda: V.tensor_tensor(out=Cst[:], in0=Cst[:],
                                                        in1=scb[:, :, c:c + 1].to_broadcast([128, 4, 258]), op=ALU.mult),
                         reads=[bC, bscb], writes=[bC])
                    if full:
                        S.op("act", lambda: A.activation(out=Cb[:], in_=Cst[:], func=AF.Copy, scale=QSCALE),
                             reads=[bC], writes=[bCb])
                        for h in range(4):
                            S.op("pe", lambda: T.matmul(patt[:, h * 128:(h + 1) * 128], lhsT=qkT[:, 4 + h, tc],
                                                        rhs=qkT[:, h, tc], start=True, stop=True),
                                 reads=[bqk[4 + h], bqk[h]], writes=[bpatt])
                        S.op("dve", lambda: V.tensor_tensor(out=stm[:].rearrange("p (h t) -> p h t", t=128),
                                                            in0=patt[:, :].rearrange("p (h t) -> p h t", t=128),
                                                            in1=maskc[:].unsqueeze(1).to_broadcast([128, 4, 128]), op=ALU.mult),
                             reads=[bpatt, bmaskc], writes=[bstm])
                        pmd, bpmd = pmm[mmc[0] % 2]
                        mmc[0] += 1
                        for h in range(4):
                            pm, bpm = po[h // 2]
                            osl = slice((h % 2) * 256, (h % 2) * 256 + 256)
                            S.op("pe", lambda: T.matmul(pm[:, osl], lhsT=stm[:, h * 128:(h + 1) * 128],
                                                        rhs=v_[:, h, 0:256], start=True, stop=False),
                                 reads=[bstm, bv_], writes=[bpm])
                            S.op("pe", lambda: T.matmul(pm[:, osl], lhsT=qkT[:, h, tc], rhs=Cb[:, h, 0:256],
                                                        start=False, stop=True), reads=[bqk[h], bCb], writes=[bpm])
                            S.op("pe", lambda: T.matmul(pmd[:, h:h + 1], lhsT=stm[:, h * 128:(h + 1) * 128],
                                                        rhs=v_[:, h, 256:257], start=True, stop=False),
                                 reads=[bstm, bv_], writes=[bpmd])
                            S.op("pe", lambda: T.matmul(pmd[:, h:h + 1], lhsT=qkT[:, h, tc], rhs=Cb[:, h, 256:257],
                                                        start=False, stop=True), reads=[bqk[h], bCb], writes=[bpmd])
                        f_ = fin
                        S.op("act", lambda: A.activation(out=f_[:, 0:4], in_=pmd[:, 0:4], func=AF.Abs),
                             reads=[bpmd], writes=[bfin])
                        S.op("dve", lambda: V.tensor_tensor(out=f_[:, 0:4], in0=f_[:, 0:4], in1=flT[:, i, :], op=ALU.max),
                             reads=[bfin, bflT], writes=[bfin])
                        S.op("dve", lambda: V.reciprocal(out=f_[:, 0:4], in_=f_[:, 0:4]), reads=[bfin], writes=[bfin])
                        for half in range(2):
                            pm, bpm = po[half]
                            S.op("act", lambda: A.activation(out=sqt[:, half * 512:(half + 1) * 512], in_=pm[:, :],
                                                             func=AF.Square), reads=[bpm], writes=[bsqt])
                        S.op("dve", lambda: V.tensor_reduce(out=f_[:, 4:8], in_=sqt[:, :].rearrange("p (h v) -> p h v", v=256),
                                                            axis=AX.X, op=ALU.add), reads=[bsqt], writes=[bfin])
                        S.op("dve", lambda: V.tensor_tensor(out=f_[:, 8:12], in0=f_[:, 0:4], in1=f_[:, 0:4], op=ALU.mult),
                             reads=[bfin], writes=[bfin])
                        S.op("dve", lambda: V.tensor_tensor(out=f_[:, 8:12], in0=f_[:, 8:12], in1=f_[:, 4:8], op=ALU.mult),
                             reads=[bfin], writes=[bfin])
                        S.op("dve", lambda: V.tensor_scalar(out=f_[:, 8:12], in0=f_[:, 8:12], scalar1=1.0 / 256, scalar2=EPS,
                                                            op0=ALU.mult, op1=ALU.add), reads=[bfin], writes=[bfin])
                        S.op("act", lambda: A.activation(out=f_[:, 8:12], in_=f_[:, 8:12], func=AF.Ln),
                             reads=[bfin], writes=[bfin])
                        S.op("act", lambda: A.activation(out=f_[:, 8:12], in_=f_[:, 8:12], func=AF.Exp, scale=-0.5),
                             reads=[bfin], writes=[bfin])
                        S.op("dve", lambda: V.tensor_tensor(out=f_[:, 8:12], in0=f_[:, 8:12], in1=f_[:, 0:4], op=ALU.mult),
                             reads=[bfin], writes=[bfin])
                        for h in range(4):
                            pm, bpm = po[h // 2]
                            osl = slice((h % 2) * 256, (h % 2) * 256 + 256)
                            gsl = slice(h * 256, (h + 1) * 256)
                            S.op("dve", lambda: V.scalar_tensor_tensor(out=GY[:, i, gsl], in0=pm[:, osl],
                                                                       scalar=f_[:, 8 + h:9 + h], in1=GY[:, i, gsl],
                                                                       op0=ALU.mult, op1=ALU.mult),
                                 reads=[bpm, bfin, bGY[i][h // 2]], writes=[bGY[i][h // 2]])
                    for half in range(2):
                        for hq in range(2):
                            h = half * 2 + hq
                            S.op("pe", lambda: T.matmul(pst[:, hq * 256:(hq + 1) * 256], lhsT=kTM[:, i, h * 128:(h + 1) * 128],
                                                        rhs=v_[:, h, 0:256], start=True, stop=True),
                                 reads=[bkTM, bv_], writes=[bpst])
                        S.op("dve", lambda: V.tensor_tensor(out=Cst[:, half * 2:half * 2 + 2, 0:256],
                                                            in0=Cst[:, half * 2:half * 2 + 2, 0:256],
                                                            in1=pst[:, :].rearrange("p (h v) -> p h v", v=256), op=ALU.add),
                             reads=[bC, bpst], writes=[bC])
                    pmn, bpmn = pmm[mmc[0] % 2]
                    mmc[0] += 1
                    for h in range(4):
                        S.op("pe", lambda: T.matmul(pmn[:, h:h + 1], lhsT=kTM[:, i, h * 128:(h + 1) * 128],
                                                    rhs=v_[:, h, 256:257], start=True, stop=True),
                             reads=[bkTM, bv_], writes=[bpmn])
                    S.op("dve", lambda: V.tensor_tensor(out=Cst[:, :, 256:257], in0=Cst[:, :, 256:257],
                                                        in1=pmn[:, 0:4].unsqueeze(2), op=ALU.add),
                         reads=[bC, bpmn], writes=[bC])
                    if full:
                        for h in range(8):
                            S.op("act", lambda: A.activation(out=Sb[:, h, :], in_=Sst[:, h, :], func=AF.Copy,
                                                             scale=eref[:, h, c:c + 1]), reads=[bS, bexpg[h]], writes=[bSb])
                        for half in range(2):
                            for hq in range(4):
                                h = half * 4 + hq
                                S.op("pe", lambda: T.matmul(patt[:, hq * 128:(hq + 1) * 128], lhsT=kd[:, h, tc], rhs=qd[:, h, tc],
                                                            start=True, stop=True), reads=[bkd[h], bqd[h]], writes=[bpatt])
                            S.op("dve", lambda: V.tensor_tensor(out=stm[:].rearrange("p (h t) -> p h t", t=128),
                                                                in0=patt[:, :].rearrange("p (h t) -> p h t", t=128),
                                                                in1=mask1[:].unsqueeze(1).to_broadcast([128, 4, 128]),
                                                                op=ALU.mult), reads=[bpatt, bmask1], writes=[bstm])
                            pm, bpm = po[half]
                            for hq in range(4):
                                h = half * 4 + hq
                                osl = slice(hq * 128, hq * 128 + 128)
                                S.op("pe", lambda: T.matmul(pm[:, osl], lhsT=stm[:, hq * 128:(hq + 1) * 128],
                                                            rhs=iv[:, i, h * 128:(h + 1) * 128], start=True, stop=False),
                                     reads=[bstm, biv[i]], writes=[bpm])
                                S.op("pe", lambda: T.matmul(pm[:, osl], lhsT=qd[:, h, tc], rhs=Sb[:, h, :],
                                                            start=False, stop=True), reads=[bqd[h], bSb], writes=[bpm])
                        f_ = fin
                        for half in range(2):
                            pm, bpm = po[half]
                            S.op("act", lambda: A.activation(out=sqt[:, half * 512:(half + 1) * 512], in_=pm[:, :],
                                                             func=AF.Square), reads=[bpm], writes=[bsqt])
                        S.op("dve", lambda: V.tensor_reduce(out=f_[:, 16:24], in_=sqt[:, :].rearrange("p (h v) -> p h v", v=128),
                                                            axis=AX.X, op=ALU.add), reads=[bsqt], writes=[bfin])
                        S.op("dve", lambda: V.tensor_scalar(out=f_[:, 16:24], in0=f_[:, 16:24], scalar1=1.0 / 128, scalar2=EPS,
                                                            op0=ALU.mult, op1=ALU.add), reads=[bfin], writes=[bfin])
                        S.op("act", lambda: A.activation(out=f_[:, 16:24], in_=f_[:, 16:24], func=AF.Ln),
                             reads=[bfin], writes=[bfin])
                        S.op("act", lambda: A.activation(out=f_[:, 16:24], in_=f_[:, 16:24], func=AF.Exp, scale=-0.5),
                             reads=[bfin], writes=[bfin])
                        for h in range(8):
                            pm, bpm = po[h // 4]
                            osl = slice((h % 4) * 128, (h % 4) * 128 + 128)
                            gsl = slice(1024 + h * 128, 1024 + (h + 1) * 128)
                            S.op("dve", lambda: V.scalar_tensor_tensor(out=GY[:, i, gsl], in0=pm[:, osl],
                                                                       scalar=f_[:, 16 + h:17 + h], in1=GY[:, i, gsl],
                                                                       op0=ALU.mult, op1=ALU.mult),
                                 reads=[bpm, bfin, bGY[i][2 + h // 4]], writes=[bGY[i][2 + h // 4]])
                    for half in range(2):
                        for hq in range(4):
                            h = half * 4 + hq
                            S.op("pe", lambda: T.matmul(pst[:, hq * 128:(hq + 1) * 128], lhsT=kgTM[:, i, h * 128:(h + 1) * 128],
                                                        rhs=iv[:, i, h * 128:(h + 1) * 128], start=True, stop=True),
                                 reads=[bkgTM[h], biv[i]], writes=[bpst])
                        for hq in range(4):
                            h = half * 4 + hq
                            S.op("dve", lambda: V.scalar_tensor_tensor(out=Sst[:, h, :], in0=Sst[:, h, :],
                                                                       scalar=expg[:, h, c:c + 1],
                                                                       in1=pst[:, hq * 128:(hq + 1) * 128],
                                                                       op0=ALU.mult, op1=ALU.add),
                                 reads=[bS, bexpg[h], bpst], writes=[bS])

                def run_pass(full, src_d, ngroups, flagcol=None):
                    def phase_a(g_):
                        ta = g_ * G
                        for i in range(NT):
                            S.dma("act", "ldx%d_%d" % (cur_par, i), XH[:, i, :], src_d[ta + i * 128:ta + (i + 1) * 128, :], writes=[bXHt[i]])
                        for i in range(NT):
                            norm_transpose(XH[:, i, :], bXHt[i], i, nwc, bnwc)

                    for g in range(ngroups):
                        t0 = g * G
                        cur_flag[0] = None if flagcol is None else flagcol(t0)
                        select(g)
                        if g == 0:
                            phase_a(g)
                        S.mark("phaseA")
                        wt, bw = load_w(win_d[:, C_I:C_I + 8], ncols=8, key="gates", rowscale=(nwc, bnwc))
                        pm_i, bpm_i = mm_fm(wt, bw, 0, 4)
                        pm_f, bpm_f = mm_fm(wt, bw, 4, 4)
                        gates_rows(pm_i, bpm_i, pm_f, bpm_f)
                        S.mark("gates")
                        for blk in ((0, 1) if full else (1,)):
                            wt, bw = load_w(win_d[:, blk * 512:(blk + 1) * 512], key="qk%d" % blk, rowscale=(nwc, bnwc))
                            for j in range(4):
                                pm, bpm = mm_fm(wt, bw, j * 128, 128)
                                conv_block(pm, bpm, blk * 4 + j)
                        S.mark("qk")
                        if (not full) and g == ngroups - 1:
                            wt, bw = load_w(win_d[:, 0:512], key="qk0", rowscale=(nwc, bnwc))
                            for j in range(4):
                                pm, bpm = mm_fm(wt, bw, j * 128, 128, ntoks=128, tok0=G - 128)
                                conv_block(pm, bpm, j, ntoks=128, only_halo=True)
                        S.mark("ktm")
                        for cb in range(2):
                            wt, bw = load_w(win_d[:, C_V + cb * 512:C_V + (cb + 1) * 512], key="v%d" % cb, rowscale=(nwc, bnwc))
                            for i in range(NT):
                                pm, bpm = mm_tm(i, wt, bw)
                                S.op("act", lambda: A.copy(out=vaug[:, i, 2 * cb:2 * cb + 2, 0:256],
                                                           in_=pm[:, :].rearrange("p (h v) -> p h v", v=256)),
                                     reads=[bpm], writes=[bvaug[i]])
                        S.mark("v")
                        if full:
                            for cb in range(2):
                                wt, bw = load_w(win_d[:, C_O + cb * 512:C_O + (cb + 1) * 512], key="o%d" % cb, rowscale=(nwc, bnwc))
                                for i in range(NT):
                                    pm, bpm = mm_tm(i, wt, bw)
                                    S.op("act", lambda: A.activation(out=GY[:, i, cb * 512:(cb + 1) * 512], in_=pm[:, :],
                                                                     func=AF.Sigmoid), reads=[bpm], writes=[bGY[i][cb]])
                            for cb in range(2):
                                wt, bw = load_w(win_d[:, C_Z + cb * 512:C_Z + (cb + 1) * 512], key="z%d" % cb, rowscale=(nwc, bnwc))
                                for i in range(NT):
                                    pm, bpm = mm_tm(i, wt, bw)
                                    zt, bzt = ztmp[(cb * NT + i) % 2]
                                    S.op("act", lambda: A.activation(out=zt[:], in_=pm[:, :], func=AF.Silu),
                                         reads=[bpm], writes=[bzt])
                                    S.op("dve", lambda: V.tensor_tensor(out=GY[:, i, cb * 512:(cb + 1) * 512],
                                                                        in0=GY[:, i, cb * 512:(cb + 1) * 512], in1=zt[:],
                                                                        op=ALU.mult), reads=[bzt, bGY[i][cb]], writes=[bGY[i][cb]])
                        S.mark("oz")
                        gates_bcast()
                        for hb in range(2):
                            wt, bw = load_w(win_d[:, C_HF + hb * 512:C_HF + (hb + 1) * 512], key="hf%d" % hb, rowscale=(nwc, bnwc))
                            if full:
                                wtq, bwq = load_w(win_d[:, C_HQ + hb * 512:C_HQ + (hb + 1) * 512], key="hq%d" % hb, rowscale=(nwc, bnwc))
                            for j in range(4):
                                h = hb * 4 + j
                                pm, bpm = mm_fm(wt, bw, j * 128, 128)
                                hf_block(pm, bpm, h, full)
                                if full:
                                    pm, bpm = mm_fm(wtq, bwq, j * 128, 128)
                                    S.op("dve", lambda: V.tensor_tensor(out=qd[:, h, :], in0=pm[:, 0:G], in1=ebf[:, h % 2, :],
                                                                        op=ALU.mult), reads=[bpm, beb[h % 2]], writes=[bqd[h]])
                        S.mark("hgrn")
                        for cb in range(2):
                            wt, bw = load_w(win_d[:, C_HI + cb * 512:C_HI + (cb + 1) * 512], key="hi%d" % cb, rowscale=(nwc, bnwc))
                            for i in range(NT):
                                pm, bpm = mm_tm(i, wt, bw)
                                S.op("act", lambda: A.copy(out=iv[:, i, cb * 512:(cb + 1) * 512], in_=pm[:, :]),
                                     reads=[bpm], writes=[biv[i]])
                        if full:
                            for cb in range(2):
                                wt, bw = load_w(win_d[:, C_HG + cb * 512:C_HG + (cb + 1) * 512], key="hg%d" % cb, rowscale=(nwc, bnwc))
                                for i in range(NT):
                                    pm, bpm = mm_tm(i, wt, bw)
                                    S.op("act", lambda: A.activation(out=GY[:, i, 1024 + cb * 512:1024 + (cb + 1) * 512],
                                                                     in_=pm[:, :], func=AF.Silu),
                                         reads=[bpm], writes=[bGY[i][2 + cb]])
                        S.mark("hi_hg")
                        k_to_tm()
                        for h_ in range(8):
                            kg_to_tm(h_)
                        if g + 1 < ngroups:
                            select(g + 1)
                            phase_a(g + 1)
                            select(g)
                        for c in range(NCH):
                            mixer_chunk(c, full)
                            S.mark("mix%d" % c)
                        S.mark("mixer")
                        if not full:
                            continue
                        for i in range(NT):
                            for hb in range(2):
                                pt, bpt = ptr[tcount[0] % 2]
                                tcount[0] += 1
                                S.ops("pe", [(lambda j=j: T.transpose(out=pt[:, j, :],
                                                                      in_=GY[:, i, (hb * 8 + j) * 128:(hb * 8 + j + 1) * 128],
                                                                      identity=ident[:])) for j in range(8)],
                                      reads=[bGY[i][2 * hb], bGY[i][2 * hb + 1], bident], writes=[bpt])
                                if hb == 0:
                                    S.op("act", lambda: A.copy(out=actT[:, 0:8, i * 128:(i + 1) * 128], in_=pt[:, :, :]),
                                         reads=[bpt], writes=[bactT])
                                else:
                                    S.op("dve", lambda: V.tensor_copy(out=actT[:, 8:16, i * 128:(i + 1) * 128], in_=pt[:, :, :]),
                                         reads=[bpt], writes=[bactT])
                        S.mark("yT")
                        for cb in range(4):
                            wt, bw = load_w(wout_d[:, cb * 512:(cb + 1) * 512], key="wo%d" % cb, rowscale=(mixc, bmixc))
                            for i in range(NT):
                                pm, bpm = mm_tm(i, wt, bw)
                                S.op("dve", lambda: V.tensor_tensor(out=XH[:, i, cb * 512:(cb + 1) * 512],
                                                                    in0=XH[:, i, cb * 512:(cb + 1) * 512], in1=pm[:, :],
                                                                    op=ALU.add), reads=[bpm, bXHt[i]], writes=[bXHt[i]])
                        S.mark("wout")
                        for i in range(NT):
                            norm_transpose(XH[:, i, :], bXHt[i], i, pnc, bpnc)
                        S.mark("hT")
                        for i in range(NT):
                            pl, bpl = ptile[i % 2]
                            pb_, bpb_ = pbt[i % 2]
                            S.dma("act", "ldp%d" % (i % 2), pl[:], p_d[t0 + i * 128:t0 + (i + 1) * 128, :], writes=[bpl])
                            S.op("dve", lambda: V.tensor_copy(out=pb_[:], in_=pl[:]), reads=[bpl], writes=[bpb_])
                            pt, bpt = ptr[tcount[0] % 2]
                            tcount[0] += 1
                            for kk in range(2):
                                S.op("pe", lambda: T.transpose(out=pt[:, kk, :], in_=pb_[:, kk * 128:(kk + 1) * 128],
                                                               identity=ident[:]), reads=[bpb_, bident], writes=[bpt])
                            S.op("act", lambda: A.copy(out=pTt[:, :, i * 128:(i + 1) * 128], in_=pt[:, 0:2, :]),
                                 reads=[bpt], writes=[bpT])
                        S.mark("pT")
                        for cb in range(4):
                            wt, bw = load_w(wpg_d[:, cb * 512:(cb + 1) * 512], key="pg%d" % cb, rowscale=(pnc, bpnc))
                            wt2, bw2 = load_w(wpe_d[:, cb * 512:(cb + 1) * 512], kchunks=2, key="pe%d" % cb)
                            for i in range(NT):
                                pm, bpm = mm_tm(i, wt, bw)
                                g_, bg_ = gt[i % 2]
                                S.op("act", lambda: A.activation(out=g_[:], in_=pm[:, :], func=AF.Sigmoid),
                                     reads=[bpm], writes=[bg_])
                                pm2, bpm2 = mm_tm(i, wt2, bw2, kchunks=2, lhs=pTt, blhs=bpT)
                                S.op("dve", lambda: V.tensor_tensor(out=g_[:], in0=g_[:], in1=pm2[:, :], op=ALU.mult),
                                     reads=[bg_, bpm2], writes=[bg_])
                                S.op("dve", lambda: V.tensor_tensor(out=XH[:, i, cb * 512:(cb + 1) * 512],
                                                                    in0=XH[:, i, cb * 512:(cb + 1) * 512], in1=g_[:],
                                                                    op=ALU.add), reads=[bg_, bXHt[i]], writes=[bXHt[i]])
                        S.mark("gate_e")
                        for i in range(NT):
                            u, bu = ub[i % 2]
                            S.op("act", lambda: A.activation(out=u[:], in_=XH[:, i, :], func=AF.Square,
                                                             accum_out=ssq[:, 4 + i:5 + i]),
                                 reads=[bXHt[i]], writes=[bu, bssq])
                            rstd_from_ssq(4 + i, D)
                            S.op("dve", lambda: V.scalar_tensor_tensor(out=XH[:, i, :], in0=XH[:, i, :],
                                                                       scalar=rstd[:, 4 + i:5 + i], in1=fnw[:],
                                                                       op0=ALU.mult, op1=ALU.mult),
                                 reads=[bXHt[i], brstd, bfnw], writes=[bXHt[i]])
                            S.dma("act", "sto%d_%d" % (cur_par, i), out_d[t0 + i * 128:t0 + (i + 1) * 128, :], XH[:, i, :], reads=[bXHt[i]])

                run_pass(full, src_d, ngroups_, flagcol_)
                S.barrier()

        def zero_state():
            S.op("dve", lambda: V.memset(Cst[:], 0.0), writes=[bC])
            S.op("dve", lambda: V.memset(Sst[:], 0.0), writes=[bS])
            S.op("dve", lambda: V.memset(Sb[:], 0.0), writes=[bSb])
            S.op("dve", lambda: V.memset(mcar[:], 0.0), writes=[bmcar])

        zero_state()
        S.op("dve", lambda: V.memset(Gseg[:], 0.0), writes=[bGseg])
        S.op("dve", lambda: V.memset(gsum[:], 0.0), writes=[bgsum])
        if nprev > 0:
            emit_pass(False, NT_SO, xprev_d, nprev * ntok // (128 * NT_SO), lambda t0: t0 // ntok)
        emit_pass(True, NT, x_d, ntok // (128 * NT))
    build_program.last_ninst = dict(S.ninst)
    return nc


_CFG = dict(ntok=SEG, NT=2, NT_SO=4, nprev=3)


def _flags_for(sgm, nprev):
    f = np.zeros((128, 16), np.float32)
    for j in range(nprev):
        if sgm - nprev + j >= 0:
            f[:, j] = 1.0
    return f


def kernel(**inputs):
    x = np.asarray(inputs["x"], np.float32)
    p = np.asarray(inputs["p"], np.float32)
    B, SQ, _ = x.shape
    nseg = NCORES // B
    seg = SQ // nseg
    cfg = dict(_CFG)
    cfg["ntok"] = seg
    cfg["nprev"] = nseg - 1
    nprev = cfg["nprev"]
    nc = build_program(**cfg)
    shared = {}
    for k in ["norm_w", "w_in", "conv_w", "conv_b", "ml_b_i", "ml_b_f", "ml_norm_w", "hg_norm_w", "w_out",
              "pe_norm_w", "w_pg", "w_pe"]:
        shared[k] = np.ascontiguousarray(np.asarray(inputs[k], np.float32)[0])
    shared["hg_lb"] = np.ascontiguousarray(np.asarray(inputs["hg_lb"], np.float32))
    shared["final_norm_w"] = np.ascontiguousarray(np.asarray(inputs["final_norm_w"], np.float32))
    in_maps = []
    for c in range(NCORES):
        b, sgm = c // nseg, c % nseg
        m = dict(shared)
        m["x"] = np.ascontiguousarray(x[b, sgm * seg:(sgm + 1) * seg])
        xp = np.zeros((nprev * seg, D), np.float32)
        if sgm > 0:
            xp[(nprev - sgm) * seg:] = x[b, 0:sgm * seg]
        m["xprev"] = xp
        m["p"] = np.ascontiguousarray(p[0, b, sgm * seg:(sgm + 1) * seg])
        m["flags"] = _flags_for(sgm, nprev)
        in_maps.append(m)
    res = run_bass_kernel_spmd(nc, in_maps, core_ids=list(range(NCORES)))
    out = np.empty((B, SQ, D), np.float32)
    for c in range(NCORES):
        b, sgm = c // nseg, c % nseg
        out[b, sgm * seg:(sgm + 1) * seg] = res.results[c]["out"]
    return out
```

```python
import numpy as np
from contextlib import ExitStack
import concourse.bass as bass
import concourse.mybir as mybir
from concourse.bass_utils import run_bass_kernel_spmd

F32 = mybir.dt.float32
BF16 = mybir.dt.bfloat16
AF = mybir.ActivationFunctionType
ALU = mybir.AluOpType
AX = mybir.AxisListType

D = 2048
INC = 8200
NCORES = 8
SEG = 4096
EPS = 1e-6
QSCALE = 128 ** -0.5
CC_INC = 16

C_Q, C_K, C_V, C_O, C_Z, C_I, C_F, C_HQ, C_HF, C_HI, C_HG = (
    0, 512, 1024, 2048, 3072, 4096, 4100, 4104, 5128, 6152, 7176)


class Buf:
    __slots__ = ("name", "w", "r")

    def __init__(self, name):
        self.name = name
        self.w = None
        self.r = {}


class Sched:
    def __init__(self, nc, stack, same_engine_sync=True):
        self.nc = nc
        self.stack = stack
        self.eng = {"pe": nc.tensor, "act": nc.scalar, "dve": nc.vector,
                    "pool": nc.gpsimd, "sp": nc.sync}
        self.sems = {}
        self.cnt = {}
        self.seen = {e: {} for e in self.eng}
        self.same = same_engine_sync
        for e in self.eng:
            self.newsem(e)
        self.ninst = {e: 0 for e in self.eng}
        self.stopped = False
        self.stop_at = None

    def mark(self, label):
        if self.stop_at is not None and label == self.stop_at:
            self.stopped = True

    def newsem(self, key):
        self.sems[key] = self.stack.enter_context(self.nc.semaphore("s_" + key))
        self.cnt[key] = 0
        return key

    def _deps(self, reads, writes):
        deps = {}

        def add(k, v):
            if deps.get(k, 0) < v:
                deps[k] = v
        for b in reads:
            if b.w is not None:
                add(*b.w)
        for b in writes:
            if b.w is not None:
                add(*b.w)
            for k, v in b.r.items():
                add(k, v)
        return deps

    def _wait(self, e, deps):
        seen = self.seen[e]
        for k, v in deps.items():
            if k == e and (not self.same or e == "pe"):
                continue
            if seen.get(k, 0) >= v:
                continue
            self.eng[e].wait_ge(self.sems[k], v)
            seen[k] = v

    def op(self, e, fn, reads=(), writes=()):
        if self.stopped:
            return None
        self._wait(e, self._deps(reads, writes))
        ins = fn()
        self.cnt[e] += 1
        v = self.cnt[e]
        ins.then_inc(self.sems[e], 1)
        self.ninst[e] += 1
        for b in reads:
            if b.r.get(e, 0) < v:
                b.r[e] = v
        for b in writes:
            b.w = (e, v)
            b.r = {}
        return ins

    def ops(self, e, fns, reads=(), writes=()):
        if self.stopped:
            return None
        self._wait(e, self._deps(reads, writes))
        ins = None
        for fn in fns:
            ins = fn()
        self.cnt[e] += 1
        v = self.cnt[e]
        ins.then_inc(self.sems[e], 1)
        self.ninst[e] += len(fns)
        for b in reads:
            if b.r.get(e, 0) < v:
                b.r[e] = v
        for b in writes:
            b.w = (e, v)
            b.r = {}
        return ins

    def dma(self, q, semkey, out, in_, reads=(), writes=(), **kw):
        if self.stopped:
            return None
        self._wait(q, self._deps(reads, writes))
        ins = self.eng[q].dma_start(out=out, in_=in_, **kw)
        self.cnt[semkey] += 16
        v = self.cnt[semkey]
        ins.then_inc(self.sems[semkey], 16)
        self.ninst[q] += 1
        for b in reads:
            if b.r.get(semkey, 0) < v:
                b.r[semkey] = v
        for b in writes:
            b.w = (semkey, v)
            b.r = {}
        return ins

    def barrier(self):
        if self.stopped:
            return
        deps = {k: v for k, v in self.cnt.items() if v > 0}
        for e in self.eng:
            self._wait(e, deps)

    def finish(self, e, bufs):
        deps = {}
        for b in bufs:
            toks = list(b.r.items())
            if b.w is not None:
                toks.append(b.w)
            for k, v in toks:
                if deps.get(k, 0) < v:
                    deps[k] = v
        self._wait(e, deps)


def build_program(ntok=SEG, NT=2, NT_SO=4, nprev=3, debug=None, stop_at=None, use_wsc=True):
    assert ntok % (128 * NT) == 0 and (nprev * ntok) % (128 * NT_SO) == 0
    nc = bass.Bass("TRN2", target_bir_lowering=False)
    dram_in = lambda n, s: nc.dram_tensor(n, s, F32, kind="ExternalInput").ap()
    x_d = dram_in("x", [ntok, D])
    xprev_d = dram_in("xprev", [max(nprev, 1) * ntok, D])
    p_d = dram_in("p", [ntok, 256])
    normw_d = dram_in("norm_w", [D])
    win_d = dram_in("w_in", [D, INC])
    convw_d = dram_in("conv_w", [4, 1024])
    convb_d = dram_in("conv_b", [1024])
    bi_d = dram_in("ml_b_i", [4])
    bf_d = dram_in("ml_b_f", [4])
    mlnw_d = dram_in("ml_norm_w", [1024])
    hglb_d = dram_in("hg_lb", [2, 1024])
    hgnw_d = dram_in("hg_norm_w", [1024])
    wout_d = dram_in("w_out", [D, D])
    penw_d = dram_in("pe_norm_w", [D])
    wpg_d = dram_in("w_pg", [D, D])
    wpe_d = dram_in("w_pe", [256, D])
    fnw_d = dram_in("final_norm_w", [D])
    flags_d = dram_in("flags", [128, 16])
    out_d = nc.dram_tensor("out", [ntok, D], F32, kind="ExternalOutput").ap()
    dbg_d = {}
    if debug:
        for n, s in debug.items():
            dbg_d[n] = nc.dram_tensor("dbg_" + n, list(s), F32, kind="ExternalOutput").ap()

    SW = 8
    with ExitStack() as st:
        S = Sched(nc, st)
        S.stop_at = stop_at

        def sb(name, shape, dt):
            t = st.enter_context(nc.sbuf_tensor(name, list(shape), dt))
            return t, Buf(name)

        def ps(name, shape, dt):
            t = st.enter_context(nc.psum_tensor(name, list(shape), dt))
            return t, Buf(name)

        V, A, P_, T = nc.vector, nc.scalar, nc.gpsimd, nc.tensor

        NW = 3
        Wb = [sb("W%d" % i, [128, 16, 512], BF16) for i in range(NW)]
        ub = [sb("ub%d" % i, [128, D], BF16) for i in range(2)]
        ptr = [ps("ptr%d" % i, [128, 8, 128], BF16) for i in range(2)]
        pmm = [ps("pmm%d" % i, [128, 512], F32) for i in range(2)]
        pst, bpst = ps("pst", [128, 512], F32)
        patt, bpatt = ps("patt", [128, 512], F32)
        po = [ps("po%d" % i, [128, 512], F32) for i in range(2)]
        ident, bident = sb("ident", [128, 128], BF16)
        identf, bidentf = sb("identf", [128, 128], F32)
        maskc, bmaskc = sb("maskc", [128, 128], F32)
        mask1, bmask1 = sb("mask1", [128, 128], F32)
        nwc, bnwc = sb("nwc", [128, 16], F32)
        pnc, bpnc = sb("pnc", [128, 16], F32)
        mixc, bmixc = sb("mixc", [128, 16], F32)
        cw, bcw = sb("cw", [128, 8, 4], F32)
        cbias, bcbias = sb("cbias", [128, 8], F32)
        lbt, blbt = sb("lbt", [128, 2, 8], F32)
        lb, blb = sb("lb", [128, 8], F32)
        oml, boml = sb("oml", [128, 8], F32)
        noml, bnoml = sb("noml", [128, 8], F32)
        gbias, bgbias = sb("gbias", [4, 3], F32)
        fnw, bfnw = sb("fnw", [128, D], F32)
        ones4, bones4 = sb("ones4", [4, 128], F32)
        flg, bflg = sb("flg", [128, 16], F32)
        halo, bhalo = sb("halo", [128, 8, 3], F32)
        mcar, bmcar = sb("mcar", [4, 1], F32)
        gsum, bgsum = sb("gsum", [4, 1], F32)
        Cst, bC = sb("Cst", [128, 4, 258], F32)
        Cb, bCb = sb("Cb", [128, 4, 258], BF16)
        Sst, bS = sb("Sst", [128, 8, 128], F32)
        Sb, bSb = sb("Sb", [128, 8, 128], BF16)
        Gseg, bGseg = sb("Gseg", [128, 8], F32)
        ssq, bssq = sb("ssq", [128, 8], F32)
        rstd, brstd = sb("rstd", [128, 8], F32)
        fin, bfin = sb("fin", [128, 64], F32)
        NTMAX = max(NT, NT_SO)
        for k in (["ldx%d_%d" % (q_, i) for q_ in range(2) for i in range(NTMAX)] + ["ldw%d" % i for i in range(NW)] +
                  ["ldc", "ldp0", "ldp1", "dbg"] + ["wbk%d" % i for i in range(NW)] +
                  ["ldwh%d" % i for i in range(NW)] + ["sto%d_%d" % (q_, i) for q_ in range(2) for i in range(NTMAX)]):
            S.newsem(k)

        def dbg(name, ap, bufs):
            if name in dbg_d:
                S.dma("sp", "dbg", dbg_d[name], ap, reads=bufs)

        def cdma(out, in_, wb, **kw):
            S.dma("sp", "ldc", out, in_, writes=[wb], **kw)
        cdma(nwc[:], normw_d.rearrange("(k p) -> p k", p=128), bnwc, allow_slow_non_contiguous=True)
        cdma(pnc[:], penw_d.rearrange("(k p) -> p k", p=128), bpnc, allow_slow_non_contiguous=True)
        cdma(mixc[:, 0:8], mlnw_d.rearrange("(k p) -> p k", p=128), bmixc, allow_slow_non_contiguous=True)
        cdma(mixc[:, 8:16], hgnw_d.rearrange("(k p) -> p k", p=128), bmixc, allow_slow_non_contiguous=True)
        for kk in range(4):
            cdma(cw[:, :, kk], convw_d[kk].rearrange("(c p) -> p c", p=128), bcw, allow_slow_non_contiguous=True)
        cdma(cbias[:], convb_d.rearrange("(c p) -> p c", p=128), bcbias, allow_slow_non_contiguous=True)
        for r_ in range(2):
            cdma(lbt[:, r_, :], hglb_d[r_].rearrange("(h p) -> p h", p=128), blbt, allow_slow_non_contiguous=True)
        cdma(gbias[:, 0:1], bi_d.rearrange("(h o) -> h o", o=1), bgbias, allow_slow_non_contiguous=True)
        cdma(gbias[:, 1:2], bf_d.rearrange("(h o) -> h o", o=1), bgbias, allow_slow_non_contiguous=True)
        cdma(fnw[:], fnw_d.partition_broadcast(128), bfnw)
        cdma(flg[:], flags_d, bflg)
        for b_ in (bnwc, bpnc, bmixc, bcw, bcbias, blbt, bgbias, bfnw, bflg):
            b_.w = ("ldc", S.cnt["ldc"])

        S.op("pool", lambda: P_.memset(identf[:], 0.0), writes=[bidentf])
        S.op("pool", lambda: P_.affine_select(out=identf[:], in_=identf[:], pattern=[[-1, 128]],
                                              compare_op=ALU.not_equal, fill=1.0, base=0, channel_multiplier=1),
             reads=[bidentf], writes=[bidentf])
        S.op("dve", lambda: V.tensor_copy(out=ident[:], in_=identf[:]), reads=[bidentf], writes=[bident])
        for (mk, bm, val) in ((maskc, bmaskc, QSCALE), (mask1, bmask1, 1.0)):
            S.op("pool", lambda: P_.memset(mk[:], val), writes=[bm])
            S.op("pool", lambda: P_.affine_select(out=mk[:], in_=mk[:], pattern=[[1, 128]],
                                                  compare_op=ALU.is_ge, fill=0.0, base=0, channel_multiplier=-1),
                 reads=[bm], writes=[bm])
        S.op("pool", lambda: P_.memset(ones4[:], 1.0), writes=[bones4])
        S.op("dve", lambda: V.tensor_scalar(out=gbias[:, 2:3], in0=gbias[:, 1:2], scalar1=-1.0, scalar2=None, op0=ALU.mult),
             reads=[bgbias], writes=[bgbias])
        S.op("dve", lambda: V.tensor_sub(out=lb[:], in0=lbt[:, 0, :], in1=lbt[:, 1, :]), reads=[blbt], writes=[blb])
        S.op("act", lambda: A.activation(out=oml[:], in_=lb[:], func=AF.Sigmoid, scale=-1.0), reads=[blb], writes=[boml])
        S.op("act", lambda: A.activation(out=lb[:], in_=lb[:], func=AF.Sigmoid), reads=[blb], writes=[blb])
        S.op("dve", lambda: V.tensor_scalar(out=noml[:], in0=oml[:], scalar1=-1.0, scalar2=None, op0=ALU.mult),
             reads=[boml], writes=[bnoml])

        S.mark("consts")
        wslot = [0]

        wsc = {}

        def load_w(src_ap, kchunks=16, ncols=512, key=None, rowscale=None):
            s = wslot[0] % NW
            wslot[0] += 1
            wt, bw = Wb[s]
            if key is not None and key in wsc:
                sc_ap, bsc = wsc[key]
                S.dma("sp", "ldwh%d" % s, wt[:, 0:kchunks, 0:ncols],
                      sc_ap.rearrange("p (k c) -> p k c", c=ncols), reads=[bsc], writes=[bw])
                return wt, bw
            v = src_ap.rearrange("(k p) c -> p k c", p=128)
            step = 4 if kchunks >= 4 else kchunks
            for k0 in range(0, kchunks, step):
                S.dma("pool", "ldw%d" % s, wt[:, k0:k0 + step, 0:ncols], v[:, k0:k0 + step, :], writes=[bw])
            if rowscale is not None:
                colw_, bcolw_ = rowscale
                for k in range(kchunks):
                    if k % 2 == 0:
                        S.op("dve", lambda: V.tensor_scalar(out=wt[:, k, 0:ncols], in0=wt[:, k, 0:ncols],
                                                            scalar1=colw_[:, k:k + 1], scalar2=None, op0=ALU.mult),
                             reads=[bw, bcolw_], writes=[bw])
                    else:
                        S.op("act", lambda: A.activation(out=wt[:, k, 0:ncols], in_=wt[:, k, 0:ncols], func=AF.Copy,
                                                         scale=colw_[:, k:k + 1]), reads=[bw, bcolw_], writes=[bw])
            if key is not None and use_wsc:
                sc_ap = nc.dram_tensor("wsc_" + key, [128, kchunks * ncols], BF16).ap()
                bsc = Buf("wsc_" + key)
                S.dma("sp", "wbk%d" % s, sc_ap.rearrange("p (k c) -> p k c", c=ncols), wt[:, 0:kchunks, 0:ncols],
                      reads=[bw], writes=[bsc])
                wsc[key] = (sc_ap, bsc)
            return wt, bw

        def rstd_from_ssq(col, n, width=1):
            sl = slice(col, col + width)
            S.op("dve", lambda: V.tensor_scalar(out=rstd[:, sl], in0=ssq[:, sl], scalar1=1.0 / n,
                                                scalar2=EPS, op0=ALU.mult, op1=ALU.add), reads=[bssq], writes=[brstd])
            S.op("act", lambda: A.activation(out=rstd[:, sl], in_=rstd[:, sl], func=AF.Ln), reads=[brstd], writes=[brstd])
            S.op("act", lambda: A.activation(out=rstd[:, sl], in_=rstd[:, sl], func=AF.Exp, scale=-0.5),
                 reads=[brstd], writes=[brstd])

        S.op("dve", lambda: V.memset(halo[:], 0.0), writes=[bhalo])

        def emit_pass(full, NT, src_d, ngroups_, flagcol_=None):
            G = 128 * NT
            NCH = NT
            QOFF = 0 if full else 4
            with ExitStack() as st2:
                def sb(name, shape, dt):
                    t = st2.enter_context(nc.sbuf_tensor(name + ("_f" if full else "_s"), list(shape), dt))
                    return t, Buf(name)

                NPAR = 2 if full else 1
                XH_l = [sb("XH%d" % q_, [128, NT, D], F32) for q_ in range(NPAR)]
                bXHt_l = [[Buf("XH%d_%d" % (q_, i)) for i in range(NT)] for q_ in range(NPAR)]
                actT_l = [sb("actT%d" % q_, [128, 16, G], BF16) for q_ in range(NPAR)]
                XH, bXHt = XH_l[0][0], bXHt_l[0]
                actT, bactT = actT_l[0]

                cur_par = 0

                def select(par):
                    nonlocal XH, bXHt, actT, bactT, cur_par
                    par = par % NPAR
                    cur_par = par
                    XH, bXHt = XH_l[par][0], bXHt_l[par]
                    actT, bactT = actT_l[par]
                mreset, bmreset = sb("mreset", [128, G], F32)
                dmask, bdmask = sb("dmask", [4, 4, NCH], F32)
                cpre = [sb("cpre%d" % i, [128, G + 3], F32) for i in range(2)]
                cacc = [sb("cacc%d" % i, [128, G], F32) for i in range(2)]
                qkT, bqkT = sb("qkT", [128, 8 if full else 4, G], BF16)
                bqk = [Buf("qk%d" % i) for i in range(8)]
                kd, bkd_ = sb("kd", [128, 8, G], BF16)
                bkd = [Buf("kd%d" % i) for i in range(8)]
                bqd = [Buf("qd%d" % i) for i in range(8)]
                beb = [Buf("eb%d" % i) for i in range(2)]
                if full:
                    qd, bqd_ = sb("qd", [128, 8, G], BF16)
                    ebf, bebf_ = sb("ebf", [128, 2, G], F32)
                expg, bexpg_ = sb("expg", [128, 8, NCH], F32)
                eref, beref_ = sb("eref", [128, 8, NCH], F32)
                egr, begr_ = sb("egr", [128, 8, NCH], F32)
                hsm, bhsm_ = sb("hsm", [128, 8, 4, NCH], F32)
                bhsm = [Buf("hsm%d" % i) for i in range(8)]
                bexpg = [Buf("expg%d" % i) for i in range(8)]
                hft = [sb("hft%d_%d" % (i, j), [128, G], F32) for i in range(2) for j in range(3)]
                kgt = [sb("kgt%d" % i, [128, G], BF16) for i in range(8)]
                vaug, bvaug_ = sb("vaug", [128, NT, 4, 258], BF16)
                bvaug = [Buf("vaug%d" % i) for i in range(NT)]
                bGY = [[Buf("GY%d_%d" % (i, j)) for j in range(4)] for i in range(NT)]
                if full:
                    GY, bGY_ = sb("GY", [128, NT, D], BF16)
                iv, biv_ = sb("iv", [128, NT, 1024], BF16)
                biv = [Buf("iv%d" % i) for i in range(NT)]
                kTM, bkTM_ = sb("kTM", [128, NT, 512], BF16)
                bkTM = Buf("kTM")
                kgTM, bkgTM_ = sb("kgTM", [128, NT, 1024], BF16)
                bkgTM = [Buf("kgTM%d" % i) for i in range(8)]
                vs = [sb("vs%d" % i, [128, 4, 258], BF16) for i in range(2)]
                (gi_r, bgi_r), (gf_r, bgf_r), (gb_r, bgb_r), (ga_r, bga_r), (gea_r, bgea_r), (gfl_r, bgfl_r) = [
                    (t[0:4, :], b) for (t, b) in hft]
                gsm, bgsm = sb("gsm", [4, 8, NCH], F32)
                scR, bscR = sb("scR", [4, 4, NCH], F32)
                scb, bscb = sb("scb", [128, 4, NCH], F32)
                eaT, beaT = sb("eaT", [128, NT, 4], F32)
                flT, bflT = sb("flT", [128, NT, 4], F32)
                if full:
                    ztmp = [sb("ztmp%d" % i, [128, 512], BF16) for i in range(2)]
                    stm, bstm = sb("stm", [128, 512], BF16)
                    sqt, bsqt = sb("sqt", [128, 1024], F32)
                    pTt, bpT = sb("pT", [128, 2, G], BF16)
                    ptile = [sb("ptile%d" % i, [128, 256], F32) for i in range(2)]
                    pbt = [sb("pbt%d" % i, [128, 256], BF16) for i in range(2)]
                    gt = [sb("gt%d" % i, [128, 512], F32) for i in range(2)]
                S.op("pool", lambda: P_.memset(mreset[:], 1.0), writes=[bmreset])
                S.op("pool", lambda: P_.memset(mreset[:].rearrange("p (c l) -> p c l", l=128)[:, :, 0:1], 0.0),
                     writes=[bmreset])
                S.op("pool", lambda: P_.memset(dmask[:], 1.0), writes=[bdmask])
                S.op("pool", lambda: P_.affine_select(out=dmask[:], in_=dmask[:], pattern=[[1, 4], [0, NCH]],
                                                      compare_op=ALU.is_equal, fill=0.0, base=0, channel_multiplier=-1),
                     reads=[bdmask], writes=[bdmask])
                S.op("pool", lambda: P_.memset(vaug[:], 1.0), writes=bvaug)

                tcount = [0]

                def norm_transpose(src_ap, src_buf, i, colw, bcolw):
                    u, bu = ub[tcount[0] % 2]
                    S.op("act", lambda: A.activation(out=u[:], in_=src_ap, func=AF.Square, accum_out=ssq[:, i:i + 1]),
                         reads=[src_buf], writes=[bu, bssq])
                    rstd_from_ssq(i, D)
                    S.op("dve", lambda: V.tensor_scalar(out=u[:], in0=src_ap, scalar1=rstd[:, i:i + 1], scalar2=None,
                                                        op0=ALU.mult), reads=[src_buf, brstd], writes=[bu])
                    transpose16(u, bu, i, colw, bcolw)

                def transpose16(u, bu, i, colw, bcolw):
                    for hb in range(2):
                        pt, bpt = ptr[tcount[0] % 2]
                        tcount[0] += 1
                        S.ops("pe", [(lambda j=j: T.transpose(out=pt[:, j, :], in_=u[:, (hb * 8 + j) * 128:(hb * 8 + j + 1) * 128],
                                                              identity=ident[:])) for j in range(8)],
                              reads=[bu, bident], writes=[bpt])
                        if hb == 0:
                            S.op("act", lambda: A.copy(out=actT[:, 0:8, i * 128:(i + 1) * 128], in_=pt[:, :, :]),
                                 reads=[bpt], writes=[bactT])
                        else:
                            S.op("dve", lambda: V.tensor_copy(out=actT[:, 8:16, i * 128:(i + 1) * 128], in_=pt[:, :, :]),
                                 reads=[bpt], writes=[bactT])

                mmc = [0]
                pj = [0]
                pbanks = pmm + po + [(patt, bpatt)]

                def mm_tm(i, wt, bw, kchunks=16, lhs=None, blhs=None, ncols=512):
                    pm, bpm = pbanks[pj[0] % len(pbanks)]
                    pj[0] += 1
                    lhs = actT if lhs is None else lhs
                    blhs = bactT if blhs is None else blhs
                    S.ops("pe", [(lambda k=k: T.matmul(pm[:, 0:ncols], lhsT=lhs[:, k, i * 128:(i + 1) * 128],
                                                       rhs=wt[:, k, 0:ncols], start=(k == 0), stop=(k == kchunks - 1)))
                                 for k in range(kchunks)], reads=[blhs, bw], writes=[bpm])
                    return pm, bpm

                def mm_fm(wt, bw, c0, m, ntoks=G, tok0=0):
                    pm, bpm = pbanks[pj[0] % len(pbanks)]
                    pj[0] += 1
                    S.ops("pe", [(lambda k=k: T.matmul(pm[0:m, 0:ntoks], lhsT=wt[:, k, c0:c0 + m],
                                                       rhs=actT[:, k, tok0:tok0 + ntoks], start=(k == 0), stop=(k == 15)))
                                 for k in range(16)], reads=[bactT, bw], writes=[bpm])
                    return pm, bpm

                cvc = [0]

                def conv_block(pm, bpm, c8, ntoks=G, only_halo=False):
                    cp, bcp = cpre[cvc[0] % 2]
                    ca, bca = cacc[cvc[0] % 2]
                    cvc[0] += 1
                    S.op("dve", lambda: V.tensor_copy(out=cp[:, 0:3], in_=halo[:, c8, :]), reads=[bhalo], writes=[bcp])
                    S.op("act", lambda: A.copy(out=cp[:, 3:3 + ntoks], in_=pm[:, 0:ntoks]), reads=[bpm], writes=[bcp])
                    S.op("dve", lambda: V.tensor_copy(out=halo[:, c8, :], in_=cp[:, ntoks:ntoks + 3]), reads=[bcp], writes=[bhalo])
                    if only_halo:
                        return
                    S.op("dve", lambda: V.tensor_scalar(out=ca[:, 0:ntoks], in0=cp[:, 0:ntoks], scalar1=cw[:, c8, 0:1],
                                                        scalar2=cbias[:, c8:c8 + 1], op0=ALU.mult, op1=ALU.add),
                         reads=[bcp, bcw, bcbias], writes=[bca])
                    for kk in range(1, 4):
                        S.op("dve", lambda: V.scalar_tensor_tensor(out=ca[:, 0:ntoks], in0=cp[:, kk:kk + ntoks],
                                                                   scalar=cw[:, c8, kk:kk + 1], in1=ca[:, 0:ntoks],
                                                                   op0=ALU.mult, op1=ALU.add),
                             reads=[bcp, bcw, bca], writes=[bca])
                    S.op("act", lambda: A.activation(out=qkT[:, c8 - QOFF, 0:ntoks], in_=ca[:, 0:ntoks], func=AF.Silu),
                         reads=[bca], writes=[bqk[c8]])

                def k_to_tm():
                    for h in range(4):
                        pt, bpt = ptr[tcount[0] % 2]
                        tcount[0] += 1
                        S.ops("pe", [(lambda i=i: T.transpose(out=pt[:, i, :], in_=qkT[:, 4 + h - QOFF, i * 128:(i + 1) * 128],
                                                              identity=ident[:])) for i in range(NT)],
                              reads=[bqk[4 + h], bident], writes=[bpt])
                        S.op("act", lambda: A.copy(out=kTM[:, :, h * 128:(h + 1) * 128], in_=pt[:, 0:NT, :]),
                             reads=[bpt], writes=[bkTM])

                hfc = [0]
                E38 = float(np.exp(38.0))

                def hf_block(pm, bpm, h, full):
                    s = hfc[0] % 2
                    hfc[0] += 1
                    (t0, b0), (t1, b1), (t2, b2) = hft[s * 3], hft[s * 3 + 1], hft[s * 3 + 2]
                    kg, bkg = kgt[h]
                    hl = h % 2
                    S.op("act", lambda: A.activation(out=t0[:], in_=pm[:, 0:G], func=AF.Exp, scale=-1.0), reads=[bpm], writes=[b0])
                    S.op("act", lambda: A.activation(out=t0[:], in_=t0[:], func=AF.Ln, bias=1.0), reads=[b0], writes=[b0])
                    S.op("act", lambda: A.activation(out=t0[:], in_=t0[:], func=AF.Exp, scale=-1.0), reads=[b0], writes=[b0])
                    S.op("act", lambda: A.activation(out=t1[:], in_=t0[:], func=AF.Ln, scale=oml[:, h:h + 1], bias=lb[:, h:h + 1]),
                         reads=[b0, boml, blb], writes=[b1])
                    S.op("act", lambda: A.activation(out=t0[:], in_=t0[:], func=AF.Identity, scale=noml[:, h:h + 1],
                                                     bias=oml[:, h:h + 1]), reads=[b0, bnoml, boml], writes=[b0])
                    S.op("dve", lambda: V.tensor_tensor_scan(out=t2[:], data0=mreset[:], data1=t1[:], initial=0.0,
                                                             op0=ALU.mult, op1=ALU.add), reads=[bmreset, b1], writes=[b2])
                    b3 = t2[:].rearrange("p (c l) -> p c l", l=128)
                    S.op("dve", lambda: V.tensor_copy(out=hsm[:, h, 0, :], in_=b3[:, :, 63]), reads=[b2], writes=[bhsm[h]])
                    S.op("dve", lambda: V.tensor_scalar(out=hsm[:, h, 1, :], in0=b3[:, :, 63], scalar1=-1.0, scalar2=None,
                                                        op0=ALU.mult), reads=[b2], writes=[bhsm[h]])
                    S.op("dve", lambda: V.tensor_sub(out=hsm[:, h, 2, :], in0=b3[:, :, 127], in1=b3[:, :, 63]),
                         reads=[b2], writes=[bhsm[h]])
                    S.op("act", lambda: A.activation(out=expg[:, h, :], in_=b3[:, :, 127], func=AF.Exp),
                         reads=[b2], writes=[bexpg[h]])
                    S.op("act", lambda: A.activation(out=eref[:, h, :], in_=hsm[:, h, 0, :], func=AF.Exp),
                         reads=[bhsm[h]], writes=[bexpg[h]])
                    S.op("act", lambda: A.activation(out=egr[:, h, :], in_=hsm[:, h, 2, :], func=AF.Exp),
                         reads=[bhsm[h]], writes=[bexpg[h]])
                    for c in range(NCH):
                        cs = slice(c * 128, (c + 1) * 128)
                        S.op("act", lambda: A.activation(out=t1[:, cs], in_=t2[:, cs], func=AF.Exp, scale=-1.0,
                                                         bias=hsm[:, h, 0, c:c + 1]), reads=[b2, bhsm[h]], writes=[b1])
                        if full:
                            S.op("act", lambda: A.activation(out=ebf[:, hl, cs], in_=t2[:, cs], func=AF.Exp,
                                                             bias=hsm[:, h, 1, c:c + 1]), reads=[b2, bhsm[h]], writes=[beb[hl]])
                    S.op("dve", lambda: V.scalar_tensor_tensor(out=kd[:, h, :], in0=t1[:], scalar=E38, in1=t0[:],
                                                               op0=ALU.min, op1=ALU.mult), reads=[b0, b1], writes=[bkd[h]])
                    S.op("dve", lambda: V.tensor_tensor(out=kg[:].rearrange("p (c l) -> p c l", l=128),
                                                        in0=kd[:, h, :].rearrange("p (c l) -> p c l", l=128),
                                                        in1=egr[:, h, :].unsqueeze(2).to_broadcast([128, NCH, 128]),
                                                        op=ALU.mult), reads=[bkd[h], bexpg[h]], writes=[bkg])

                def kg_to_tm(h):
                    kg, bkg = kgt[h]
                    pt, bpt = ptr[tcount[0] % 2]
                    tcount[0] += 1
                    S.ops("pe", [(lambda i=i: T.transpose(out=pt[:, i, :], in_=kg[:, i * 128:(i + 1) * 128], identity=ident[:]))
                                 for i in range(NT)], reads=[bkg, bident], writes=[bpt])
                    S.op("act", lambda: A.copy(out=kgTM[:, :, h * 128:(h + 1) * 128], in_=pt[:, 0:NT, :]),
                         reads=[bpt], writes=[bkgTM[h]])

                def gates_rows(pm_i, bpm_i, pm_f, bpm_f):
                    S.op("act", lambda: A.activation(out=gi_r, in_=pm_i[0:4, 0:G], func=AF.Identity, bias=gbias[:, 0:1]),
                         reads=[bpm_i, bgbias], writes=[bgi_r])
                    S.op("act", lambda: A.activation(out=gf_r, in_=pm_f[0:4, 0:G], func=AF.Exp, scale=-1.0, bias=gbias[:, 2:3]),
                         reads=[bpm_f, bgbias], writes=[bgf_r])
                    S.op("act", lambda: A.activation(out=gf_r, in_=gf_r, func=AF.Ln, bias=1.0), reads=[bgf_r], writes=[bgf_r])
                    S.op("dve", lambda: V.tensor_scalar(out=gf_r, in0=gf_r, scalar1=-1.0, scalar2=None, op0=ALU.mult),
                         reads=[bgf_r], writes=[bgf_r])
                    S.op("dve", lambda: V.tensor_tensor_scan(out=gb_r, data0=mreset[0:4, :], data1=gf_r, initial=0.0,
                                                             op0=ALU.mult, op1=ALU.add), reads=[bmreset, bgf_r], writes=[bgb_r])
                    S.op("dve", lambda: V.tensor_sub(out=ga_r, in0=gi_r, in1=gb_r), reads=[bgi_r, bgb_r], writes=[bga_r])
                    a3 = ga_r.rearrange("p (c l) -> p c l", l=128)
                    b3 = gb_r.rearrange("p (c l) -> p c l", l=128)
                    AMAX, GSH, MM, GM, MPREV, SC = range(6)
                    S.op("dve", lambda: V.tensor_reduce(out=gsm[:, AMAX, :], in_=a3, axis=AX.X, op=ALU.max),
                         reads=[bga_r], writes=[bgsm])
                    S.op("dve", lambda: V.memset(gsm[:, GSH, 0:1], 0.0), writes=[bgsm])
                    if NCH > 1:
                        S.op("dve", lambda: V.tensor_copy(out=gsm[:, GSH, 1:NCH], in_=b3[:, 0:NCH - 1, 127]),
                             reads=[bgb_r], writes=[bgsm])
                    S.op("dve", lambda: V.tensor_tensor_scan(out=gsm[:, MM, :], data0=gsm[:, GSH, :], data1=gsm[:, AMAX, :],
                                                             initial=mcar[:, 0:1], op0=ALU.add, op1=ALU.max),
                         reads=[bgsm, bmcar], writes=[bgsm])
                    S.op("dve", lambda: V.tensor_add(out=gsm[:, GM, :], in0=gsm[:, MM, :], in1=b3[:, :, 127]),
                         reads=[bgsm, bgb_r], writes=[bgsm])
                    S.op("dve", lambda: V.tensor_copy(out=gsm[:, MPREV, 0:1], in_=mcar[:, 0:1]), reads=[bmcar], writes=[bgsm])
                    if NCH > 1:
                        S.op("dve", lambda: V.tensor_copy(out=gsm[:, MPREV, 1:NCH], in_=gsm[:, GM, 0:NCH - 1]),
                             reads=[bgsm], writes=[bgsm])
                    S.op("dve", lambda: V.tensor_copy(out=mcar[:, 0:1], in_=gsm[:, GM, NCH - 1:NCH]), reads=[bgsm], writes=[bmcar])
                    S.op("dve", lambda: V.tensor_sub(out=gsm[:, SC, :], in0=gsm[:, MPREV, :], in1=gsm[:, MM, :]),
                         reads=[bgsm], writes=[bgsm])
                    S.op("act", lambda: A.activation(out=gsm[:, SC, :], in_=gsm[:, SC, :], func=AF.Exp), reads=[bgsm], writes=[bgsm])
                    Mb = gsm[:, MM, :].unsqueeze(2).to_broadcast([4, NCH, 128])
                    S.op("dve", lambda: V.tensor_tensor(out=gea_r.rearrange("p (c l) -> p c l", l=128), in0=a3, in1=Mb,
                                                        op=ALU.subtract), reads=[bga_r, bgsm], writes=[bgea_r])
                    S.op("act", lambda: A.activation(out=gea_r, in_=gea_r, func=AF.Exp), reads=[bgea_r], writes=[bgea_r])
                    S.op("dve", lambda: V.tensor_tensor(out=gfl_r.rearrange("p (c l) -> p c l", l=128), in0=b3, in1=Mb,
                                                        op=ALU.add), reads=[bgb_r, bgsm], writes=[bgfl_r])
                    S.op("act", lambda: A.activation(out=gfl_r, in_=gfl_r, func=AF.Exp, scale=-1.0),
                         reads=[bgfl_r], writes=[bgfl_r])
                    S.op("dve", lambda: V.tensor_tensor(out=scR[:], in0=dmask[:],
                                                        in1=gsm[:, SC, :].unsqueeze(1).to_broadcast([4, 4, NCH]), op=ALU.mult),
                         reads=[bdmask, bgsm], writes=[bscR])

                def gates_bcast():
                    SC = 5
                    pm, bpm = pmm[mmc[0] % 2]
                    mmc[0] += 1
                    S.op("pe", lambda: T.matmul(pm[:, 0:4 * NCH], lhsT=ones4[:], rhs=scR[:].rearrange("p h c -> p (h c)"),
                                                start=True, stop=True), reads=[bones4, bscR], writes=[bpm])
                    S.op("dve", lambda: V.tensor_copy(out=scb[:].rearrange("p h c -> p (h c)"), in_=pm[:, 0:4 * NCH]),
                         reads=[bpm], writes=[bscb])
                    pm, bpm = pmm[mmc[0] % 2]
                    mmc[0] += 1
                    for i in range(NT):
                        S.op("pe", lambda: T.matmul(pm[:, 8 * i:8 * i + 4], lhsT=gea_r[:, i * 128:(i + 1) * 128],
                                                    rhs=identf[0:4, 0:4], start=True, stop=True),
                             reads=[bgea_r, bidentf], writes=[bpm])
                        S.op("pe", lambda: T.matmul(pm[:, 8 * i + 4:8 * i + 8], lhsT=gfl_r[:, i * 128:(i + 1) * 128],
                                                    rhs=identf[0:4, 0:4], start=True, stop=True),
                             reads=[bgfl_r, bidentf], writes=[bpm])
                    pv = pm[:, 0:8 * NT].rearrange("p (i e) -> p i e", e=8)
                    if cur_flag[0] is None:
                        S.op("dve", lambda: V.tensor_copy(out=eaT[:], in_=pv[:, :, 0:4]), reads=[bpm], writes=[beaT])
                    else:
                        fc = cur_flag[0]
                        S.op("dve", lambda: V.tensor_scalar(out=eaT[:], in0=pv[:, :, 0:4], scalar1=flg[:, fc:fc + 1], scalar2=None,
                                                            op0=ALU.mult), reads=[bpm, bflg], writes=[beaT])
                    S.op("dve", lambda: V.tensor_copy(out=flT[:], in_=pv[:, :, 4:8]), reads=[bpm], writes=[bflT])

                vsc = [0]
                cur_flag = [None]

                def mixer_chunk_so(c):
                    i = c
                    v_, bv_ = vs[vsc[0] % 2]
                    vsc[0] += 1
                    S.op("dve", lambda: V.tensor_tensor(out=v_[:], in0=vaug[:, i, :, :],
                                                        in1=eaT[:, i, :].unsqueeze(2).to_broadcast([128, 4, 258]),
                                                        op=ALU.mult), reads=[bvaug[i], beaT], writes=[bv_])
                    cbanks = [(pst, bpst), (patt, bpatt)]
                    for half in range(2):
                        pb_, bpb_ = cbanks[half]
                        S.ops("pe", [(lambda hq=hq: T.matmul(pb_[:, hq * 256:(hq + 1) * 256],
                                                             lhsT=kTM[:, i, (half * 2 + hq) * 128:(half * 2 + hq + 1) * 128],
                                                             rhs=v_[:, half * 2 + hq, 0:256], start=True, stop=True))
                                     for hq in range(2)], reads=[bkTM, bv_], writes=[bpb_])
                    pmn, bpmn = pmm[mmc[0] % 2]
                    mmc[0] += 1
                    S.ops("pe", [(lambda h=h: T.matmul(pmn[:, h:h + 1], lhsT=kTM[:, i, h * 128:(h + 1) * 128],
                                                       rhs=v_[:, h, 256:257], start=True, stop=True)) for h in range(4)],
                          reads=[bkTM, bv_], writes=[bpmn])
                    for half in range(2):
                        pb_, bpb_ = po[half]
                        S.ops("pe", [(lambda hq=hq: T.matmul(pb_[:, hq * 128:(hq + 1) * 128],
                                                             lhsT=kgTM[:, i, (half * 4 + hq) * 128:(half * 4 + hq + 1) * 128],
                                                             rhs=iv[:, i, (half * 4 + hq) * 128:(half * 4 + hq + 1) * 128],
                                                             start=True, stop=True)) for hq in range(4)],
                              reads=[bkgTM[half * 4 + hq_] for hq_ in range(4)] + [biv[i]], writes=[bpb_])
                    S.op("dve", lambda: V.tensor_tensor(out=Cst[:], in0=Cst[:],
                                                        in1=scb[:, :, c:c + 1].to_broadcast([128, 4, 258]), op=ALU.mult),
                         reads=[bC, bscb], writes=[bC])
                    for half in range(2):
                        pb_, bpb_ = cbanks[half]
                        S.op("dve", lambda: V.tensor_tensor(out=Cst[:, half * 2:half * 2 + 2, 0:256],
                                                            in0=Cst[:, half * 2:half * 2 + 2, 0:256],
                                                            in1=pb_[:, :].rearrange("p (h v) -> p h v", v=256), op=ALU.add),
                             reads=[bC, bpb_], writes=[bC])
                    S.op("dve", lambda: V.tensor_tensor(out=Cst[:, :, 256:257], in0=Cst[:, :, 256:257],
                                                        in1=pmn[:, 0:4].unsqueeze(2), op=ALU.add),
                         reads=[bC, bpmn], writes=[bC])
                    for half in range(2):
                        pb_, bpb_ = po[half]
                        for hq in range(4):
                            h = half * 4 + hq
                            S.op("dve", lambda: V.scalar_tensor_tensor(out=Sst[:, h, :], in0=Sst[:, h, :],
                                                                       scalar=expg[:, h, c:c + 1],
                                                                       in1=pb_[:, hq * 128:(hq + 1) * 128],
                                                                       op0=ALU.mult, op1=ALU.add),
                                 reads=[bS, bexpg[h], bpb_], writes=[bS])

                def mixer_chunk(c, full):
                    if not full:
                        return mixer_chunk_so(c)
                    i = c
                    tc = slice(c * 128, (c + 1) * 128)
                    v_, bv_ = vs[vsc[0] % 2]
                    vsc[0] += 1
                    S.op("dve", lambda: V.tensor_tensor(out=v_[:], in0=vaug[:, i, :, :],
                                                        in1=eaT[:, i, :].unsqueeze(2).to_broadcast([128, 4, 258]),
                                                        op=ALU.mult), reads=[bvaug[i], beaT], writes=[bv_])
                    S.op("dve", lambda: V.tensor_tensor(out=Cst[:], in0=Cst[:],
                                                        in1=scb[:, :, c:c + 1].to_broadcast([128, 4, 258]), op=ALU.mult),
                         reads=[bC, bscb], writes=[bC])
                    if full:
                        S.op("act", lambda: A.activation(out=Cb[:], in_=Cst[:], func=AF.Copy, scale=QSCALE),
                             reads=[bC], writes=[bCb])
                        for h in range(4):
                            S.op("pe", lambda: T.matmul(patt[:, h * 128:(h + 1) * 128], lhsT=qkT[:, 4 + h, tc],
                                                        rhs=qkT[:, h, tc], start=True, stop=True),
                                 reads=[bqk[4 + h], bqk[h]], writes=[bpatt])
                        S.op("dve", lambda: V.tensor_tensor(out=stm[:].rearrange("p (h t) -> p h t", t=128),
                                                            in0=patt[:, :].rearrange("p (h t) -> p h t", t=128),
                                                            in1=maskc[:].unsqueeze(1).to_broadcast([128, 4, 128]), op=ALU.mult),
                             reads=[bpatt, bmaskc], writes=[bstm])
                        pmd, bpmd = pmm[mmc[0] % 2]
                        mmc[0] += 1
                        for h in range(4):
                            pm, bpm = po[h // 2]
                            osl = slice((h % 2) * 256, (h % 2) * 256 + 256)
                            S.op("pe", lambda: T.matmul(pm[:, osl], lhsT=stm[:, h * 128:(h + 1) * 128],
                                                        rhs=v_[:, h, 0:256], start=True, stop=False),
                                 reads=[bstm, bv_], writes=[bpm])
                            S.op("pe", lambda: T.matmul(pm[:, osl], lhsT=qkT[:, h, tc], rhs=Cb[:, h, 0:256],
                                                        start=False, stop=True), reads=[bqk[h], bCb], writes=[bpm])
                            S.op("pe", lambda: T.matmul(pmd[:, h:h + 1], lhsT=stm[:, h * 128:(h + 1) * 128],
                                                        rhs=v_[:, h, 256:257], start=True, stop=False),
                                 reads=[bstm, bv_], writes=[bpmd])
                            S.op("pe", lambda: T.matmul(pmd[:, h:h + 1], lhsT=qkT[:, h, tc], rhs=Cb[:, h, 256:257],
                                                        start=False, stop=True), reads=[bqk[h], bCb], writes=[bpmd])
                        f_ = fin
                        S.op("act", lambda: A.activation(out=f_[:, 0:4], in_=pmd[:, 0:4], func=AF.Abs),
                             reads=[bpmd], writes=[bfin])
                        S.op("dve", lambda: V.tensor_tensor(out=f_[:, 0:4], in0=f_[:, 0:4], in1=flT[:, i, :], op=ALU.max),
                             reads=[bfin, bflT], writes=[bfin])
                        S.op("dve", lambda: V.reciprocal(out=f_[:, 0:4], in_=f_[:, 0:4]), reads=[bfin], writes=[bfin])
                        for half in range(2):
                            pm, bpm = po[half]
                            S.op("act", lambda: A.activation(out=sqt[:, half * 512:(half + 1) * 512], in_=pm[:, :],
                                                             func=AF.Square), reads=[bpm], writes=[bsqt])
                        S.op("dve", lambda: V.tensor_reduce(out=f_[:, 4:8], in_=sqt[:, :].rearrange("p (h v) -> p h v", v=256),
                                                            axis=AX.X, op=ALU.add), reads=[bsqt], writes=[bfin])
                        S.op("dve", lambda: V.tensor_tensor(out=f_[:, 8:12], in0=f_[:, 0:4], in1=f_[:, 0:4], op=ALU.mult),
                             reads=[bfin], writes=[bfin])
                        S.op("dve", lambda: V.tensor_tensor(out=f_[:, 8:12], in0=f_[:, 8:12], in1=f_[:, 4:8], op=ALU.mult),
                             reads=[bfin], writes=[bfin])
                        S.op("dve", lambda: V.tensor_scalar(out=f_[:, 8:12], in0=f_[:, 8:12], scalar1=1.0 / 256, scalar2=EPS,
                                                            op0=ALU.mult, op1=ALU.add), reads=[bfin], writes=[bfin])
                        S.op("act", lambda: A.activation(out=f_[:, 8:12], in_=f_[:, 8:12], func=AF.Ln),
                             reads=[bfin], writes=[bfin])
                        S.op("act", lambda: A.activation(out=f_[:, 8:12], in_=f_[:, 8:12], func=AF.Exp, scale=-0.5),
                             reads=[bfin], writes=[bfin])
                        S.op("dve", lambda: V.tensor_tensor(out=f_[:, 8:12], in0=f_[:, 8:12], in1=f_[:, 0:4], op=ALU.mult),
                             reads=[bfin], writes=[bfin])
                        for h in range(4):
                            pm, bpm = po[h // 2]
                            osl = slice((h % 2) * 256, (h % 2) * 256 + 256)
                            gsl = slice(h * 256, (h + 1) * 256)
                            S.op("dve", lambda: V.scalar_tensor_tensor(out=GY[:, i, gsl], in0=pm[:, osl],
                                                                       scalar=f_[:, 8 + h:9 + h], in1=GY[:, i, gsl],
                                                                       op0=ALU.mult, op1=ALU.mult),
                                 reads=[bpm, bfin, bGY[i][h // 2]], writes=[bGY[i][h // 2]])
                    for half in range(2):
                        for hq in range(2):
                            h = half * 2 + hq
                            S.op("pe", lambda: T.matmul(pst[:, hq * 256:(hq + 1) * 256], lhsT=kTM[:, i, h * 128:(h + 1) * 128],
                                                        rhs=v_[:, h, 0:256], start=True, stop=True),
                                 reads=[bkTM, bv_], writes=[bpst])
                        S.op("dve", lambda: V.tensor_tensor(out=Cst[:, half * 2:half * 2 + 2, 0:256],
                                                            in0=Cst[:, half * 2:half * 2 + 2, 0:256],
                                                            in1=pst[:, :].rearrange("p (h v) -> p h v", v=256), op=ALU.add),
                             reads=[bC, bpst], writes=[bC])
                    pmn, bpmn = pmm[mmc[0] % 2]
                    mmc[0] += 1
                    for h in range(4):
                        S.op("pe", lambda: T.matmul(pmn[:, h:h + 1], lhsT=kTM[:, i, h * 128:(h + 1) * 128],
                                                    rhs=v_[:, h, 256:257], start=True, stop=True),
                             reads=[bkTM, bv_], writes=[bpmn])
                    S.op("dve", lambda: V.tensor_tensor(out=Cst[:, :, 256:257], in0=Cst[:, :, 256:257],
                                                        in1=pmn[:, 0:4].unsqueeze(2), op=ALU.add),
                         reads=[bC, bpmn], writes=[bC])
                    if full:
                        for h in range(8):
                            S.op("act", lambda: A.activation(out=Sb[:, h, :], in_=Sst[:, h, :], func=AF.Copy,
                                                             scale=eref[:, h, c:c + 1]), reads=[bS, bexpg[h]], writes=[bSb])
                        for half in range(2):
                            for hq in range(4):
                                h = half * 4 + hq
                                S.op("pe", lambda: T.matmul(patt[:, hq * 128:(hq + 1) * 128], lhsT=kd[:, h, tc], rhs=qd[:, h, tc],
                                                            start=True, stop=True), reads=[bkd[h], bqd[h]], writes=[bpatt])
                            S.op("dve", lambda: V.tensor_tensor(out=stm[:].rearrange("p (h t) -> p h t", t=128),
                                                                in0=patt[:, :].rearrange("p (h t) -> p h t", t=128),
                                                                in1=mask1[:].unsqueeze(1).to_broadcast([128, 4, 128]),
                                                                op=ALU.mult), reads=[bpatt, bmask1], writes=[bstm])
                            pm, bpm = po[half]
                            for hq in range(4):
                                h = half * 4 + hq
                                osl = slice(hq * 128, hq * 128 + 128)
                                S.op("pe", lambda: T.matmul(pm[:, osl], lhsT=stm[:, hq * 128:(hq + 1) * 128],
                                                            rhs=iv[:, i, h * 128:(h + 1) * 128], start=True, stop=False),
                                     reads=[bstm, biv[i]], writes=[bpm])
                                S.op("pe", lambda: T.matmul(pm[:, osl], lhsT=qd[:, h, tc], rhs=Sb[:, h, :],
                                                            start=False, stop=True), reads=[bqd[h], bSb], writes=[bpm])
                        f_ = fin
                        for half in range(2):
                            pm, bpm = po[half]
                            S.op("act", lambda: A.activation(out=sqt[:, half * 512:(half + 1) * 512], in_=pm[:, :],
                                                             func=AF.Square), reads=[bpm], writes=[bsqt])
                        S.op("dve", lambda: V.tensor_reduce(out=f_[:, 16:24], in_=sqt[:, :].rearrange("p (h v) -> p h v", v=128),
                                                            axis=AX.X, op=ALU.add), reads=[bsqt], writes=[bfin])
                        S.op("dve", lambda: V.tensor_scalar(out=f_[:, 16:24], in0=f_[:, 16:24], scalar1=1.0 / 128, scalar2=EPS,
                                                            op0=ALU.mult, op1=ALU.add), reads=[bfin], writes=[bfin])
                        S.op("act", lambda: A.activation(out=f_[:, 16:24], in_=f_[:, 16:24], func=AF.Ln),
                             reads=[bfin], writes=[bfin])
                        S.op("act", lambda: A.activation(out=f_[:, 16:24], in_=f_[:, 16:24], func=AF.Exp, scale=-0.5),
                             reads=[bfin], writes=[bfin])
                        for h in range(8):
                            pm, bpm = po[h // 4]
                            osl = slice((h % 4) * 128, (h % 4) * 128 + 128)
                            gsl = slice(1024 + h * 128, 1024 + (h + 1) * 128)
                            S.op("dve", lambda: V.scalar_tensor_tensor(out=GY[:, i, gsl], in0=pm[:, osl],
                                                                       scalar=f_[:, 16 + h:17 + h], in1=GY[:, i, gsl],
                                                                       op0=ALU.mult, op1=ALU.mult),
                                 reads=[bpm, bfin, bGY[i][2 + h // 4]], writes=[bGY[i][2 + h // 4]])
                    for half in range(2):
                        for hq in range(4):
                            h = half * 4 + hq
                            S.op("pe", lambda: T.matmul(pst[:, hq * 128:(hq + 1) * 128], lhsT=kgTM[:, i, h * 128:(h + 1) * 128],
                                                        rhs=iv[:, i, h * 128:(h + 1) * 128], start=True, stop=True),
                                 reads=[bkgTM[h], biv[i]], writes=[bpst])
                        for hq in range(4):
                            h = half * 4 + hq
                            S.op("dve", lambda: V.scalar_tensor_tensor(out=Sst[:, h, :], in0=Sst[:, h, :],
                                                                       scalar=expg[:, h, c:c + 1],
                                                                       in1=pst[:, hq * 128:(hq + 1) * 128],
                                                                       op0=ALU.mult, op1=ALU.add),
                                 reads=[bS, bexpg[h], bpst], writes=[bS])

                def run_pass(full, src_d, ngroups, flagcol=None):
                    def phase_a(g_):
                        ta = g_ * G
                        for i in range(NT):
                            S.dma("act", "ldx%d_%d" % (cur_par, i), XH[:, i, :], src_d[ta + i * 128:ta + (i + 1) * 128, :], writes=[bXHt[i]])
                        for i in range(NT):
                            norm_transpose(XH[:, i, :], bXHt[i], i, nwc, bnwc)

                    for g in range(ngroups):
                        t0 = g * G
                        cur_flag[0] = None if flagcol is None else flagcol(t0)
                        select(g)
                        if g == 0:
                            phase_a(g)
                        S.mark("phaseA")
                        wt, bw = load_w(win_d[:, C_I:C_I + 8], ncols=8, key="gates", rowscale=(nwc, bnwc))
                        pm_i, bpm_i = mm_fm(wt, bw, 0, 4)
                        pm_f, bpm_f = mm_fm(wt, bw, 4, 4)
                        gates_rows(pm_i, bpm_i, pm_f, bpm_f)
                        S.mark("gates")
                        for blk in ((0, 1) if full else (1,)):
                            wt, bw = load_w(win_d[:, blk * 512:(blk + 1) * 512], key="qk%d" % blk, rowscale=(nwc, bnwc))
                            for j in range(4):
                                pm, bpm = mm_fm(wt, bw, j * 128, 128)
                                conv_block(pm, bpm, blk * 4 + j)
                        S.mark("qk")
                        if (not full) and g == ngroups - 1:
                            wt, bw = load_w(win_d[:, 0:512], key="qk0", rowscale=(nwc, bnwc))
                            for j in range(4):
                                pm, bpm = mm_fm(wt, bw, j * 128, 128, ntoks=128, tok0=G - 128)
                                conv_block(pm, bpm, j, ntoks=128, only_halo=True)
                        S.mark("ktm")
                        for cb in range(2):
                            wt, bw = load_w(win_d[:, C_V + cb * 512:C_V + (cb + 1) * 512], key="v%d" % cb, rowscale=(nwc, bnwc))
                            for i in range(NT):
                                pm, bpm = mm_tm(i, wt, bw)
                                S.op("act", lambda: A.copy(out=vaug[:, i, 2 * cb:2 * cb + 2, 0:256],
                                                           in_=pm[:, :].rearrange("p (h v) -> p h v", v=256)),
                                     reads=[bpm], writes=[bvaug[i]])
                        S.mark("v")
                        if full:
                            for cb in range(2):
                                wt, bw = load_w(win_d[:, C_O + cb * 512:C_O + (cb + 1) * 512], key="o%d" % cb, rowscale=(nwc, bnwc))
                                for i in range(NT):
                                    pm, bpm = mm_tm(i, wt, bw)
                                    S.op("act", lambda: A.activation(out=GY[:, i, cb * 512:(cb + 1) * 512], in_=pm[:, :],
                                                                     func=AF.Sigmoid), reads=[bpm], writes=[bGY[i][cb]])
                            for cb in range(2):
                                wt, bw = load_w(win_d[:, C_Z + cb * 512:C_Z + (cb + 1) * 512], key="z%d" % cb, rowscale=(nwc, bnwc))
                                for i in range(NT):
                                    pm, bpm = mm_tm(i, wt, bw)
                                    zt, bzt = ztmp[(cb * NT + i) % 2]
                                    S.op("act", lambda: A.activation(out=zt[:], in_=pm[:, :], func=AF.Silu),
                                         reads=[bpm], writes=[bzt])
                                    S.op("dve", lambda: V.tensor_tensor(out=GY[:, i, cb * 512:(cb + 1) * 512],
                                                                        in0=GY[:, i, cb * 512:(cb + 1) * 512], in1=zt[:],
                                                                        op=ALU.mult), reads=[bzt, bGY[i][cb]], writes=[bGY[i][cb]])
                        S.mark("oz")
                        gates_bcast()
                        for hb in range(2):
                            wt, bw = load_w(win_d[:, C_HF + hb * 512:C_HF + (hb + 1) * 512], key="hf%d" % hb, rowscale=(nwc, bnwc))
                            if full:
                                wtq, bwq = load_w(win_d[:, C_HQ + hb * 512:C_HQ + (hb + 1) * 512], key="hq%d" % hb, rowscale=(nwc, bnwc))
                            for j in range(4):
                                h = hb * 4 + j
                                pm, bpm = mm_fm(wt, bw, j * 128, 128)
                                hf_block(pm, bpm, h, full)
                                if full:
                                    pm, bpm = mm_fm(wtq, bwq, j * 128, 128)
                                    S.op("dve", lambda: V.tensor_tensor(out=qd[:, h, :], in0=pm[:, 0:G], in1=ebf[:, h % 2, :],
                                                                        op=ALU.mult), reads=[bpm, beb[h % 2]], writes=[bqd[h]])
                        S.mark("hgrn")
                        for cb in range(2):
                            wt, bw = load_w(win_d[:, C_HI + cb * 512:C_HI + (cb + 1) * 512], key="hi%d" % cb, rowscale=(nwc, bnwc))
                            for i in range(NT):
                                pm, bpm = mm_tm(i, wt, bw)
                                S.op("act", lambda: A.copy(out=iv[:, i, cb * 512:(cb + 1) * 512], in_=pm[:, :]),
                                     reads=[bpm], writes=[biv[i]])
                        if full:
                            for cb in range(2):
                                wt, bw = load_w(win_d[:, C_HG + cb * 512:C_HG + (cb + 1) * 512], key="hg%d" % cb, rowscale=(nwc, bnwc))
                                for i in range(NT):
                                    pm, bpm = mm_tm(i, wt, bw)
                                    S.op("act", lambda: A.activation(out=GY[:, i, 1024 + cb * 512:1024 + (cb + 1) * 512],
                                                                     in_=pm[:, :], func=AF.Silu),
                                         reads=[bpm], writes=[bGY[i][2 + cb]])
                        S.mark("hi_hg")
                        k_to_tm()
                        for h_ in range(8):
                            kg_to_tm(h_)
                        if g + 1 < ngroups:
                            select(g + 1)
                            phase_a(g + 1)
                            select(g)
                        for c in range(NCH):
                            mixer_chunk(c, full)
                            S.mark("mix%d" % c)
                        S.mark("mixer")
                        if not full:
                            continue
                        for i in range(NT):
                            for hb in range(2):
                                pt, bpt = ptr[tcount[0] % 2]
                                tcount[0] += 1
                                S.ops("pe", [(lambda j=j: T.transpose(out=pt[:, j, :],
                                                                      in_=GY[:, i, (hb * 8 + j) * 128:(hb * 8 + j + 1) * 128],
                                                                      identity=ident[:])) for j in range(8)],
                                      reads=[bGY[i][2 * hb], bGY[i][2 * hb + 1], bident], writes=[bpt])
                                if hb == 0:
                                    S.op("act", lambda: A.copy(out=actT[:, 0:8, i * 128:(i + 1) * 128], in_=pt[:, :, :]),
                                         reads=[bpt], writes=[bactT])
                                else:
                                    S.op("dve", lambda: V.tensor_copy(out=actT[:, 8:16, i * 128:(i + 1) * 128], in_=pt[:, :, :]),
                                         reads=[bpt], writes=[bactT])
                        S.mark("yT")
                        for cb in range(4):
                            wt, bw = load_w(wout_d[:, cb * 512:(cb + 1) * 512], key="wo%d" % cb, rowscale=(mixc, bmixc))
                            for i in range(NT):
                                pm, bpm = mm_tm(i, wt, bw)
                                S.op("dve", lambda: V.tensor_tensor(out=XH[:, i, cb * 512:(cb + 1) * 512],
                                                                    in0=XH[:, i, cb * 512:(cb + 1) * 512], in1=pm[:, :],
                                                                    op=ALU.add), reads=[bpm, bXHt[i]], writes=[bXHt[i]])
                        S.mark("wout")
                        for i in range(NT):
                            norm_transpose(XH[:, i, :], bXHt[i], i, pnc, bpnc)
                        S.mark("hT")
                        for i in range(NT):
                            pl, bpl = ptile[i % 2]
                            pb_, bpb_ = pbt[i % 2]
                            S.dma("act", "ldp%d" % (i % 2), pl[:], p_d[t0 + i * 128:t0 + (i + 1) * 128, :], writes=[bpl])
                            S.op("dve", lambda: V.tensor_copy(out=pb_[:], in_=pl[:]), reads=[bpl], writes=[bpb_])
                            pt, bpt = ptr[tcount[0] % 2]
                            tcount[0] += 1
                            for kk in range(2):
                                S.op("pe", lambda: T.transpose(out=pt[:, kk, :], in_=pb_[:, kk * 128:(kk + 1) * 128],
                                                               identity=ident[:]), reads=[bpb_, bident], writes=[bpt])
                            S.op("act", lambda: A.copy(out=pTt[:, :, i * 128:(i + 1) * 128], in_=pt[:, 0:2, :]),
                                 reads=[bpt], writes=[bpT])
                        S.mark("pT")
                        for cb in range(4):
                            wt, bw = load_w(wpg_d[:, cb * 512:(cb + 1) * 512], key="pg%d" % cb, rowscale=(pnc, bpnc))
                            wt2, bw2 = load_w(wpe_d[:, cb * 512:(cb + 1) * 512], kchunks=2, key="pe%d" % cb)
                            for i in range(NT):
                                pm, bpm = mm_tm(i, wt, bw)
                                g_, bg_ = gt[i % 2]
                                S.op("act", lambda: A.activation(out=g_[:], in_=pm[:, :], func=AF.Sigmoid),
                                     reads=[bpm], writes=[bg_])
                                pm2, bpm2 = mm_tm(i, wt2, bw2, kchunks=2, lhs=pTt, blhs=bpT)
                                S.op("dve", lambda: V.tensor_tensor(out=g_[:], in0=g_[:], in1=pm2[:, :], op=ALU.mult),
                                     reads=[bg_, bpm2], writes=[bg_])
                                S.op("dve", lambda: V.tensor_tensor(out=XH[:, i, cb * 512:(cb + 1) * 512],
                                                                    in0=XH[:, i, cb * 512:(cb + 1) * 512], in1=g_[:],
                                                                    op=ALU.add), reads=[bg_, bXHt[i]], writes=[bXHt[i]])
                        S.mark("gate_e")
                        for i in range(NT):
                            u, bu = ub[i % 2]
                            S.op("act", lambda: A.activation(out=u[:], in_=XH[:, i, :], func=AF.Square,
                                                             accum_out=ssq[:, 4 + i:5 + i]),
                                 reads=[bXHt[i]], writes=[bu, bssq])
                            rstd_from_ssq(4 + i, D)
                            S.op("dve", lambda: V.scalar_tensor_tensor(out=XH[:, i, :], in0=XH[:, i, :],
                                                                       scalar=rstd[:, 4 + i:5 + i], in1=fnw[:],
                                                                       op0=ALU.mult, op1=ALU.mult),
                                 reads=[bXHt[i], brstd, bfnw], writes=[bXHt[i]])
                            S.dma("act", "sto%d_%d" % (cur_par, i), out_d[t0 + i * 128:t0 + (i + 1) * 128, :], XH[:, i, :], reads=[bXHt[i]])

                run_pass(full, src_d, ngroups_, flagcol_)
                S.barrier()

        def zero_state():
            S.op("dve", lambda: V.memset(Cst[:], 0.0), writes=[bC])
            S.op("dve", lambda: V.memset(Sst[:], 0.0), writes=[bS])
            S.op("dve", lambda: V.memset(Sb[:], 0.0), writes=[bSb])
            S.op("dve", lambda: V.memset(mcar[:], 0.0), writes=[bmcar])

        zero_state()
        S.op("dve", lambda: V.memset(Gseg[:], 0.0), writes=[bGseg])
        S.op("dve", lambda: V.memset(gsum[:], 0.0), writes=[bgsum])
        if nprev > 0:
            emit_pass(False, NT_SO, xprev_d, nprev * ntok // (128 * NT_SO), lambda t0: t0 // ntok)
        emit_pass(True, NT, x_d, ntok // (128 * NT))
    build_program.last_ninst = dict(S.ninst)
    return nc


_CFG = dict(ntok=SEG, NT=2, NT_SO=4, nprev=3)


def _flags_for(sgm, nprev):
    f = np.zeros((128, 16), np.float32)
    for j in range(nprev):
        if sgm - nprev + j >= 0:
            f[:, j] = 1.0
    return f


def kernel(**inputs):
    x = np.asarray(inputs["x"], np.float32)
    p = np.asarray(inputs["p"], np.float32)
    B, SQ, _ = x.shape
    nseg = NCORES // B
    seg = SQ // nseg
    cfg = dict(_CFG)
    cfg["ntok"] = seg
    cfg["nprev"] = nseg - 1
    nprev = cfg["nprev"]
    nc = build_program(**cfg)
    shared = {}
    for k in ["norm_w", "w_in", "conv_w", "conv_b", "ml_b_i", "ml_b_f", "ml_norm_w", "hg_norm_w", "w_out",
              "pe_norm_w", "w_pg", "w_pe"]:
        shared[k] = np.ascontiguousarray(np.asarray(inputs[k], np.float32)[0])
    shared["hg_lb"] = np.ascontiguousarray(np.asarray(inputs["hg_lb"], np.float32))
    shared["final_norm_w"] = np.ascontiguousarray(np.asarray(inputs["final_norm_w"], np.float32))
    in_maps = []
    for c in range(NCORES):
        b, sgm = c // nseg, c % nseg
        m = dict(shared)
        m["x"] = np.ascontiguousarray(x[b, sgm * seg:(sgm + 1) * seg])
        xp = np.zeros((nprev * seg, D), np.float32)
        if sgm > 0:
            xp[(nprev - sgm) * seg:] = x[b, 0:sgm * seg]
        m["xprev"] = xp
        m["p"] = np.ascontiguousarray(p[0, b, sgm * seg:(sgm + 1) * seg])
        m["flags"] = _flags_for(sgm, nprev)
        in_maps.append(m)
    res = run_bass_kernel_spmd(nc, in_maps, core_ids=list(range(NCORES)))
    out = np.empty((B, SQ, D), np.float32)
    for c in range(NCORES):
        b, sgm = c // nseg, c % nseg
        out[b, sgm * seg:(sgm + 1) * seg] = res.results[c]["out"]
    return out
```

```python
import numpy as np
from contextlib import ExitStack
import concourse.bass as bass
import concourse.mybir as mybir
from concourse.bass_utils import run_bass_kernel_spmd

F32 = mybir.dt.float32
BF16 = mybir.dt.bfloat16
AF = mybir.ActivationFunctionType
ALU = mybir.AluOpType
AX = mybir.AxisListType

D = 2048
INC = 8200
NCORES = 8
SEG = 4096
EPS = 1e-6
QSCALE = 128 ** -0.5
CC_INC = 16

C_Q, C_K, C_V, C_O, C_Z, C_I, C_F, C_HQ, C_HF, C_HI, C_HG = (
    0, 512, 1024, 2048, 3072, 4096, 4100, 4104, 5128, 6152, 7176)


class Buf:
    __slots__ = ("name", "w", "r")

    def __init__(self, name):
        self.name = name
        self.w = None
        self.r = {}


class Sched:
    def __init__(self, nc, stack, same_engine_sync=True):
        self.nc = nc
        self.stack = stack
        self.eng = {"pe": nc.tensor, "act": nc.scalar, "dve": nc.vector,
                    "pool": nc.gpsimd, "sp": nc.sync}
        self.sems = {}
        self.cnt = {}
        self.seen = {e: {} for e in self.eng}
        self.same = same_engine_sync
        for e in self.eng:
            self.newsem(e)
        self.ninst = {e: 0 for e in self.eng}
        self.stopped = False
        self.stop_at = None

    def mark(self, label):
        if self.stop_at is not None and label == self.stop_at:
            self.stopped = True

    def newsem(self, key):
        self.sems[key] = self.stack.enter_context(self.nc.semaphore("s_" + key))
        self.cnt[key] = 0
        return key

    def _deps(self, reads, writes):
        deps = {}

        def add(k, v):
            if deps.get(k, 0) < v:
                deps[k] = v
        for b in reads:
            if b.w is not None:
                add(*b.w)
        for b in writes:
            if b.w is not None:
                add(*b.w)
            for k, v in b.r.items():
                add(k, v)
        return deps

    def _wait(self, e, deps):
        seen = self.seen[e]
        for k, v in deps.items():
            if k == e and (not self.same or e == "pe"):
                continue
            if seen.get(k, 0) >= v:
                continue
            self.eng[e].wait_ge(self.sems[k], v)
            seen[k] = v

    def op(self, e, fn, reads=(), writes=()):
        if self.stopped:
            return None
        self._wait(e, self._deps(reads, writes))
        ins = fn()
        self.cnt[e] += 1
        v = self.cnt[e]
        ins.then_inc(self.sems[e], 1)
        self.ninst[e] += 1
        for b in reads:
            if b.r.get(e, 0) < v:
                b.r[e] = v
        for b in writes:
            b.w = (e, v)
            b.r = {}
        return ins

    def ops(self, e, fns, reads=(), writes=()):
        if self.stopped:
            return None
        self._wait(e, self._deps(reads, writes))
        ins = None
        for fn in fns:
            ins = fn()
        self.cnt[e] += 1
        v = self.cnt[e]
        ins.then_inc(self.sems[e], 1)
        self.ninst[e] += len(fns)
        for b in reads:
            if b.r.get(e, 0) < v:
                b.r[e] = v
        for b in writes:
            b.w = (e, v)
            b.r = {}
        return ins

    def dma(self, q, semkey, out, in_, reads=(), writes=(), **kw):
        if self.stopped:
            return None
        self._wait(q, self._deps(reads, writes))
        ins = self.eng[q].dma_start(out=out, in_=in_, **kw)
        self.cnt[semkey] += 16
        v = self.cnt[semkey]
        ins.then_inc(self.sems[semkey], 16)
        self.ninst[q] += 1
        for b in reads:
            if b.r.get(semkey, 0) < v:
                b.r[semkey] = v
        for b in writes:
            b.w = (semkey, v)
            b.r = {}
        return ins

    def barrier(self):
        if self.stopped:
            return
        deps = {k: v for k, v in self.cnt.items() if v > 0}
        for e in self.eng:
            self._wait(e, deps)

    def finish(self, e, bufs):
        deps = {}
        for b in bufs:
            toks = list(b.r.items())
            if b.w is not None:
                toks.append(b.w)
            for k, v in toks:
                if deps.get(k, 0) < v:
                    deps[k] = v
        self._wait(e, deps)


def build_program(ntok=SEG, NT=2, NT_SO=4, nprev=3, debug=None, stop_at=None, use_wsc=True):
    assert ntok % (128 * NT) == 0 and (nprev * ntok) % (128 * NT_SO) == 0
    nc = bass.Bass("TRN2", target_bir_lowering=False)
    dram_in = lambda n, s: nc.dram_tensor(n, s, F32, kind="ExternalInput").ap()
    x_d = dram_in("x", [ntok, D])
    xprev_d = dram_in("xprev", [max(nprev, 1) * ntok, D])
    p_d = dram_in("p", [ntok, 256])
    normw_d = dram_in("norm_w", [D])
    win_d = dram_in("w_in", [D, INC])
    convw_d = dram_in("conv_w", [4, 1024])
    convb_d = dram_in("conv_b", [1024])
    bi_d = dram_in("ml_b_i", [4])
    bf_d = dram_in("ml_b_f", [4])
    mlnw_d = dram_in("ml_norm_w", [1024])
    hglb_d = dram_in("hg_lb", [2, 1024])
    hgnw_d = dram_in("hg_norm_w", [1024])
    wout_d = dram_in("w_out", [D, D])
    penw_d = dram_in("pe_norm_w", [D])
    wpg_d = dram_in("w_pg", [D, D])
    wpe_d = dram_in("w_pe", [256, D])
    fnw_d = dram_in("final_norm_w", [D])
    flags_d = dram_in("flags", [128, 16])
    out_d = nc.dram_tensor("out", [ntok, D], F32, kind="ExternalOutput").ap()
    dbg_d = {}
    if debug:
        for n, s in debug.items():
            dbg_d[n] = nc.dram_tensor("dbg_" + n, list(s), F32, kind="ExternalOutput").ap()

    SW = 8
    with ExitStack() as st:
        S = Sched(nc, st)
        S.stop_at = stop_at

        def sb(name, shape, dt):
            t = st.enter_context(nc.sbuf_tensor(name, list(shape), dt))
            return t, Buf(name)

        def ps(name, shape, dt):
            t = st.enter_context(nc.psum_tensor(name, list(shape), dt))
            return t, Buf(name)

        V, A, P_, T = nc.vector, nc.scalar, nc.gpsimd, nc.tensor

        NW = 3
        Wb = [sb("W%d" % i, [128, 16, 512], BF16) for i in range(NW)]
        ub = [sb("ub%d" % i, [128, D], BF16) for i in range(2)]
        ptr = [ps("ptr%d" % i, [128, 8, 128], BF16) for i in range(2)]
        pmm = [ps("pmm%d" % i, [128, 512], F32) for i in range(2)]
        pst, bpst = ps("pst", [128, 512], F32)
        patt, bpatt = ps("patt", [128, 512], F32)
        po = [ps("po%d" % i, [128, 512], F32) for i in range(2)]
        ident, bident = sb("ident", [128, 128], BF16)
        identf, bidentf = sb("identf", [128, 128], F32)
        maskc, bmaskc = sb("maskc", [128, 128], F32)
        mask1, bmask1 = sb("mask1", [128, 128], F32)
        nwc, bnwc = sb("nwc", [128, 16], F32)
        pnc, bpnc = sb("pnc", [128, 16], F32)
        mixc, bmixc = sb("mixc", [128, 16], F32)
        cw, bcw = sb("cw", [128, 8, 4], F32)
        cbias, bcbias = sb("cbias", [128, 8], F32)
        lbt, blbt = sb("lbt", [128, 2, 8], F32)
        lb, blb = sb("lb", [128, 8], F32)
        oml, boml = sb("oml", [128, 8], F32)
        noml, bnoml = sb("noml", [128, 8], F32)
        gbias, bgbias = sb("gbias", [4, 3], F32)
        fnw, bfnw = sb("fnw", [128, D], F32)
        ones4, bones4 = sb("ones4", [4, 128], F32)
        flg, bflg = sb("flg", [128, 16], F32)
        halo, bhalo = sb("halo", [128, 8, 3], F32)
        mcar, bmcar = sb("mcar", [4, 1], F32)
        gsum, bgsum = sb("gsum", [4, 1], F32)
        Cst, bC = sb("Cst", [128, 4, 258], F32)
        Cb, bCb = sb("Cb", [128, 4, 258], BF16)
        Sst, bS = sb("Sst", [128, 8, 128], F32)
        Sb, bSb = sb("Sb", [128, 8, 128], BF16)
        Gseg, bGseg = sb("Gseg", [128, 8], F32)
        ssq, bssq = sb("ssq", [128, 8], F32)
        rstd, brstd = sb("rstd", [128, 8], F32)
        fin, bfin = sb("fin", [128, 64], F32)
        NTMAX = max(NT, NT_SO)
        for k in (["ldx%d_%d" % (q_, i) for q_ in range(2) for i in range(NTMAX)] + ["ldw%d" % i for i in range(NW)] +
                  ["ldc", "ldp0", "ldp1", "dbg"] + ["wbk%d" % i for i in range(NW)] +
                  ["ldwh%d" % i for i in range(NW)] + ["sto%d_%d" % (q_, i) for q_ in range(2) for i in range(NTMAX)]):
            S.newsem(k)

        def dbg(name, ap, bufs):
            if name in dbg_d:
                S.dma("sp", "dbg", dbg_d[name], ap, reads=bufs)

        def cdma(out, in_, wb, **kw):
            S.dma("sp", "ldc", out, in_, writes=[wb], **kw)
        cdma(nwc[:], normw_d.rearrange("(k p) -> p k", p=128), bnwc, allow_slow_non_contiguous=True)
        cdma(pnc[:], penw_d.rearrange("(k p) -> p k", p=128), bpnc, allow_slow_non_contiguous=True)
        cdma(mixc[:, 0:8], mlnw_d.rearrange("(k p) -> p k", p=128), bmixc, allow_slow_non_contiguous=True)
        cdma(mixc[:, 8:16], hgnw_d.rearrange("(k p) -> p k", p=128), bmixc, allow_slow_non_contiguous=True)
        for kk in range(4):
            cdma(cw[:, :, kk], convw_d[kk].rearrange("(c p) -> p c", p=128), bcw, allow_slow_non_contiguous=True)
        cdma(cbias[:], convb_d.rearrange("(c p) -> p c", p=128), bcbias, allow_slow_non_contiguous=True)
        for r_ in range(2):
            cdma(lbt[:, r_, :], hglb_d[r_].rearrange("(h p) -> p h", p=128), blbt, allow_slow_non_contiguous=True)
        cdma(gbias[:, 0:1], bi_d.rearrange("(h o) -> h o", o=1), bgbias, allow_slow_non_contiguous=True)
        cdma(gbias[:, 1:2], bf_d.rearrange("(h o) -> h o", o=1), bgbias, allow_slow_non_contiguous=True)
        cdma(fnw[:], fnw_d.partition_broadcast(128), bfnw)
        cdma(flg[:], flags_d, bflg)
        for b_ in (bnwc, bpnc, bmixc, bcw, bcbias, blbt, bgbias, bfnw, bflg):
            b_.w = ("ldc", S.cnt["ldc"])

        S.op("pool", lambda: P_.memset(identf[:], 0.0), writes=[bidentf])
        S.op("pool", lambda: P_.affine_select(out=identf[:], in_=identf[:], pattern=[[-1, 128]],
                                              compare_op=ALU.not_equal, fill=1.0, base=0, channel_multiplier=1),
             reads=[bidentf], writes=[bidentf])
        S.op("dve", lambda: V.tensor_copy(out=ident[:], in_=identf[:]), reads=[bidentf], writes=[bident])
        for (mk, bm, val) in ((maskc, bmaskc, QSCALE), (mask1, bmask1, 1.0)):
            S.op("pool", lambda: P_.memset(mk[:], val), writes=[bm])
            S.op("pool", lambda: P_.affine_select(out=mk[:], in_=mk[:], pattern=[[1, 128]],
                                                  compare_op=ALU.is_ge, fill=0.0, base=0, channel_multiplier=-1),
                 reads=[bm], writes=[bm])
        S.op("pool", lambda: P_.memset(ones4[:], 1.0), writes=[bones4])
        S.op("dve", lambda: V.tensor_scalar(out=gbias[:, 2:3], in0=gbias[:, 1:2], scalar1=-1.0, scalar2=None, op0=ALU.mult),
             reads=[bgbias], writes=[bgbias])
        S.op("dve", lambda: V.tensor_sub(out=lb[:], in0=lbt[:, 0, :], in1=lbt[:, 1, :]), reads=[blbt], writes=[blb])
        S.op("act", lambda: A.activation(out=oml[:], in_=lb[:], func=AF.Sigmoid, scale=-1.0), reads=[blb], writes=[boml])
        S.op("act", lambda: A.activation(out=lb[:], in_=lb[:], func=AF.Sigmoid), reads=[blb], writes=[blb])
        S.op("dve", lambda: V.tensor_scalar(out=noml[:], in0=oml[:], scalar1=-1.0, scalar2=None, op0=ALU.mult),
             reads=[boml], writes=[bnoml])

        S.mark("consts")
        wslot = [0]

        wsc = {}

        def load_w(src_ap, kchunks=16, ncols=512, key=None, rowscale=None):
            s = wslot[0] % NW
            wslot[0] += 1
            wt, bw = Wb[s]
            if key is not None and key in wsc:
                sc_ap, bsc = wsc[key]
                S.dma("sp", "ldwh%d" % s, wt[:, 0:kchunks, 0:ncols],
                      sc_ap.rearrange("p (k c) -> p k c", c=ncols), reads=[bsc], writes=[bw])
                return wt, bw
            v = src_ap.rearrange("(k p) c -> p k c", p=128)
            step = 4 if kchunks >= 4 else kchunks
            for k0 in range(0, kchunks, step):
                S.dma("pool", "ldw%d" % s, wt[:, k0:k0 + step, 0:ncols], v[:, k0:k0 + step, :], writes=[bw])
            if rowscale is not None:
                colw_, bcolw_ = rowscale
                for k in range(kchunks):
                    if k % 2 == 0:
                        S.op("dve", lambda: V.tensor_scalar(out=wt[:, k, 0:ncols], in0=wt[:, k, 0:ncols],
                                                            scalar1=colw_[:, k:k + 1], scalar2=None, op0=ALU.mult),
                             reads=[bw, bcolw_], writes=[bw])
                    else:
                        S.op("act", lambda: A.activation(out=wt[:, k, 0:ncols], in_=wt[:, k, 0:ncols], func=AF.Copy,
                                                         scale=colw_[:, k:k + 1]), reads=[bw, bcolw_], writes=[bw])
            if key is not None and use_wsc:
                sc_ap = nc.dram_tensor("wsc_" + key, [128, kchunks * ncols], BF16).ap()
                bsc = Buf("wsc_" + key)
                S.dma("sp", "wbk%d" % s, sc_ap.rearrange("p (k c) -> p k c", c=ncols), wt[:, 0:kchunks, 0:ncols],
                      reads=[bw], writes=[bsc])
                wsc[key] = (sc_ap, bsc)
            return wt, bw

        def rstd_from_ssq(col, n, width=1):
            sl = slice(col, col + width)
            S.op("dve", lambda: V.tensor_scalar(out=rstd[:, sl], in0=ssq[:, sl], scalar1=1.0 / n,
                                                scalar2=EPS, op0=ALU.mult, op1=ALU.add), reads=[bssq], writes=[brstd])
            S.op("act", lambda: A.activation(out=rstd[:, sl], in_=rstd[:, sl], func=AF.Ln), reads=[brstd], writes=[brstd])
            S.op("act", lambda: A.activation(out=rstd[:, sl], in_=rstd[:, sl], func=AF.Exp, scale=-0.5),
                 reads=[brstd], writes=[brstd])

        S.op("dve", lambda: V.memset(halo[:], 0.0), writes=[bhalo])

        def emit_pass(full, NT, src_d, ngroups_, flagcol_=None):
            G = 128 * NT
            NCH = NT
            QOFF = 0 if full else 4
            with ExitStack() as st2:
                def sb(name, shape, dt):
                    t = st2.enter_context(nc.sbuf_tensor(name + ("_f" if full else "_s"), list(shape), dt))
                    return t, Buf(name)

                NPAR = 2 if full else 1
                XH_l = [sb("XH%d" % q_, [128, NT, D], F32) for q_ in range(NPAR)]
                bXHt_l = [[Buf("XH%d_%d" % (q_, i)) for i in range(NT)] for q_ in range(NPAR)]
                actT_l = [sb("actT%d" % q_, [128, 16, G], BF16) for q_ in range(NPAR)]
                XH, bXHt = XH_l[0][0], bXHt_l[0]
                actT, bactT = actT_l[0]

                cur_par = 0

                def select(par):
                    nonlocal XH, bXHt, actT, bactT, cur_par
                    par = par % NPAR
                    cur_par = par
                    XH, bXHt = XH_l[par][0], bXHt_l[par]
                    actT, bactT = actT_l[par]
                mreset, bmreset = sb("mreset", [128, G], F32)
                dmask, bdmask = sb("dmask", [4, 4, NCH], F32)
                cpre = [sb("cpre%d" % i, [128, G + 3], F32) for i in range(2)]
                cacc = [sb("cacc%d" % i, [128, G], F32) for i in range(2)]
                qkT, bqkT = sb("qkT", [128, 8 if full else 4, G], BF16)
                bqk = [Buf("qk%d" % i) for i in range(8)]
                kd, bkd_ = sb("kd", [128, 8, G], BF16)
                bkd = [Buf("kd%d" % i) for i in range(8)]
                bqd = [Buf("qd%d" % i) for i in range(8)]
                beb = [Buf("eb%d" % i) for i in range(2)]
                if full:
                    qd, bqd_ = sb("qd", [128, 8, G], BF16)
                    ebf, bebf_ = sb("ebf", [128, 2, G], F32)
                expg, bexpg_ = sb("expg", [128, 8, NCH], F32)
                eref, beref_ = sb("eref", [128, 8, NCH], F32)
                egr, begr_ = sb("egr", [128, 8, NCH], F32)
                hsm, bhsm_ = sb("hsm", [128, 8, 4, NCH], F32)
                bhsm = [Buf("hsm%d" % i) for i in range(8)]
                bexpg = [Buf("expg%d" % i) for i in range(8)]
                hft = [sb("hft%d_%d" % (i, j), [128, G], F32) for i in range(2) for j in range(3)]
                kgt = [sb("kgt%d" % i, [128, G], BF16) for i in range(8)]
                vaug, bvaug_ = sb("vaug", [128, NT, 4, 258], BF16)
                bvaug = [Buf("vaug%d" % i) for i in range(NT)]
                bGY = [[Buf("GY%d_%d" % (i, j)) for j in range(4)] for i in range(NT)]
                if full:
                    GY, bGY_ = sb("GY", [128, NT, D], BF16)
                iv, biv_ = sb("iv", [128, NT, 1024], BF16)
                biv = [Buf("iv%d" % i) for i in range(NT)]
                kTM, bkTM_ = sb("kTM", [128, NT, 512], BF16)
                bkTM = Buf("kTM")
                kgTM, bkgTM_ = sb("kgTM", [128, NT, 1024], BF16)
                bkgTM = [Buf("kgTM%d" % i) for i in range(8)]
                vs = [sb("vs%d" % i, [128, 4, 258], BF16) for i in range(2)]
                (gi_r, bgi_r), (gf_r, bgf_r), (gb_r, bgb_r), (ga_r, bga_r), (gea_r, bgea_r), (gfl_r, bgfl_r) = [
                    (t[0:4, :], b) for (t, b) in hft]
                gsm, bgsm = sb("gsm", [4, 8, NCH], F32)
                scR, bscR = sb("scR", [4, 4, NCH], F32)
                scb, bscb = sb("scb", [128, 4, NCH], F32)
                eaT, beaT = sb("eaT", [128, NT, 4], F32)
                flT, bflT = sb("flT", [128, NT, 4], F32)
                if full:
                    ztmp = [sb("ztmp%d" % i, [128, 512], BF16) for i in range(2)]
                    stm, bstm = sb("stm", [128, 512], BF16)
                    stA = [sb("stA%d" % q_, [128, 512], BF16) for q_ in range(2)]
                    sqt, bsqt = sb("sqt", [128, 1024], F32)
                    pTt, bpT = sb("pT", [128, 2, G], BF16)
                    ptile = [sb("ptile%d" % i, [128, 256], F32) for i in range(2)]
                    pbt = [sb("pbt%d" % i, [128, 256], BF16) for i in range(2)]
                    gt = [sb("gt%d" % i, [128, 512], F32) for i in range(2)]
                S.op("pool", lambda: P_.memset(mreset[:], 1.0), writes=[bmreset])
                S.op("pool", lambda: P_.memset(mreset[:].rearrange("p (c l) -> p c l", l=128)[:, :, 0:1], 0.0),
                     writes=[bmreset])
                S.op("pool", lambda: P_.memset(dmask[:], 1.0), writes=[bdmask])
                S.op("pool", lambda: P_.affine_select(out=dmask[:], in_=dmask[:], pattern=[[1, 4], [0, NCH]],
                                                      compare_op=ALU.is_equal, fill=0.0, base=0, channel_multiplier=-1),
                     reads=[bdmask], writes=[bdmask])
                S.op("pool", lambda: P_.memset(vaug[:], 1.0), writes=bvaug)

                tcount = [0]

                def norm_transpose(src_ap, src_buf, i, colw, bcolw):
                    u, bu = ub[tcount[0] % 2]
                    S.op("act", lambda: A.activation(out=u[:], in_=src_ap, func=AF.Square, accum_out=ssq[:, i:i + 1]),
                         reads=[src_buf], writes=[bu, bssq])
                    rstd_from_ssq(i, D)
                    S.op("dve", lambda: V.tensor_scalar(out=u[:], in0=src_ap, scalar1=rstd[:, i:i + 1], scalar2=None,
                                                        op0=ALU.mult), reads=[src_buf, brstd], writes=[bu])
                    transpose16(u, bu, i, colw, bcolw)

                def transpose16(u, bu, i, colw, bcolw):
                    for hb in range(2):
                        pt, bpt = ptr[tcount[0] % 2]
                        tcount[0] += 1
                        S.ops("pe", [(lambda j=j: T.transpose(out=pt[:, j, :], in_=u[:, (hb * 8 + j) * 128:(hb * 8 + j + 1) * 128],
                                                              identity=ident[:])) for j in range(8)],
                              reads=[bu, bident], writes=[bpt])
                        if hb == 0:
                            S.op("act", lambda: A.copy(out=actT[:, 0:8, i * 128:(i + 1) * 128], in_=pt[:, :, :]),
                                 reads=[bpt], writes=[bactT])
                        else:
                            S.op("dve", lambda: V.tensor_copy(out=actT[:, 8:16, i * 128:(i + 1) * 128], in_=pt[:, :, :]),
                                 reads=[bpt], writes=[bactT])

                mmc = [0]
                pj = [0]
                pbanks = pmm + po + [(patt, bpatt)]

                def mm_tm(i, wt, bw, kchunks=16, lhs=None, blhs=None, ncols=512):
                    pm, bpm = pbanks[pj[0] % len(pbanks)]
                    pj[0] += 1
                    lhs = actT if lhs is None else lhs
                    blhs = bactT if blhs is None else blhs
                    S.ops("pe", [(lambda k=k: T.matmul(pm[:, 0:ncols], lhsT=lhs[:, k, i * 128:(i + 1) * 128],
                                                       rhs=wt[:, k, 0:ncols], start=(k == 0), stop=(k == kchunks - 1)))
                                 for k in range(kchunks)], reads=[blhs, bw], writes=[bpm])
                    return pm, bpm

                def mm_fm(wt, bw, c0, m, ntoks=G, tok0=0):
                    pm, bpm = pbanks[pj[0] % len(pbanks)]
                    pj[0] += 1
                    S.ops("pe", [(lambda k=k: T.matmul(pm[0:m, 0:ntoks], lhsT=wt[:, k, c0:c0 + m],
                                                       rhs=actT[:, k, tok0:tok0 + ntoks], start=(k == 0), stop=(k == 15)))
                                 for k in range(16)], reads=[bactT, bw], writes=[bpm])
                    return pm, bpm

                cvc = [0]

                def conv_block(pm, bpm, c8, ntoks=G, only_halo=False):
                    cp, bcp = cpre[cvc[0] % 2]
                    ca, bca = cacc[cvc[0] % 2]
                    cvc[0] += 1
                    S.op("dve", lambda: V.tensor_copy(out=cp[:, 0:3], in_=halo[:, c8, :]), reads=[bhalo], writes=[bcp])
                    S.op("act", lambda: A.copy(out=cp[:, 3:3 + ntoks], in_=pm[:, 0:ntoks]), reads=[bpm], writes=[bcp])
                    S.op("dve", lambda: V.tensor_copy(out=halo[:, c8, :], in_=cp[:, ntoks:ntoks + 3]), reads=[bcp], writes=[bhalo])
                    if only_halo:
                        return
                    S.op("dve", lambda: V.tensor_scalar(out=ca[:, 0:ntoks], in0=cp[:, 0:ntoks], scalar1=cw[:, c8, 0:1],
                                                        scalar2=cbias[:, c8:c8 + 1], op0=ALU.mult, op1=ALU.add),
                         reads=[bcp, bcw, bcbias], writes=[bca])
                    for kk in range(1, 4):
                        S.op("dve", lambda: V.scalar_tensor_tensor(out=ca[:, 0:ntoks], in0=cp[:, kk:kk + ntoks],
                                                                   scalar=cw[:, c8, kk:kk + 1], in1=ca[:, 0:ntoks],
                                                                   op0=ALU.mult, op1=ALU.add),
                             reads=[bcp, bcw, bca], writes=[bca])
                    S.op("act", lambda: A.activation(out=qkT[:, c8 - QOFF, 0:ntoks], in_=ca[:, 0:ntoks], func=AF.Silu),
                         reads=[bca], writes=[bqk[c8]])

                def k_to_tm():
                    for h in range(4):
                        pt, bpt = ptr[tcount[0] % 2]
                        tcount[0] += 1
                        S.ops("pe", [(lambda i=i: T.transpose(out=pt[:, i, :], in_=qkT[:, 4 + h - QOFF, i * 128:(i + 1) * 128],
                                                              identity=ident[:])) for i in range(NT)],
                              reads=[bqk[4 + h], bident], writes=[bpt])
                        S.op("act", lambda: A.copy(out=kTM[:, :, h * 128:(h + 1) * 128], in_=pt[:, 0:NT, :]),
                             reads=[bpt], writes=[bkTM])

                hfc = [0]
                E38 = float(np.exp(38.0))

                def hf_block(pm, bpm, h, full):
                    s = hfc[0] % 2
                    hfc[0] += 1
                    (t0, b0), (t1, b1), (t2, b2) = hft[s * 3], hft[s * 3 + 1], hft[s * 3 + 2]
                    kg, bkg = kgt[h]
                    hl = h % 2
                    S.op("act", lambda: A.activation(out=t0[:], in_=pm[:, 0:G], func=AF.Exp, scale=-1.0), reads=[bpm], writes=[b0])
                    S.op("act", lambda: A.activation(out=t0[:], in_=t0[:], func=AF.Ln, bias=1.0), reads=[b0], writes=[b0])
                    S.op("act", lambda: A.activation(out=t0[:], in_=t0[:], func=AF.Exp, scale=-1.0), reads=[b0], writes=[b0])
                    S.op("act", lambda: A.activation(out=t1[:], in_=t0[:], func=AF.Ln, scale=oml[:, h:h + 1], bias=lb[:, h:h + 1]),
                         reads=[b0, boml, blb], writes=[b1])
                    S.op("act", lambda: A.activation(out=t0[:], in_=t0[:], func=AF.Identity, scale=noml[:, h:h + 1],
                                                     bias=oml[:, h:h + 1]), reads=[b0, bnoml, boml], writes=[b0])
                    S.op("dve", lambda: V.tensor_tensor_scan(out=t2[:], data0=mreset[:], data1=t1[:], initial=0.0,
                                                             op0=ALU.mult, op1=ALU.add), reads=[bmreset, b1], writes=[b2])
                    b3 = t2[:].rearrange("p (c l) -> p c l", l=128)
                    S.op("dve", lambda: V.tensor_copy(out=hsm[:, h, 0, :], in_=b3[:, :, 63]), reads=[b2], writes=[bhsm[h]])
                    S.op("dve", lambda: V.tensor_scalar(out=hsm[:, h, 1, :], in0=b3[:, :, 63], scalar1=-1.0, scalar2=None,
                                                        op0=ALU.mult), reads=[b2], writes=[bhsm[h]])
                    S.op("dve", lambda: V.tensor_sub(out=hsm[:, h, 2, :], in0=b3[:, :, 127], in1=b3[:, :, 63]),
                         reads=[b2], writes=[bhsm[h]])
                    S.op("act", lambda: A.activation(out=expg[:, h, :], in_=b3[:, :, 127], func=AF.Exp),
                         reads=[b2], writes=[bexpg[h]])
                    S.op("act", lambda: A.activation(out=eref[:, h, :], in_=hsm[:, h, 0, :], func=AF.Exp),
                         reads=[bhsm[h]], writes=[bexpg[h]])
                    S.op("act", lambda: A.activation(out=egr[:, h, :], in_=hsm[:, h, 2, :], func=AF.Exp),
                         reads=[bhsm[h]], writes=[bexpg[h]])
                    for c in range(NCH):
                        cs = slice(c * 128, (c + 1) * 128)
                        S.op("act", lambda: A.activation(out=t1[:, cs], in_=t2[:, cs], func=AF.Exp, scale=-1.0,
                                                         bias=hsm[:, h, 0, c:c + 1]), reads=[b2, bhsm[h]], writes=[b1])
                        if full:
                            S.op("act", lambda: A.activation(out=ebf[:, hl, cs], in_=t2[:, cs], func=AF.Exp,
                                                             bias=hsm[:, h, 1, c:c + 1]), reads=[b2, bhsm[h]], writes=[beb[hl]])
                    S.op("dve", lambda: V.scalar_tensor_tensor(out=kd[:, h, :], in0=t1[:], scalar=E38, in1=t0[:],
                                                               op0=ALU.min, op1=ALU.mult), reads=[b0, b1], writes=[bkd[h]])
                    S.op("dve", lambda: V.tensor_tensor(out=kg[:].rearrange("p (c l) -> p c l", l=128),
                                                        in0=kd[:, h, :].rearrange("p (c l) -> p c l", l=128),
                                                        in1=egr[:, h, :].unsqueeze(2).to_broadcast([128, NCH, 128]),
                                                        op=ALU.mult), reads=[bkd[h], bexpg[h]], writes=[bkg])

                def kg_to_tm(h):
                    kg, bkg = kgt[h]
                    pt, bpt = ptr[tcount[0] % 2]
                    tcount[0] += 1
                    S.ops("pe", [(lambda i=i: T.transpose(out=pt[:, i, :], in_=kg[:, i * 128:(i + 1) * 128], identity=ident[:]))
                                 for i in range(NT)], reads=[bkg, bident], writes=[bpt])
                    S.op("act", lambda: A.copy(out=kgTM[:, :, h * 128:(h + 1) * 128], in_=pt[:, 0:NT, :]),
                         reads=[bpt], writes=[bkgTM[h]])

                def gates_rows(pm_i, bpm_i, pm_f, bpm_f):
                    S.op("act", lambda: A.activation(out=gi_r, in_=pm_i[0:4, 0:G], func=AF.Identity, bias=gbias[:, 0:1]),
                         reads=[bpm_i, bgbias], writes=[bgi_r])
                    S.op("act", lambda: A.activation(out=gf_r, in_=pm_f[0:4, 0:G], func=AF.Exp, scale=-1.0, bias=gbias[:, 2:3]),
                         reads=[bpm_f, bgbias], writes=[bgf_r])
                    S.op("act", lambda: A.activation(out=gf_r, in_=gf_r, func=AF.Ln, bias=1.0), reads=[bgf_r], writes=[bgf_r])
                    S.op("dve", lambda: V.tensor_scalar(out=gf_r, in0=gf_r, scalar1=-1.0, scalar2=None, op0=ALU.mult),
                         reads=[bgf_r], writes=[bgf_r])
                    S.op("dve", lambda: V.tensor_tensor_scan(out=gb_r, data0=mreset[0:4, :], data1=gf_r, initial=0.0,
                                                             op0=ALU.mult, op1=ALU.add), reads=[bmreset, bgf_r], writes=[bgb_r])
                    S.op("dve", lambda: V.tensor_sub(out=ga_r, in0=gi_r, in1=gb_r), reads=[bgi_r, bgb_r], writes=[bga_r])
                    a3 = ga_r.rearrange("p (c l) -> p c l", l=128)
                    b3 = gb_r.rearrange("p (c l) -> p c l", l=128)
                    AMAX, GSH, MM, GM, MPREV, SC = range(6)
                    S.op("dve", lambda: V.tensor_reduce(out=gsm[:, AMAX, :], in_=a3, axis=AX.X, op=ALU.max),
                         reads=[bga_r], writes=[bgsm])
                    S.op("dve", lambda: V.memset(gsm[:, GSH, 0:1], 0.0), writes=[bgsm])
                    if NCH > 1:
                        S.op("dve", lambda: V.tensor_copy(out=gsm[:, GSH, 1:NCH], in_=b3[:, 0:NCH - 1, 127]),
                             reads=[bgb_r], writes=[bgsm])
                    S.op("dve", lambda: V.tensor_tensor_scan(out=gsm[:, MM, :], data0=gsm[:, GSH, :], data1=gsm[:, AMAX, :],
                                                             initial=mcar[:, 0:1], op0=ALU.add, op1=ALU.max),
                         reads=[bgsm, bmcar], writes=[bgsm])
                    S.op("dve", lambda: V.tensor_add(out=gsm[:, GM, :], in0=gsm[:, MM, :], in1=b3[:, :, 127]),
                         reads=[bgsm, bgb_r], writes=[bgsm])
                    S.op("dve", lambda: V.tensor_copy(out=gsm[:, MPREV, 0:1], in_=mcar[:, 0:1]), reads=[bmcar], writes=[bgsm])
                    if NCH > 1:
                        S.op("dve", lambda: V.tensor_copy(out=gsm[:, MPREV, 1:NCH], in_=gsm[:, GM, 0:NCH - 1]),
                             reads=[bgsm], writes=[bgsm])
                    S.op("dve", lambda: V.tensor_copy(out=mcar[:, 0:1], in_=gsm[:, GM, NCH - 1:NCH]), reads=[bgsm], writes=[bmcar])
                    S.op("dve", lambda: V.tensor_sub(out=gsm[:, SC, :], in0=gsm[:, MPREV, :], in1=gsm[:, MM, :]),
                         reads=[bgsm], writes=[bgsm])
                    S.op("act", lambda: A.activation(out=gsm[:, SC, :], in_=gsm[:, SC, :], func=AF.Exp), reads=[bgsm], writes=[bgsm])
                    Mb = gsm[:, MM, :].unsqueeze(2).to_broadcast([4, NCH, 128])
                    S.op("dve", lambda: V.tensor_tensor(out=gea_r.rearrange("p (c l) -> p c l", l=128), in0=a3, in1=Mb,
                                                        op=ALU.subtract), reads=[bga_r, bgsm], writes=[bgea_r])
                    S.op("act", lambda: A.activation(out=gea_r, in_=gea_r, func=AF.Exp), reads=[bgea_r], writes=[bgea_r])
                    S.op("dve", lambda: V.tensor_tensor(out=gfl_r.rearrange("p (c l) -> p c l", l=128), in0=b3, in1=Mb,
                                                        op=ALU.add), reads=[bgb_r, bgsm], writes=[bgfl_r])
                    S.op("act", lambda: A.activation(out=gfl_r, in_=gfl_r, func=AF.Exp, scale=-1.0),
                         reads=[bgfl_r], writes=[bgfl_r])
                    S.op("dve", lambda: V.tensor_tensor(out=scR[:], in0=dmask[:],
                                                        in1=gsm[:, SC, :].unsqueeze(1).to_broadcast([4, 4, NCH]), op=ALU.mult),
                         reads=[bdmask, bgsm], writes=[bscR])

                def gates_bcast():
                    SC = 5
                    pm, bpm = pmm[mmc[0] % 2]
                    mmc[0] += 1
                    S.op("pe", lambda: T.matmul(pm[:, 0:4 * NCH], lhsT=ones4[:], rhs=scR[:].rearrange("p h c -> p (h c)"),
                                                start=True, stop=True), reads=[bones4, bscR], writes=[bpm])
                    S.op("dve", lambda: V.tensor_copy(out=scb[:].rearrange("p h c -> p (h c)"), in_=pm[:, 0:4 * NCH]),
                         reads=[bpm], writes=[bscb])
                    pm, bpm = pmm[mmc[0] % 2]
                    mmc[0] += 1
                    for i in range(NT):
                        S.op("pe", lambda: T.matmul(pm[:, 8 * i:8 * i + 4], lhsT=gea_r[:, i * 128:(i + 1) * 128],
                                                    rhs=identf[0:4, 0:4], start=True, stop=True),
                             reads=[bgea_r, bidentf], writes=[bpm])
                        S.op("pe", lambda: T.matmul(pm[:, 8 * i + 4:8 * i + 8], lhsT=gfl_r[:, i * 128:(i + 1) * 128],
                                                    rhs=identf[0:4, 0:4], start=True, stop=True),
                             reads=[bgfl_r, bidentf], writes=[bpm])
                    pv = pm[:, 0:8 * NT].rearrange("p (i e) -> p i e", e=8)
                    if cur_flag[0] is None:
                        S.op("dve", lambda: V.tensor_copy(out=eaT[:], in_=pv[:, :, 0:4]), reads=[bpm], writes=[beaT])
                    else:
                        fc = cur_flag[0]
                        S.op("dve", lambda: V.tensor_scalar(out=eaT[:], in0=pv[:, :, 0:4], scalar1=flg[:, fc:fc + 1], scalar2=None,
                                                            op0=ALU.mult), reads=[bpm, bflg], writes=[beaT])
                    S.op("dve", lambda: V.tensor_copy(out=flT[:], in_=pv[:, :, 4:8]), reads=[bpm], writes=[bflT])

                vsc = [0]
                cur_flag = [None]

                def mixer_chunk_so(c):
                    i = c
                    v_, bv_ = vs[vsc[0] % 2]
                    vsc[0] += 1
                    S.op("dve", lambda: V.tensor_tensor(out=v_[:], in0=vaug[:, i, :, :],
                                                        in1=eaT[:, i, :].unsqueeze(2).to_broadcast([128, 4, 258]),
                                                        op=ALU.mult), reads=[bvaug[i], beaT], writes=[bv_])
                    cbanks = [(pst, bpst), (patt, bpatt)]
                    for half in range(2):
                        pb_, bpb_ = cbanks[half]
                        S.ops("pe", [(lambda hq=hq: T.matmul(pb_[:, hq * 256:(hq + 1) * 256],
                                                             lhsT=kTM[:, i, (half * 2 + hq) * 128:(half * 2 + hq + 1) * 128],
                                                             rhs=v_[:, half * 2 + hq, 0:256], start=True, stop=True))
                                     for hq in range(2)], reads=[bkTM, bv_], writes=[bpb_])
                    pmn, bpmn = pmm[mmc[0] % 2]
                    mmc[0] += 1
                    S.ops("pe", [(lambda h=h: T.matmul(pmn[:, h:h + 1], lhsT=kTM[:, i, h * 128:(h + 1) * 128],
                                                       rhs=v_[:, h, 256:257], start=True, stop=True)) for h in range(4)],
                          reads=[bkTM, bv_], writes=[bpmn])
                    for half in range(2):
                        pb_, bpb_ = po[half]
                        S.ops("pe", [(lambda hq=hq: T.matmul(pb_[:, hq * 128:(hq + 1) * 128],
                                                             lhsT=kgTM[:, i, (half * 4 + hq) * 128:(half * 4 + hq + 1) * 128],
                                                             rhs=iv[:, i, (half * 4 + hq) * 128:(half * 4 + hq + 1) * 128],
                                                             start=True, stop=True)) for hq in range(4)],
                              reads=[bkgTM[half * 4 + hq_] for hq_ in range(4)] + [biv[i]], writes=[bpb_])
                    S.op("dve", lambda: V.tensor_tensor(out=Cst[:], in0=Cst[:],
                                                        in1=scb[:, :, c:c + 1].to_broadcast([128, 4, 258]), op=ALU.mult),
                         reads=[bC, bscb], writes=[bC])
                    for half in range(2):
                        pb_, bpb_ = cbanks[half]
                        S.op("dve", lambda: V.tensor_tensor(out=Cst[:, half * 2:half * 2 + 2, 0:256],
                                                            in0=Cst[:, half * 2:half * 2 + 2, 0:256],
                                                            in1=pb_[:, :].rearrange("p (h v) -> p h v", v=256), op=ALU.add),
                             reads=[bC, bpb_], writes=[bC])
                    S.op("dve", lambda: V.tensor_tensor(out=Cst[:, :, 256:257], in0=Cst[:, :, 256:257],
                                                        in1=pmn[:, 0:4].unsqueeze(2), op=ALU.add),
                         reads=[bC, bpmn], writes=[bC])
                    for half in range(2):
                        pb_, bpb_ = po[half]
                        for hq in range(4):
                            h = half * 4 + hq
                            S.op("dve", lambda: V.scalar_tensor_tensor(out=Sst[:, h, :], in0=Sst[:, h, :],
                                                                       scalar=expg[:, h, c:c + 1],
                                                                       in1=pb_[:, hq * 128:(hq + 1) * 128],
                                                                       op0=ALU.mult, op1=ALU.add),
                                 reads=[bS, bexpg[h], bpb_], writes=[bS])

                def mixer_chunk_full(c):
                    i = c
                    tc = slice(c * 128, (c + 1) * 128)
                    f_ = fin
                    v_, bv_ = vs[vsc[0] % 2]
                    vsc[0] += 1
                    S.op("dve", lambda: V.tensor_tensor(out=v_[:], in0=vaug[:, i, :, :],
                                                        in1=eaT[:, i, :].unsqueeze(2).to_broadcast([128, 4, 258]),
                                                        op=ALU.mult), reads=[bvaug[i], beaT], writes=[bv_])
                    S.op("dve", lambda: V.tensor_tensor(out=Cst[:], in0=Cst[:],
                                                        in1=scb[:, :, c:c + 1].to_broadcast([128, 4, 258]), op=ALU.mult),
                         reads=[bC, bscb], writes=[bC])
                    S.op("act", lambda: A.activation(out=Cb[:], in_=Cst[:], func=AF.Copy, scale=QSCALE),
                         reads=[bC], writes=[bCb])
                    for h in range(8):
                        S.op("act", lambda: A.activation(out=Sb[:, h, :], in_=Sst[:, h, :], func=AF.Copy,
                                                         scale=eref[:, h, c:c + 1]), reads=[bS, bexpg[h]], writes=[bSb])
                    S.ops("pe", [(lambda h=h: T.matmul(patt[:, h * 128:(h + 1) * 128], lhsT=qkT[:, 4 + h, tc],
                                                       rhs=qkT[:, h, tc], start=True, stop=True)) for h in range(4)],
                          reads=bqk, writes=[bpatt])
                    for half in range(2):
                        pa, bpa = pmm[half]
                        S.ops("pe", [(lambda hq=hq: T.matmul(pa[:, hq * 128:(hq + 1) * 128], lhsT=kd[:, half * 4 + hq, tc],
                                                             rhs=qd[:, half * 4 + hq, tc], start=True, stop=True))
                                     for hq in range(4)],
                              reads=[bkd[half * 4 + q_] for q_ in range(4)] + [bqd[half * 4 + q_] for q_ in range(4)],
                              writes=[bpa])
                    S.op("dve", lambda: V.tensor_tensor(out=stm[:].rearrange("p (h t) -> p h t", t=128),
                                                        in0=patt[:, :].rearrange("p (h t) -> p h t", t=128),
                                                        in1=maskc[:].unsqueeze(1).to_broadcast([128, 4, 128]), op=ALU.mult),
                         reads=[bpatt, bmaskc], writes=[bstm])
                    for half in range(2):
                        pa, bpa = pmm[half]
                        sa, bsa = stA[half]
                        S.op("dve", lambda: V.tensor_tensor(out=sa[:].rearrange("p (h t) -> p h t", t=128),
                                                            in0=pa[:, :].rearrange("p (h t) -> p h t", t=128),
                                                            in1=mask1[:].unsqueeze(1).to_broadcast([128, 4, 128]),
                                                            op=ALU.mult), reads=[bpa, bmask1], writes=[bsa])
                    for half in range(2):
                        pm, bpm = po[half]
                        fns = []
                        for hq in range(2):
                            h = half * 2 + hq
                            osl = slice(hq * 256, hq * 256 + 256)
                            fns.append(lambda h=h, osl=osl: T.matmul(pm[:, osl], lhsT=stm[:, h * 128:(h + 1) * 128],
                                                                     rhs=v_[:, h, 0:256], start=True, stop=False))
                            fns.append(lambda h=h, osl=osl: T.matmul(pm[:, osl], lhsT=qkT[:, h, tc], rhs=Cb[:, h, 0:256],
                                                                     start=False, stop=True))
                        S.ops("pe", fns, reads=[bstm, bv_, bCb] + bqk[0:4], writes=[bpm])
                    fns = []
                    for h in range(4):
                        fns.append(lambda h=h: T.matmul(patt[:, h:h + 1], lhsT=stm[:, h * 128:(h + 1) * 128],
                                                        rhs=v_[:, h, 256:257], start=True, stop=False))
                        fns.append(lambda h=h: T.matmul(patt[:, h:h + 1], lhsT=qkT[:, h, tc], rhs=Cb[:, h, 256:257],
                                                        start=False, stop=True))
                    S.ops("pe", fns, reads=[bstm, bv_, bCb] + bqk[0:4], writes=[bpatt])
                    S.ops("pe", [(lambda hq=hq: T.matmul(pst[:, hq * 256:(hq + 1) * 256], lhsT=kTM[:, i, hq * 128:(hq + 1) * 128],
                                                         rhs=v_[:, hq, 0:256], start=True, stop=True)) for hq in range(2)],
                          reads=[bkTM, bv_], writes=[bpst])
                    for half in range(2):
                        pm, bpm = pmm[half]
                        sa, bsa = stA[half]
                        fns = []
                        for hq in range(4):
                            h = half * 4 + hq
                            osl = slice(hq * 128, hq * 128 + 128)
                            fns.append(lambda h=h, hq=hq, osl=osl: T.matmul(pm[:, osl], lhsT=sa[:, hq * 128:(hq + 1) * 128],
                                                                            rhs=iv[:, i, h * 128:(h + 1) * 128],
                                                                            start=True, stop=False))
                            fns.append(lambda h=h, osl=osl: T.matmul(pm[:, osl], lhsT=qd[:, h, tc], rhs=Sb[:, h, :],
                                                                     start=False, stop=True))
                        S.ops("pe", fns, reads=[bsa, biv[i], bSb] + [bqd[half * 4 + q_] for q_ in range(4)], writes=[bpm])
                    S.op("act", lambda: A.activation(out=f_[:, 0:4], in_=patt[:, 0:4], func=AF.Abs), reads=[bpatt], writes=[bfin])
                    S.op("dve", lambda: V.tensor_tensor(out=f_[:, 0:4], in0=f_[:, 0:4], in1=flT[:, i, :], op=ALU.max),
                         reads=[bfin, bflT], writes=[bfin])
                    S.op("dve", lambda: V.reciprocal(out=f_[:, 0:4], in_=f_[:, 0:4]), reads=[bfin], writes=[bfin])
                    for half in range(2):
                        pm, bpm = po[half]
                        S.op("act", lambda: A.activation(out=sqt[:, half * 512:(half + 1) * 512], in_=pm[:, :],
                                                         func=AF.Square), reads=[bpm], writes=[bsqt])
                    S.op("dve", lambda: V.tensor_reduce(out=f_[:, 4:8], in_=sqt[:, :].rearrange("p (h v) -> p h v", v=256),
                                                        axis=AX.X, op=ALU.add), reads=[bsqt], writes=[bfin])
                    S.op("dve", lambda: V.tensor_tensor(out=f_[:, 8:12], in0=f_[:, 0:4], in1=f_[:, 0:4], op=ALU.mult),
                         reads=[bfin], writes=[bfin])
                    S.op("dve", lambda: V.tensor_tensor(out=f_[:, 8:12], in0=f_[:, 8:12], in1=f_[:, 4:8], op=ALU.mult),
                         reads=[bfin], writes=[bfin])
                    S.op("dve", lambda: V.tensor_scalar(out=f_[:, 8:12], in0=f_[:, 8:12], scalar1=1.0 / 256, scalar2=EPS,
                                                        op0=ALU.mult, op1=ALU.add), reads=[bfin], writes=[bfin])
                    S.op("act", lambda: A.activation(out=f_[:, 8:12], in_=f_[:, 8:12], func=AF.Ln), reads=[bfin], writes=[bfin])
                    S.op("act", lambda: A.activation(out=f_[:, 8:12], in_=f_[:, 8:12], func=AF.Exp, scale=-0.5),
                         reads=[bfin], writes=[bfin])
                    S.op("dve", lambda: V.tensor_tensor(out=f_[:, 8:12], in0=f_[:, 8:12], in1=f_[:, 0:4], op=ALU.mult),
                         reads=[bfin], writes=[bfin])
                    for h in range(4):
                        pm, bpm = po[h // 2]
                        osl = slice((h % 2) * 256, (h % 2) * 256 + 256)
                        gsl = slice(h * 256, (h + 1) * 256)
                        S.op("dve", lambda: V.scalar_tensor_tensor(out=GY[:, i, gsl], in0=pm[:, osl],
                                                                   scalar=f_[:, 8 + h:9 + h], in1=GY[:, i, gsl],
                                                                   op0=ALU.mult, op1=ALU.mult),
                             reads=[bpm, bfin, bGY[i][h // 2]], writes=[bGY[i][h // 2]])
                    S.op("dve", lambda: V.tensor_tensor(out=Cst[:, 0:2, 0:256], in0=Cst[:, 0:2, 0:256],
                                                        in1=pst[:, :].rearrange("p (h v) -> p h v", v=256), op=ALU.add),
                         reads=[bC, bpst], writes=[bC])
                    S.ops("pe", [(lambda hq=hq: T.matmul(pst[:, hq * 256:(hq + 1) * 256],
                                                         lhsT=kTM[:, i, (2 + hq) * 128:(3 + hq) * 128],
                                                         rhs=v_[:, 2 + hq, 0:256], start=True, stop=True)) for hq in range(2)],
                          reads=[bkTM, bv_], writes=[bpst])
                    S.ops("pe", [(lambda h=h: T.matmul(patt[:, 8 + h:9 + h], lhsT=kTM[:, i, h * 128:(h + 1) * 128],
                                                       rhs=v_[:, h, 256:257], start=True, stop=True)) for h in range(4)],
                          reads=[bkTM, bv_], writes=[bpatt])
                    for half in range(2):
                        pm, bpm = pmm[half]
                        S.op("act", lambda: A.activation(out=sqt[:, half * 512:(half + 1) * 512], in_=pm[:, :],
                                                         func=AF.Square), reads=[bpm], writes=[bsqt])
                    S.op("dve", lambda: V.tensor_reduce(out=f_[:, 16:24], in_=sqt[:, :].rearrange("p (h v) -> p h v", v=128),
                                                        axis=AX.X, op=ALU.add), reads=[bsqt], writes=[bfin])
                    S.op("dve", lambda: V.tensor_scalar(out=f_[:, 16:24], in0=f_[:, 16:24], scalar1=1.0 / 128, scalar2=EPS,
                                                        op0=ALU.mult, op1=ALU.add), reads=[bfin], writes=[bfin])
                    S.op("act", lambda: A.activation(out=f_[:, 16:24], in_=f_[:, 16:24], func=AF.Ln), reads=[bfin], writes=[bfin])
                    S.op("act", lambda: A.activation(out=f_[:, 16:24], in_=f_[:, 16:24], func=AF.Exp, scale=-0.5),
                         reads=[bfin], writes=[bfin])
                    for h in range(8):
                        pm, bpm = pmm[h // 4]
                        osl = slice((h % 4) * 128, (h % 4) * 128 + 128)
                        gsl = slice(1024 + h * 128, 1024 + (h + 1) * 128)
                        S.op("dve", lambda: V.scalar_tensor_tensor(out=GY[:, i, gsl], in0=pm[:, osl],
                                                                   scalar=f_[:, 16 + h:17 + h], in1=GY[:, i, gsl],
                                                                   op0=ALU.mult, op1=ALU.mult),
                             reads=[bpm, bfin, bGY[i][2 + h // 4]], writes=[bGY[i][2 + h // 4]])
                    S.op("dve", lambda: V.tensor_tensor(out=Cst[:, 2:4, 0:256], in0=Cst[:, 2:4, 0:256],
                                                        in1=pst[:, :].rearrange("p (h v) -> p h v", v=256), op=ALU.add),
                         reads=[bC, bpst], writes=[bC])
                    S.op("dve", lambda: V.tensor_tensor(out=Cst[:, :, 256:257], in0=Cst[:, :, 256:257],
                                                        in1=patt[:, 8:12].unsqueeze(2), op=ALU.add),
                         reads=[bC, bpatt], writes=[bC])
                    for half in range(2):
                        S.ops("pe", [(lambda hq=hq: T.matmul(pst[:, hq * 128:(hq + 1) * 128],
                                                             lhsT=kgTM[:, i, (half * 4 + hq) * 128:(half * 4 + hq + 1) * 128],
                                                             rhs=iv[:, i, (half * 4 + hq) * 128:(half * 4 + hq + 1) * 128],
                                                             start=True, stop=True)) for hq in range(4)],
                              reads=[bkgTM[half * 4 + q_] for q_ in range(4)] + [biv[i]], writes=[bpst])
                        for hq in range(4):
                            h = half * 4 + hq
                            S.op("dve", lambda: V.scalar_tensor_tensor(out=Sst[:, h, :], in0=Sst[:, h, :],
                                                                       scalar=expg[:, h, c:c + 1],
                                                                       in1=pst[:, hq * 128:(hq + 1) * 128],
                                                                       op0=ALU.mult, op1=ALU.add),
                                 reads=[bS, bexpg[h], bpst], writes=[bS])

                def mixer_chunk(c, full):
                    if not full:
                        return mixer_chunk_so(c)
                    return mixer_chunk_full(c)
                    i = c
                    tc = slice(c * 128, (c + 1) * 128)
                    v_, bv_ = vs[vsc[0] % 2]
                    vsc[0] += 1
                    S.op("dve", lambda: V.tensor_tensor(out=v_[:], in0=vaug[:, i, :, :],
                                                        in1=eaT[:, i, :].unsqueeze(2).to_broadcast([128, 4, 258]),
                                                        op=ALU.mult), reads=[bvaug[i], beaT], writes=[bv_])
                    S.op("dve", lambda: V.tensor_tensor(out=Cst[:], in0=Cst[:],
                                                        in1=scb[:, :, c:c + 1].to_broadcast([128, 4, 258]), op=ALU.mult),
                         reads=[bC, bscb], writes=[bC])
                    if full:
                        S.op("act", lambda: A.activation(out=Cb[:], in_=Cst[:], func=AF.Copy, scale=QSCALE),
                             reads=[bC], writes=[bCb])
                        for h in range(4):
                            S.op("pe", lambda: T.matmul(patt[:, h * 128:(h + 1) * 128], lhsT=qkT[:, 4 + h, tc],
                                                        rhs=qkT[:, h, tc], start=True, stop=True),
                                 reads=[bqk[4 + h], bqk[h]], writes=[bpatt])
                        S.op("dve", lambda: V.tensor_tensor(out=stm[:].rearrange("p (h t) -> p h t", t=128),
                                                            in0=patt[:, :].rearrange("p (h t) -> p h t", t=128),
                                                            in1=maskc[:].unsqueeze(1).to_broadcast([128, 4, 128]), op=ALU.mult),
                             reads=[bpatt, bmaskc], writes=[bstm])
                        pmd, bpmd = pmm[mmc[0] % 2]
                        mmc[0] += 1
                        for h in range(4):
                            pm, bpm = po[h // 2]
                            osl = slice((h % 2) * 256, (h % 2) * 256 + 256)
                            S.op("pe", lambda: T.matmul(pm[:, osl], lhsT=stm[:, h * 128:(h + 1) * 128],
                                                        rhs=v_[:, h, 0:256], start=True, stop=False),
                                 reads=[bstm, bv_], writes=[bpm])
                            S.op("pe", lambda: T.matmul(pm[:, osl], lhsT=qkT[:, h, tc], rhs=Cb[:, h, 0:256],
                                                        start=False, stop=True), reads=[bqk[h], bCb], writes=[bpm])
                            S.op("pe", lambda: T.matmul(pmd[:, h:h + 1], lhsT=stm[:, h * 128:(h + 1) * 128],
                                                        rhs=v_[:, h, 256:257], start=True, stop=False),
                                 reads=[bstm, bv_], writes=[bpmd])
                            S.op("pe", lambda: T.matmul(pmd[:, h:h + 1], lhsT=qkT[:, h, tc], rhs=Cb[:, h, 256:257],
                                                        start=False, stop=True), reads=[bqk[h], bCb], writes=[bpmd])
                        f_ = fin
                        S.op("act", lambda: A.activation(out=f_[:, 0:4], in_=pmd[:, 0:4], func=AF.Abs),
                             reads=[bpmd], writes=[bfin])
                        S.op("dve", lambda: V.tensor_tensor(out=f_[:, 0:4], in0=f_[:, 0:4], in1=flT[:, i, :], op=ALU.max),
                             reads=[bfin, bflT], writes=[bfin])
                        S.op("dve", lambda: V.reciprocal(out=f_[:, 0:4], in_=f_[:, 0:4]), reads=[bfin], writes=[bfin])
                        for half in range(2):
                            pm, bpm = po[half]
                            S.op("act", lambda: A.activation(out=sqt[:, half * 512:(half + 1) * 512], in_=pm[:, :],
                                                             func=AF.Square), reads=[bpm], writes=[bsqt])
                        S.op("dve", lambda: V.tensor_reduce(out=f_[:, 4:8], in_=sqt[:, :].rearrange("p (h v) -> p h v", v=256),
                                                            axis=AX.X, op=ALU.add), reads=[bsqt], writes=[bfin])
                        S.op("dve", lambda: V.tensor_tensor(out=f_[:, 8:12], in0=f_[:, 0:4], in1=f_[:, 0:4], op=ALU.mult),
                             reads=[bfin], writes=[bfin])
                        S.op("dve", lambda: V.tensor_tensor(out=f_[:, 8:12], in0=f_[:, 8:12], in1=f_[:, 4:8], op=ALU.mult),
                             reads=[bfin], writes=[bfin])
                        S.op("dve", lambda: V.tensor_scalar(out=f_[:, 8:12], in0=f_[:, 8:12], scalar1=1.0 / 256, scalar2=EPS,
                                                            op0=ALU.mult, op1=ALU.add), reads=[bfin], writes=[bfin])
                        S.op("act", lambda: A.activation(out=f_[:, 8:12], in_=f_[:, 8:12], func=AF.Ln),
                             reads=[bfin], writes=[bfin])
                        S.op("act", lambda: A.activation(out=f_[:, 8:12], in_=f_[:, 8:12], func=AF.Exp, scale=-0.5),
                             reads=[bfin], writes=[bfin])
                        S.op("dve", lambda: V.tensor_tensor(out=f_[:, 8:12], in0=f_[:, 8:12], in1=f_[:, 0:4], op=ALU.mult),
                             reads=[bfin], writes=[bfin])
                        for h in range(4):
                            pm, bpm = po[h // 2]
                            osl = slice((h % 2) * 256, (h % 2) * 256 + 256)
                            gsl = slice(h * 256, (h + 1) * 256)
                            S.op("dve", lambda: V.scalar_tensor_tensor(out=GY[:, i, gsl], in0=pm[:, osl],
                                                                       scalar=f_[:, 8 + h:9 + h], in1=GY[:, i, gsl],
                                                                       op0=ALU.mult, op1=ALU.mult),
                                 reads=[bpm, bfin, bGY[i][h // 2]], writes=[bGY[i][h // 2]])
                    for half in range(2):
                        for hq in range(2):
                            h = half * 2 + hq
                            S.op("pe", lambda: T.matmul(pst[:, hq * 256:(hq + 1) * 256], lhsT=kTM[:, i, h * 128:(h + 1) * 128],
                                                        rhs=v_[:, h, 0:256], start=True, stop=True),
                                 reads=[bkTM, bv_], writes=[bpst])
                        S.op("dve", lambda: V.tensor_tensor(out=Cst[:, half * 2:half * 2 + 2, 0:256],
                                                            in0=Cst[:, half * 2:half * 2 + 2, 0:256],
                                                            in1=pst[:, :].rearrange("p (h v) -> p h v", v=256), op=ALU.add),
                             reads=[bC, bpst], writes=[bC])
                    pmn, bpmn = pmm[mmc[0] % 2]
                    mmc[0] += 1
                    for h in range(4):
                        S.op("pe", lambda: T.matmul(pmn[:, h:h + 1], lhsT=kTM[:, i, h * 128:(h + 1) * 128],
                                                    rhs=v_[:, h, 256:257], start=True, stop=True),
                             reads=[bkTM, bv_], writes=[bpmn])
                    S.op("dve", lambda: V.tensor_tensor(out=Cst[:, :, 256:257], in0=Cst[:, :, 256:257],
                                                        in1=pmn[:, 0:4].unsqueeze(2), op=ALU.add),
                         reads=[bC, bpmn], writes=[bC])
                    if full:
                        for h in range(8):
                            S.op("act", lambda: A.activation(out=Sb[:, h, :], in_=Sst[:, h, :], func=AF.Copy,
                                                             scale=eref[:, h, c:c + 1]), reads=[bS, bexpg[h]], writes=[bSb])
                        for half in range(2):
                            for hq in range(4):
                                h = half * 4 + hq
                                S.op("pe", lambda: T.matmul(patt[:, hq * 128:(hq + 1) * 128], lhsT=kd[:, h, tc], rhs=qd[:, h, tc],
                                                            start=True, stop=True), reads=[bkd[h], bqd[h]], writes=[bpatt])
                            S.op("dve", lambda: V.tensor_tensor(out=stm[:].rearrange("p (h t) -> p h t", t=128),
                                                                in0=patt[:, :].rearrange("p (h t) -> p h t", t=128),
                                                                in1=mask1[:].unsqueeze(1).to_broadcast([128, 4, 128]),
                                                                op=ALU.mult), reads=[bpatt, bmask1], writes=[bstm])
                            pm, bpm = po[half]
                            for hq in range(4):
                                h = half * 4 + hq
                                osl = slice(hq * 128, hq * 128 + 128)
                                S.op("pe", lambda: T.matmul(pm[:, osl], lhsT=stm[:, hq * 128:(hq + 1) * 128],
                                                            rhs=iv[:, i, h * 128:(h + 1) * 128], start=True, stop=False),
                                     reads=[bstm, biv[i]], writes=[bpm])
                                S.op("pe", lambda: T.matmul(pm[:, osl], lhsT=qd[:, h, tc], rhs=Sb[:, h, :],
                                                            start=False, stop=True), reads=[bqd[h], bSb], writes=[bpm])
                        f_ = fin
                        for half in range(2):
                            pm, bpm = po[half]
                            S.op("act", lambda: A.activation(out=sqt[:, half * 512:(half + 1) * 512], in_=pm[:, :],
                                                             func=AF.Square), reads=[bpm], writes=[bsqt])
                        S.op("dve", lambda: V.tensor_reduce(out=f_[:, 16:24], in_=sqt[:, :].rearrange("p (h v) -> p h v", v=128),
                                                            axis=AX.X, op=ALU.add), reads=[bsqt], writes=[bfin])
                        S.op("dve", lambda: V.tensor_scalar(out=f_[:, 16:24], in0=f_[:, 16:24], scalar1=1.0 / 128, scalar2=EPS,
                                                            op0=ALU.mult, op1=ALU.add), reads=[bfin], writes=[bfin])
                        S.op("act", lambda: A.activation(out=f_[:, 16:24], in_=f_[:, 16:24], func=AF.Ln),
                             reads=[bfin], writes=[bfin])
                        S.op("act", lambda: A.activation(out=f_[:, 16:24], in_=f_[:, 16:24], func=AF.Exp, scale=-0.5),
                             reads=[bfin], writes=[bfin])
                        for h in range(8):
                            pm, bpm = po[h // 4]
                            osl = slice((h % 4) * 128, (h % 4) * 128 + 128)
                            gsl = slice(1024 + h * 128, 1024 + (h + 1) * 128)
                            S.op("dve", lambda: V.scalar_tensor_tensor(out=GY[:, i, gsl], in0=pm[:, osl],
                                                                       scalar=f_[:, 16 + h:17 + h], in1=GY[:, i, gsl],
                                                                       op0=ALU.mult, op1=ALU.mult),
                                 reads=[bpm, bfin, bGY[i][2 + h // 4]], writes=[bGY[i][2 + h // 4]])
                    for half in range(2):
                        for hq in range(4):
                            h = half * 4 + hq
                            S.op("pe", lambda: T.matmul(pst[:, hq * 128:(hq + 1) * 128], lhsT=kgTM[:, i, h * 128:(h + 1) * 128],
                                                        rhs=iv[:, i, h * 128:(h + 1) * 128], start=True, stop=True),
                                 reads=[bkgTM[h], biv[i]], writes=[bpst])
                        for hq in range(4):
                            h = half * 4 + hq
                            S.op("dve", lambda: V.scalar_tensor_tensor(out=Sst[:, h, :], in0=Sst[:, h, :],
                                                                       scalar=expg[:, h, c:c + 1],
                                                                       in1=pst[:, hq * 128:(hq + 1) * 128],
                                                                       op0=ALU.mult, op1=ALU.add),
                                 reads=[bS, bexpg[h], bpst], writes=[bS])

                def run_pass(full, src_d, ngroups, flagcol=None):
                    def phase_a(g_):
                        ta = g_ * G
                        for i in range(NT):
                            S.dma("act", "ldx%d_%d" % (cur_par, i), XH[:, i, :], src_d[ta + i * 128:ta + (i + 1) * 128, :], writes=[bXHt[i]])
                        for i in range(NT):
                            norm_transpose(XH[:, i, :], bXHt[i], i, nwc, bnwc)

                    for g in range(ngroups):
                        t0 = g * G
                        cur_flag[0] = None if flagcol is None else flagcol(t0)
                        select(g)
                        if g == 0:
                            phase_a(g)
                        S.mark("phaseA")
                        wt, bw = load_w(win_d[:, C_I:C_I + 8], ncols=8, key="gates", rowscale=(nwc, bnwc))
                        pm_i, bpm_i = mm_fm(wt, bw, 0, 4)
                        pm_f, bpm_f = mm_fm(wt, bw, 4, 4)
                        gates_rows(pm_i, bpm_i, pm_f, bpm_f)
                        S.mark("gates")
                        for blk in ((0, 1) if full else (1,)):
                            wt, bw = load_w(win_d[:, blk * 512:(blk + 1) * 512], key="qk%d" % blk, rowscale=(nwc, bnwc))
                            for j in range(4):
                                pm, bpm = mm_fm(wt, bw, j * 128, 128)
                                conv_block(pm, bpm, blk * 4 + j)
                        S.mark("qk")
                        if (not full) and g == ngroups - 1:
                            wt, bw = load_w(win_d[:, 0:512], key="qk0", rowscale=(nwc, bnwc))
                            for j in range(4):
                                pm, bpm = mm_fm(wt, bw, j * 128, 128, ntoks=128, tok0=G - 128)
                                conv_block(pm, bpm, j, ntoks=128, only_halo=True)
                        S.mark("ktm")
                        for cb in range(2):
                            wt, bw = load_w(win_d[:, C_V + cb * 512:C_V + (cb + 1) * 512], key="v%d" % cb, rowscale=(nwc, bnwc))
                            for i in range(NT):
                                pm, bpm = mm_tm(i, wt, bw)
                                S.op("act", lambda: A.copy(out=vaug[:, i, 2 * cb:2 * cb + 2, 0:256],
                                                           in_=pm[:, :].rearrange("p (h v) -> p h v", v=256)),
                                     reads=[bpm], writes=[bvaug[i]])
                        S.mark("v")
                        if full:
                            for cb in range(2):
                                wt, bw = load_w(win_d[:, C_O + cb * 512:C_O + (cb + 1) * 512], key="o%d" % cb, rowscale=(nwc, bnwc))
                                for i in range(NT):
                                    pm, bpm = mm_tm(i, wt, bw)
                                    S.op("act", lambda: A.activation(out=GY[:, i, cb * 512:(cb + 1) * 512], in_=pm[:, :],
                                                                     func=AF.Sigmoid), reads=[bpm], writes=[bGY[i][cb]])
                            for cb in range(2):
                                wt, bw = load_w(win_d[:, C_Z + cb * 512:C_Z + (cb + 1) * 512], key="z%d" % cb, rowscale=(nwc, bnwc))
                                for i in range(NT):
                                    pm, bpm = mm_tm(i, wt, bw)
                                    zt, bzt = ztmp[(cb * NT + i) % 2]
                                    S.op("act", lambda: A.activation(out=zt[:], in_=pm[:, :], func=AF.Silu),
                                         reads=[bpm], writes=[bzt])
                                    S.op("dve", lambda: V.tensor_tensor(out=GY[:, i, cb * 512:(cb + 1) * 512],
                                                                        in0=GY[:, i, cb * 512:(cb + 1) * 512], in1=zt[:],
                                                                        op=ALU.mult), reads=[bzt, bGY[i][cb]], writes=[bGY[i][cb]])
                        S.mark("oz")
                        gates_bcast()
                        for hb in range(2):
                            wt, bw = load_w(win_d[:, C_HF + hb * 512:C_HF + (hb + 1) * 512], key="hf%d" % hb, rowscale=(nwc, bnwc))
                            if full:
                                wtq, bwq = load_w(win_d[:, C_HQ + hb * 512:C_HQ + (hb + 1) * 512], key="hq%d" % hb, rowscale=(nwc, bnwc))
                            for j in range(4):
                                h = hb * 4 + j
                                pm, bpm = mm_fm(wt, bw, j * 128, 128)
                                hf_block(pm, bpm, h, full)
                                if full:
                                    pm, bpm = mm_fm(wtq, bwq, j * 128, 128)
                                    S.op("dve", lambda: V.tensor_tensor(out=qd[:, h, :], in0=pm[:, 0:G], in1=ebf[:, h % 2, :],
                                                                        op=ALU.mult), reads=[bpm, beb[h % 2]], writes=[bqd[h]])
                        S.mark("hgrn")
                        for cb in range(2):
                            wt, bw = load_w(win_d[:, C_HI + cb * 512:C_HI + (cb + 1) * 512], key="hi%d" % cb, rowscale=(nwc, bnwc))
                            for i in range(NT):
                                pm, bpm = mm_tm(i, wt, bw)
                                S.op("act", lambda: A.copy(out=iv[:, i, cb * 512:(cb + 1) * 512], in_=pm[:, :]),
                                     reads=[bpm], writes=[biv[i]])
                        if full:
                            for cb in range(2):
                                wt, bw = load_w(win_d[:, C_HG + cb * 512:C_HG + (cb + 1) * 512], key="hg%d" % cb, rowscale=(nwc, bnwc))
                                for i in range(NT):
                                    pm, bpm = mm_tm(i, wt, bw)
                                    S.op("act", lambda: A.activation(out=GY[:, i, 1024 + cb * 512:1024 + (cb + 1) * 512],
                                                                     in_=pm[:, :], func=AF.Silu),
                                         reads=[bpm], writes=[bGY[i][2 + cb]])
                        S.mark("hi_hg")
                        k_to_tm()
                        for h_ in range(8):
                            kg_to_tm(h_)
                        if g + 1 < ngroups:
                            select(g + 1)
                            phase_a(g + 1)
                            select(g)
                        for c in range(NCH):
                            mixer_chunk(c, full)
                            S.mark("mix%d" % c)
                        S.mark("mixer")
                        if not full:
                            continue
                        for i in range(NT):
                            for hb in range(2):
                                pt, bpt = ptr[tcount[0] % 2]
                                tcount[0] += 1
                                S.ops("pe", [(lambda j=j: T.transpose(out=pt[:, j, :],
                                                                      in_=GY[:, i, (hb * 8 + j) * 128:(hb * 8 + j + 1) * 128],
                                                                      identity=ident[:])) for j in range(8)],
                                      reads=[bGY[i][2 * hb], bGY[i][2 * hb + 1], bident], writes=[bpt])
                                if hb == 0:
                                    S.op("act", lambda: A.copy(out=actT[:, 0:8, i * 128:(i + 1) * 128], in_=pt[:, :, :]),
                                         reads=[bpt], writes=[bactT])
                                else:
                                    S.op("dve", lambda: V.tensor_copy(out=actT[:, 8:16, i * 128:(i + 1) * 128], in_=pt[:, :, :]),
                                         reads=[bpt], writes=[bactT])
                        S.mark("yT")
                        for cb in range(4):
                            wt, bw = load_w(wout_d[:, cb * 512:(cb + 1) * 512], key="wo%d" % cb, rowscale=(mixc, bmixc))
                            for i in range(NT):
                                pm, bpm = mm_tm(i, wt, bw)
                                S.op("dve", lambda: V.tensor_tensor(out=XH[:, i, cb * 512:(cb + 1) * 512],
                                                                    in0=XH[:, i, cb * 512:(cb + 1) * 512], in1=pm[:, :],
                                                                    op=ALU.add), reads=[bpm, bXHt[i]], writes=[bXHt[i]])
                        S.mark("wout")
                        for i in range(NT):
                            norm_transpose(XH[:, i, :], bXHt[i], i, pnc, bpnc)
                        S.mark("hT")
                        for i in range(NT):
                            pl, bpl = ptile[i % 2]
                            pb_, bpb_ = pbt[i % 2]
                            S.dma("act", "ldp%d" % (i % 2), pl[:], p_d[t0 + i * 128:t0 + (i + 1) * 128, :], writes=[bpl])
                            S.op("dve", lambda: V.tensor_copy(out=pb_[:], in_=pl[:]), reads=[bpl], writes=[bpb_])
                            pt, bpt = ptr[tcount[0] % 2]
                            tcount[0] += 1
                            for kk in range(2):
                                S.op("pe", lambda: T.transpose(out=pt[:, kk, :], in_=pb_[:, kk * 128:(kk + 1) * 128],
                                                               identity=ident[:]), reads=[bpb_, bident], writes=[bpt])
                            S.op("act", lambda: A.copy(out=pTt[:, :, i * 128:(i + 1) * 128], in_=pt[:, 0:2, :]),
                                 reads=[bpt], writes=[bpT])
                        S.mark("pT")
                        for cb in range(4):
                            wt, bw = load_w(wpg_d[:, cb * 512:(cb + 1) * 512], key="pg%d" % cb, rowscale=(pnc, bpnc))
                            wt2, bw2 = load_w(wpe_d[:, cb * 512:(cb + 1) * 512], kchunks=2, key="pe%d" % cb)
                            for i in range(NT):
                                pm, bpm = mm_tm(i, wt, bw)
                                g_, bg_ = gt[i % 2]
                                S.op("act", lambda: A.activation(out=g_[:], in_=pm[:, :], func=AF.Sigmoid),
                                     reads=[bpm], writes=[bg_])
                                pm2, bpm2 = mm_tm(i, wt2, bw2, kchunks=2, lhs=pTt, blhs=bpT)
                                S.op("dve", lambda: V.tensor_tensor(out=g_[:], in0=g_[:], in1=pm2[:, :], op=ALU.mult),
                                     reads=[bg_, bpm2], writes=[bg_])
                                S.op("dve", lambda: V.tensor_tensor(out=XH[:, i, cb * 512:(cb + 1) * 512],
                                                                    in0=XH[:, i, cb * 512:(cb + 1) * 512], in1=g_[:],
                                                                    op=ALU.add), reads=[bg_, bXHt[i]], writes=[bXHt[i]])
                        S.mark("gate_e")
                        for i in range(NT):
                            u, bu = ub[i % 2]
                            S.op("act", lambda: A.activation(out=u[:], in_=XH[:, i, :], func=AF.Square,
                                                             accum_out=ssq[:, 4 + i:5 + i]),
                                 reads=[bXHt[i]], writes=[bu, bssq])
                            rstd_from_ssq(4 + i, D)
                            S.op("dve", lambda: V.scalar_tensor_tensor(out=XH[:, i, :], in0=XH[:, i, :],
                                                                       scalar=rstd[:, 4 + i:5 + i], in1=fnw[:],
                                                                       op0=ALU.mult, op1=ALU.mult),
                                 reads=[bXHt[i], brstd, bfnw], writes=[bXHt[i]])
                            S.dma("act", "sto%d_%d" % (cur_par, i), out_d[t0 + i * 128:t0 + (i + 1) * 128, :], XH[:, i, :], reads=[bXHt[i]])

                run_pass(full, src_d, ngroups_, flagcol_)
                S.barrier()

        def zero_state():
            S.op("dve", lambda: V.memset(Cst[:], 0.0), writes=[bC])
            S.op("dve", lambda: V.memset(Sst[:], 0.0), writes=[bS])
            S.op("dve", lambda: V.memset(Sb[:], 0.0), writes=[bSb])
            S.op("dve", lambda: V.memset(mcar[:], 0.0), writes=[bmcar])

        zero_state()
        S.op("dve", lambda: V.memset(Gseg[:], 0.0), writes=[bGseg])
        S.op("dve", lambda: V.memset(gsum[:], 0.0), writes=[bgsum])
        if nprev > 0:
            emit_pass(False, NT_SO, xprev_d, nprev * ntok // (128 * NT_SO), lambda t0: t0 // ntok)
        emit_pass(True, NT, x_d, ntok // (128 * NT))
    build_program.last_ninst = dict(S.ninst)
    return nc


_CFG = dict(ntok=SEG, NT=2, NT_SO=4, nprev=3)


def _flags_for(sgm, nprev):
    f = np.zeros((128, 16), np.float32)
    for j in range(nprev):
        if sgm - nprev + j >= 0:
            f[:, j] = 1.0
    return f


def kernel(**inputs):
    x = np.asarray(inputs["x"], np.float32)
    p = np.asarray(inputs["p"], np.float32)
    B, SQ, _ = x.shape
    nseg = NCORES // B
    seg = SQ // nseg
    cfg = dict(_CFG)
    cfg["ntok"] = seg
    cfg["nprev"] = nseg - 1
    nprev = cfg["nprev"]
    nc = build_program(**cfg)
    shared = {}
    for k in ["norm_w", "w_in", "conv_w", "conv_b", "ml_b_i", "ml_b_f", "ml_norm_w", "hg_norm_w", "w_out",
              "pe_norm_w", "w_pg", "w_pe"]:
        shared[k] = np.ascontiguousarray(np.asarray(inputs[k], np.float32)[0])
    shared["hg_lb"] = np.ascontiguousarray(np.asarray(inputs["hg_lb"], np.float32))
    shared["final_norm_w"] = np.ascontiguousarray(np.asarray(inputs["final_norm_w"], np.float32))
    in_maps = []
    for c in range(NCORES):
        b, sgm = c // nseg, c % nseg
        m = dict(shared)
        m["x"] = np.ascontiguousarray(x[b, sgm * seg:(sgm + 1) * seg])
        xp = np.zeros((nprev * seg, D), np.float32)
        if sgm > 0:
            xp[(nprev - sgm) * seg:] = x[b, 0:sgm * seg]
        m["xprev"] = xp
        m["p"] = np.ascontiguousarray(p[0, b, sgm * seg:(sgm + 1) * seg])
        m["flags"] = _flags_for(sgm, nprev)
        in_maps.append(m)
    res = run_bass_kernel_spmd(nc, in_maps, core_ids=list(range(NCORES)))
    out = np.empty((B, SQ, D), np.float32)
    for c in range(NCORES):
        b, sgm = c // nseg, c % nseg
        out[b, sgm * seg:(sgm + 1) * seg] = res.results[c]["out"]
    return out
```
